# Optimizing a Trainium2 kernel written in Bass

```python
import math
import jax, jax.numpy as jnp
from jax import lax
import numpy as np

D_MODEL = 4096
BATCH = 1
SEQ = 16384
DEPTH = 1

HEAD_DIM = 128
DSA_HEADS = 16
DSA_KV_HEADS = 4
IDX_HEADS = 32
IDX_DIM = 64
DSA_TOPK = 256
NSA_HEADS = 16
NSA_KV_HEADS = 4
CMP_LEN = 32
CMP_STRIDE = 16
CMP_HIDDEN = 256
SLC_LEN = 64
SLC_TOP = 16
WIN = 512
FORCE_SCORE = 1e9
PEER_HEADS = 8
PEER_NKEYS = 128
PEER_QDIM = 256
PEER_TOPK = 16
N_EXPERTS = PEER_NKEYS * PEER_NKEYS
REL_BUCKETS = 32
REL_MAX_DIST = 2048
N_ATTN_HEADS = DSA_HEADS + NSA_HEADS
QBLK = 128
EPS = 1e-6
IN_SPLITS = (DSA_HEADS * HEAD_DIM, DSA_KV_HEADS * HEAD_DIM, DSA_KV_HEADS * HEAD_DIM,
             IDX_HEADS * IDX_DIM, IDX_DIM, IDX_HEADS,
             NSA_HEADS * HEAD_DIM, 6 * NSA_KV_HEADS * HEAD_DIM, 3 * NSA_HEADS,
             2 * D_MODEL)
IN_COLS = sum(IN_SPLITS)

kernel_name = 'hybrid_dsa_nsa_peer_block'


def rms_norm(x, g):
    xf = x.astype(jnp.float32)
    y = xf * lax.rsqrt(jnp.mean(xf * xf, axis=-1, keepdims=True) + EPS)
    return (y * g.astype(jnp.float32)).astype(x.dtype)


def rel_bucket(dist):
    n = jnp.maximum(dist, 0)
    exact = REL_BUCKETS // 2
    nf = jnp.maximum(n, 1).astype(jnp.float32)
    log_b = exact + (jnp.log(nf / exact) / math.log(REL_MAX_DIST / exact) * (REL_BUCKETS - exact)).astype(jnp.int32)
    return jnp.where(n < exact, n, jnp.minimum(log_b, REL_BUCKETS - 1))


def masked_softmax(logits, mask):
    s = jnp.where(mask, logits.astype(jnp.float32), -jnp.inf)
    m = jnp.max(s, axis=-1, keepdims=True)
    m = jnp.where(jnp.isfinite(m), m, 0.0)
    e = jnp.exp(s - m)
    return e / jnp.maximum(jnp.sum(e, axis=-1, keepdims=True), 1e-30)


def map_blocks(fn, n_tokens):
    nb = n_tokens // QBLK
    out = lax.map(fn, jnp.arange(nb) * QBLK)
    out = jnp.moveaxis(out, 0, 1)
    return out.reshape(out.shape[0], nb * QBLK, *out.shape[3:])


def dsa_mixer(q, k, v, qi, ki, wi, table):
    B, T, H, dh = q.shape
    G = k.shape[2]
    R = H // G
    n_top = min(DSA_TOPK, T // 4)
    key_pos = jnp.arange(T)
    scale = dh ** -0.5
    wi = wi * IDX_HEADS ** -0.5

    def block(q0):
        tq = q0 + jnp.arange(QBLK)
        qi_b = lax.dynamic_slice_in_dim(qi, q0, QBLK, axis=1)
        wi_b = lax.dynamic_slice_in_dim(wi, q0, QBLK, axis=1)
        s = jnp.einsum('bqhd,bkd->bqhk', qi_b, ki) * IDX_DIM ** -0.5
        score = jnp.einsum('bqhk,bqh->bqk', jax.nn.relu(s), wi_b).astype(jnp.float32)
        score = jnp.where((key_pos[None, :] <= tq[:, None])[None], score, -jnp.inf)
        _, idx = lax.top_k(score, n_top)
        valid = idx <= tq[None, :, None]
        k_sel = jax.vmap(lambda a, i: a[i])(k, idx)
        v_sel = jax.vmap(lambda a, i: a[i])(v, idx)
        q_b = lax.dynamic_slice_in_dim(q, q0, QBLK, axis=1).reshape(B, QBLK, G, R, dh)
        logits = jnp.einsum('bqgrd,bqkgd->bqgrk', q_b, k_sel).astype(jnp.float32) * scale
        bias = table[rel_bucket(tq[None, :, None] - idx)]
        bias = jnp.moveaxis(bias.reshape(B, QBLK, n_top, G, R), 2, -1)
        p = masked_softmax(logits + bias, valid[:, :, None, None, :])
        o = jnp.einsum('bqgrk,bqkgd->bqgrd', p.astype(v.dtype), v_sel)
        return o.reshape(B, QBLK, H * dh)

    return map_blocks(block, T)


def compress_blocks(x, pe, w1, w2):
    T = x.shape[1]
    n_cmp = (T - CMP_LEN) // CMP_STRIDE + 1
    pos = (jnp.arange(n_cmp) * CMP_STRIDE)[:, None] + jnp.arange(CMP_LEN)[None, :]
    blk = x[:, pos] + pe[None, None, :, None, :]
    hid = jax.nn.gelu(jnp.einsum('bnlgd,lde->bnge', blk, w1))
    return jnp.einsum('bnge,ed->bngd', hid, w2)


def nsa_mixer(q, k_cmp, v_cmp, ks, vs, kw, vw, gates, table):
    B, T, H, dh = q.shape
    G = ks.shape[2]
    R = H // G
    n_cmp = k_cmp.shape[1]
    n_slc = T // SLC_LEN
    n_sel = min(SLC_TOP, n_slc)
    scale = dh ** -0.5
    cmp_end = jnp.arange(n_cmp) * CMP_STRIDE + CMP_LEN - 1
    c_start = jnp.arange(n_cmp)[:, None] * CMP_STRIDE
    s_start = jnp.arange(n_slc)[None, :] * SLC_LEN
    overlap = ((c_start < s_start + SLC_LEN) & (c_start + CMP_LEN > s_start)).astype(jnp.float32)
    tbl = table.reshape(REL_BUCKETS, G, R)
    ks_blk = jnp.moveaxis(ks.reshape(B, n_slc, SLC_LEN, G, dh), 3, 1)
    vs_blk = jnp.moveaxis(vs.reshape(B, n_slc, SLC_LEN, G, dh), 3, 1)
    kw_pad = jnp.pad(kw, ((0, 0), (WIN, 0), (0, 0), (0, 0)))
    vw_pad = jnp.pad(vw, ((0, 0), (WIN, 0), (0, 0), (0, 0)))
    b_idx = jnp.arange(B)[:, None, None, None]
    g_idx = jnp.arange(G)[None, None, :, None]
    blk_id = jnp.arange(n_slc)

    def block(q0):
        tq = q0 + jnp.arange(QBLK)
        q_b = lax.dynamic_slice_in_dim(q, q0, QBLK, axis=1).reshape(B, QBLK, G, R, dh)
        lc = jnp.einsum('bqgrd,bngd->bqgrn', q_b, k_cmp).astype(jnp.float32) * scale
        dist_c = tq[:, None] - cmp_end[None, :]
        lc = lc + jnp.transpose(tbl[rel_bucket(dist_c)], (0, 2, 3, 1))
        pc = masked_softmax(lc, (dist_c >= 0)[:, None, None, :])
        o_c = jnp.einsum('bqgrn,bngd->bqgrd', pc.astype(v_cmp.dtype), v_cmp)
        imp = jnp.einsum('bqgn,nm->bqgm', pc.sum(axis=3), overlap)
        cur = tq // SLC_LEN
        forced = (blk_id[None, :] == 0) | (blk_id[None, :] == cur[:, None]) | (blk_id[None, :] == cur[:, None] - 1)
        admissible = blk_id[None, :] * SLC_LEN <= tq[:, None]
        imp = jnp.where(forced[None, :, None, :], FORCE_SCORE, imp)
        imp = jnp.where(admissible[None, :, None, :], imp, -jnp.inf)
        _, sel = lax.top_k(imp, n_sel)
        k_sel = ks_blk[b_idx, g_idx, sel]
        v_sel = vs_blk[b_idx, g_idx, sel]
        pos = sel[..., None] * SLC_LEN + jnp.arange(SLC_LEN)
        dist_s = tq[None, :, None, None, None] - pos
        ls = jnp.einsum('bqgrd,bqgnld->bqgrnl', q_b, k_sel).astype(jnp.float32) * scale
        bias_s = tbl[rel_bucket(dist_s), g_idx[..., None]]
        ls = (ls + jnp.moveaxis(bias_s, -1, 3)).reshape(B, QBLK, G, R, n_sel * SLC_LEN)
        ps = masked_softmax(ls, (dist_s >= 0).reshape(B, QBLK, G, 1, n_sel * SLC_LEN))
        o_s = jnp.einsum('bqgrk,bqgkd->bqgrd', ps.astype(vs.dtype), v_sel.reshape(B, QBLK, G, n_sel * SLC_LEN, dh))
        kw_b = lax.dynamic_slice_in_dim(kw_pad, q0, WIN + QBLK, axis=1)
        vw_b = lax.dynamic_slice_in_dim(vw_pad, q0, WIN + QBLK, axis=1)
        kpos = q0 - WIN + jnp.arange(WIN + QBLK)
        dist_w = tq[:, None] - kpos[None, :]
        mask_w = (dist_w >= 0) & (dist_w < WIN) & (kpos[None, :] >= 0)
        lw = jnp.einsum('bqgrd,bkgd->bqgrk', q_b, kw_b).astype(jnp.float32) * scale
        lw = lw + jnp.transpose(tbl[rel_bucket(dist_w)], (0, 2, 3, 1))
        pw = masked_softmax(lw, mask_w[:, None, None, :])
        o_w = jnp.einsum('bqgrk,bkgd->bqgrd', pw.astype(vw.dtype), vw_b)
        g_b = jax.nn.sigmoid(lax.dynamic_slice_in_dim(gates, q0, QBLK, axis=1).astype(jnp.float32)).reshape(B, QBLK, G, R, 3)
        o = g_b[..., 0:1] * o_c + g_b[..., 1:2] * o_s + g_b[..., 2:3] * o_w
        return o.astype(q.dtype).reshape(B, QBLK, H * dh)

    return map_blocks(block, T)


def token_mixer(h, table, w_in, gq_a, gk_a, gq_b, gk_cmp, gk_slc, gk_win, cmp_pe_k, cmp_w1_k, cmp_w2_k,
                cmp_pe_v, cmp_w1_v, cmp_w2_v, w_branch_a, w_branch_b, w_out):
    B, T, _ = h.shape
    proj = h @ w_in
    offs = [int(o) for o in np.cumsum(IN_SPLITS)[:-1]]
    qa, ka, va, qi, ki, wi, qb, kvb, gb, gm = jnp.split(proj, offs, axis=-1)
    qa = rms_norm(qa.reshape(B, T, DSA_HEADS, HEAD_DIM), gq_a)
    ka = rms_norm(ka.reshape(B, T, DSA_KV_HEADS, HEAD_DIM), gk_a)
    va = va.reshape(B, T, DSA_KV_HEADS, HEAD_DIM)
    y_a = dsa_mixer(qa, ka, va, qi.reshape(B, T, IDX_HEADS, IDX_DIM), ki, wi, table[:, :DSA_HEADS])
    qb = rms_norm(qb.reshape(B, T, NSA_HEADS, HEAD_DIM), gq_b)
    kvb = kvb.reshape(B, T, 6, NSA_KV_HEADS, HEAD_DIM)
    k_cmp = rms_norm(compress_blocks(kvb[:, :, 0], cmp_pe_k, cmp_w1_k, cmp_w2_k), gk_cmp)
    v_cmp = compress_blocks(kvb[:, :, 1], cmp_pe_v, cmp_w1_v, cmp_w2_v)
    y_b = nsa_mixer(qb, k_cmp, v_cmp, rms_norm(kvb[:, :, 2], gk_slc), kvb[:, :, 3],
                    rms_norm(kvb[:, :, 4], gk_win), kvb[:, :, 5], gb, table[:, DSA_HEADS:])
    g_a, g_b = jnp.split(jax.nn.sigmoid(gm), 2, axis=-1)
    merged = g_a * (y_a @ w_branch_a) + g_b * (y_b @ w_branch_b)
    return merged @ w_out


def peer_ffn(h, w_q, sub_keys, u, v):
    B, T, _ = h.shape
    q = (h @ w_q).reshape(B, T, PEER_HEADS, 2, PEER_QDIM // 2)
    s = jnp.einsum('bthcd,hcnd->bthcn', q, sub_keys).astype(jnp.float32)
    s1, i1 = lax.top_k(s[..., 0, :], PEER_TOPK)
    s2, i2 = lax.top_k(s[..., 1, :], PEER_TOPK)
    cand = (s1[..., :, None] + s2[..., None, :]).reshape(B, T, PEER_HEADS, PEER_TOPK * PEER_TOPK)
    cand_id = (i1[..., :, None] * PEER_NKEYS + i2[..., None, :]).reshape(B, T, PEER_HEADS, PEER_TOPK * PEER_TOPK)
    top_s, top_pos = lax.top_k(cand, PEER_TOPK)
    expert = jnp.take_along_axis(cand_id, top_pos, axis=-1).reshape(B, T, PEER_HEADS * PEER_TOPK)
    gate = jax.nn.softmax(top_s, axis=-1).reshape(B, T, PEER_HEADS * PEER_TOPK)

    def block(t0):
        h_b = lax.dynamic_slice_in_dim(h, t0, QBLK, axis=1)
        e_b = lax.dynamic_slice_in_dim(expert, t0, QBLK, axis=1)
        g_b = lax.dynamic_slice_in_dim(gate, t0, QBLK, axis=1)
        a = jnp.einsum('bqkd,bqd->bqk', u[e_b], h_b).astype(jnp.float32)
        act = (jax.nn.gelu(a) * g_b).astype(h.dtype)
        return jnp.einsum('bqk,bqkd->bqd', act, v[e_b])

    return map_blocks(block, T)


def setup_inputs(seed: int = 0) -> dict:
    key = jax.random.key(seed)
    ks = jax.random.split(key, 32)
    D, dh, L = D_MODEL, HEAD_DIM, DEPTH

    def nrm(k, shape, scale):
        return jax.random.normal(k, shape, jnp.float32) * scale

    return {
        'x': nrm(ks[0], (BATCH, SEQ, D), 1.0),
        'c': nrm(ks[1], (BATCH, D), 1.0),
        'rel_bias': nrm(ks[2], (REL_BUCKETS, N_ATTN_HEADS), 0.5),
        'w_ada': nrm(ks[3], (L, D, 6 * D), 0.5 * D ** -0.5),
        'b_ada': nrm(ks[4], (L, 6 * D), 0.02),
        'g_mix': 1.0 + nrm(ks[5], (L, D), 0.02),
        'w_in': nrm(ks[6], (L, D, IN_COLS), D ** -0.5),
        'gq_a': 1.0 + nrm(ks[7], (L, dh), 0.02),
        'gk_a': 1.0 + nrm(ks[8], (L, dh), 0.02),
        'gq_b': 1.0 + nrm(ks[9], (L, dh), 0.02),
        'gk_cmp': 1.0 + nrm(ks[10], (L, dh), 0.02),
        'gk_slc': 1.0 + nrm(ks[11], (L, dh), 0.02),
        'gk_win': 1.0 + nrm(ks[12], (L, dh), 0.02),
        'cmp_pe_k': nrm(ks[13], (L, CMP_LEN, dh), 0.1),
        'cmp_w1_k': nrm(ks[14], (L, CMP_LEN, dh, CMP_HIDDEN), (CMP_LEN * dh) ** -0.5),
        'cmp_w2_k': nrm(ks[15], (L, CMP_HIDDEN, dh), CMP_HIDDEN ** -0.5),
        'cmp_pe_v': nrm(ks[16], (L, CMP_LEN, dh), 0.1),
        'cmp_w1_v': nrm(ks[17], (L, CMP_LEN, dh, CMP_HIDDEN), (CMP_LEN * dh) ** -0.5),
        'cmp_w2_v': nrm(ks[18], (L, CMP_HIDDEN, dh), CMP_HIDDEN ** -0.5),
        'w_branch_a': nrm(ks[19], (L, DSA_HEADS * dh, D), (DSA_HEADS * dh) ** -0.5),
        'w_branch_b': nrm(ks[20], (L, NSA_HEADS * dh, D), (NSA_HEADS * dh) ** -0.5),
        'w_out': nrm(ks[21], (L, D, D), D ** -0.5),
        'g_ffn': 1.0 + nrm(ks[22], (L, D), 0.02),
        'w_peer_q': nrm(ks[23], (L, D, PEER_HEADS * PEER_QDIM), D ** -0.5),
        'peer_sub_keys': nrm(ks[24], (L, PEER_HEADS, 2, PEER_NKEYS, PEER_QDIM // 2), (PEER_QDIM // 2) ** -0.5),
        'peer_u': nrm(ks[25], (L, N_EXPERTS, D), D ** -0.5),
        'peer_v': nrm(ks[26], (L, N_EXPERTS, D), 0.2),
    }


def reference(x, c, rel_bias, w_ada, b_ada, g_mix, w_in, gq_a, gk_a, gq_b, gk_cmp, gk_slc, gk_win,
              cmp_pe_k, cmp_w1_k, cmp_w2_k, cmp_pe_v, cmp_w1_v, cmp_w2_v, w_branch_a, w_branch_b, w_out,
              g_ffn, w_peer_q, peer_sub_keys, peer_u, peer_v):
    for i in range(DEPTH):
        mod = (c @ w_ada[i] + b_ada[i])[:, None, :]
        sh1, sc1, gt1, sh2, sc2, gt2 = jnp.split(mod, 6, axis=-1)
        h = rms_norm(x, g_mix[i]) * (1 + sc1) + sh1
        x = x + gt1 * token_mixer(h, rel_bias, w_in[i], gq_a[i], gk_a[i], gq_b[i], gk_cmp[i], gk_slc[i], gk_win[i],
                                  cmp_pe_k[i], cmp_w1_k[i], cmp_w2_k[i], cmp_pe_v[i], cmp_w1_v[i], cmp_w2_v[i],
                                  w_branch_a[i], w_branch_b[i], w_out[i])
        h = rms_norm(x, g_ffn[i]) * (1 + sc2) + sh2
        x = x + gt2 * peer_ffn(h, w_peer_q[i], peer_sub_keys[i], peer_u[i], peer_v[i])
    return x
```

```python
import contextlib
import numpy as np
import ml_dtypes
import concourse.bass as bass
import concourse.mybir as mybir
from concourse.bass_utils import run_bass_kernel_spmd

F32 = mybir.dt.float32
BF16 = mybir.dt.bfloat16
AF = mybir.ActivationFunctionType
ALU = mybir.AluOpType
AX = mybir.AxisListType

NCORES = 8
T = 16384
D = 4096
NTB = T // 128
NOWN = 2048
NJ = 16
EPS = 1e-6
KC = 32


class Tok:
    __slots__ = ("lw", "rd", "dsem", "dcnt", "name")

    def __init__(self, name=""):
        self.lw = None
        self.rd = {}
        self.dsem = None
        self.dcnt = 0
        self.name = name


class Buf:
    def __init__(self, t, tok):
        self.t = t
        self.tok = tok

    def __getitem__(self, k):
        return self.t[k]


class Ring:
    def __init__(self, bufs):
        self.bufs = bufs
        self.i = 0

    def next(self):
        b = self.bufs[self.i % len(self.bufs)]
        self.i += 1
        return b


class Sched:
    def __init__(self, nc):
        self.nc = nc
        self.E = {"pe": nc.tensor, "act": nc.scalar, "dve": nc.vector, "pool": nc.gpsimd, "sp": nc.sync}
        self.sem = {k: nc.semaphore("se_" + k).__enter__() for k in self.E}
        self.cnt = {k: 0 for k in self.E}
        self.seen = {k: {} for k in self.E}
        self.dsems = []
        self.free_dsems = []
        self.dsem_cnt = {}
        self.scope_toks = [[]]
        self.ninst = 0
        self.stack = None

    def scope(self):
        return _Scope(self)

    def _enter(self, cm):
        return self.stack.enter_context(cm)

    def _nm(self, name):
        self.nuid = getattr(self, "nuid", 0) + 1
        return f"{name}_{self.nuid}"

    def sb(self, name, shape, dtype):
        return Buf(self._enter(self.nc.sbuf_tensor(self._nm(name), list(shape), dtype)), Tok(name))

    def ps(self, name, shape, dtype):
        return Buf(self._enter(self.nc.psum_tensor(self._nm(name), list(shape), dtype)), Tok(name))

    def dram(self, name, shape, dtype):
        return Buf(self.nc.dram_tensor(name, list(shape), dtype, kind="Internal"), Tok(name))

    def ring(self, name, shape, dtype, n):
        return Ring([self.sb(f"{name}{i}", shape, dtype) for i in range(n)])

    def psring(self, name, shape, dtype, n):
        return Ring([self.ps(f"{name}{i}", shape, dtype) for i in range(n)])

    @staticmethod
    def _toks(xs):
        return [x.tok if isinstance(x, Buf) else x for x in xs]

    @staticmethod
    def _deps(r, w):
        deps = []
        for t in r:
            if t.lw is not None:
                deps.append(t.lw)
        for t in w:
            if t.lw is not None:
                deps.append(t.lw)
            deps.extend(t.rd.values())
        return deps

    def _wait(self, eng, deps, skip_own=False):
        seen = self.seen[eng]
        own = self.sem[eng]
        for sem, val in deps:
            if skip_own and sem is own:
                continue
            k = id(sem)
            if seen.get(k, 0) < val:
                self.E[eng].wait_ge(sem, val)
                seen[k] = val

    @staticmethod
    def _commit(me, r, w):
        k = id(me[0])
        for t in r:
            t.rd[k] = me
        for t in w:
            t.lw = me
            t.rd = {}

    def op(self, eng, meth, *args, r=(), w=(), **kw):
        r = self._toks(r)
        w = self._toks(w)
        self._wait(eng, self._deps(r, w), skip_own=(eng == "pe"))
        inst = getattr(self.E[eng], meth)(*args, **kw)
        self.cnt[eng] += 1
        inst.then_inc(self.sem[eng], 1)
        self._commit((self.sem[eng], self.cnt[eng]), r, w)
        self.ninst += 1
        return inst

    def dma(self, q, out, in_, r=(), w=(), **kw):
        r = self._toks(r)
        w = self._toks(w)
        self._wait(q, self._deps(r, w))
        inst = self.E[q].dma_start(out=out, in_=in_, **kw)
        t = w[0]
        if t.dsem is None:
            if self.free_dsems:
                t.dsem, t.dcnt = self.free_dsems.pop()
            else:
                t.dsem = self.nc.semaphore(f"sd{len(self.dsems)}").__enter__()
                t.dcnt = 0
                self.dsems.append(t.dsem)
            self.scope_toks[-1].append(t)
        t.dcnt += 16
        inst.then_inc(t.dsem, 16)
        self.dsem_cnt[id(t.dsem)] = (t.dsem, t.dcnt)
        self._commit((t.dsem, t.dcnt), r, w)
        self.ninst += 1
        return inst

    def barrier(self):
        deps = [(self.sem[k], self.cnt[k]) for k in self.E if self.cnt[k] > 0]
        deps += list(self.dsem_cnt.values())
        for eng in self.E:
            self._wait(eng, deps)

    def release_scope_sems(self, toks):
        for t in toks:
            if t.dsem is not None:
                self.free_dsems.append((t.dsem, t.dcnt))
                t.dsem = None
                t.dcnt = 0


class _Scope:
    def __init__(self, S):
        self.S = S

    def __enter__(self):
        self.prev = self.S.stack
        self.es = contextlib.ExitStack()
        self.es.__enter__()
        self.S.stack = self.es
        self.S.scope_toks.append([])
        return self

    def __exit__(self, *a):
        self.S.barrier()
        self.S.release_scope_sems(self.S.scope_toks.pop())
        self.S.stack = self.prev
        return self.es.__exit__(*a)


KCH = (["n1"] * 4) + ["c"] + (["c"] * 8) + (["n4"] * 4) + (["n5"] * 4)
NKCH = len(KCH)
QCH = (["n0"] * 16) + (["c"] * 16) + (["n2"] * 16) + (["s"] * 64)
NQCH = len(QCH)


class Prog:
    def __init__(self, stop="all", dbg=()):
        self.stop = stop
        self.dbg = set(dbg)
        nc = bass.Bass("TRN2", target_bir_lowering=False)
        self.nc = nc
        self.S = Sched(nc)
        self.inp = {}
        self.outs = {}

    def din(self, name, shape, dtype=F32):
        t = self.nc.dram_tensor(name, list(shape), dtype, kind="ExternalInput")
        self.inp[name] = t
        return t

    INSHAPES = {
        "xfull": [T, D], "xown": [NOWN, D], "cT": [128, KC], "w_ada": [D, 6 * D], "b_adaT": [128, 192],
        "gnT": [128, 64], "gains": [128, 6], "w_kT": [D, 21 * 128], "w_v": [D, 1536],
        "w_qT": [D, 112 * 128], "w_qs": [D, 80],
    }

    def __getattr__(self, name):
        if name.startswith("i_") and name[2:] in self.INSHAPES:
            nm = name[2:]
            if nm not in self.inp:
                shp = list(self.INSHAPES[nm])
                if "small" in self.dbg and nm == "xfull":
                    shp[0] = 1024
                if "small" in self.dbg and nm == "w_qT":
                    shp[1] = 1024
                self.din(nm, shp)
            return self.inp[nm]
        raise AttributeError(name)

    def dout(self, name, shape, dtype=F32):
        t = self.nc.dram_tensor(name, list(shape), dtype, kind="ExternalOutput")
        self.outs[name] = (t, Tok(name))
        return t

    def build(self):
        nc, S = self.nc, self.S
        self.hT_all = S.dram("hT_all", [32, 128, KC, 512], BF16)
        self.hT_own = S.dram("hT_own", [4, 128, KC, 512], BF16)
        self.kT_all = S.dram("kT_all", [NKCH, 128, T], BF16)
        self.v_all = S.dram("v_all", [3, 4, 128, NTB, 128], BF16)
        self.qT_all = S.dram("qT_all", [NQCH, 128, NOWN], BF16)

        with S.scope():
            self.consts()
            with S.scope():
                if "nomod" in self.dbg:
                    self.din("modc_in", [128, 192])
                    S.dma("sp", self.modc[:], self.inp["modc_in"].ap()[:, :], w=[self.modc])
                    self.mod_to_AB()
                else:
                    self.phase0()
            if self.stop == "p0":
                return self.finish()
            if "inj" in self.dbg:
                self.inject()
                self.attention_and_rest()
                return self.finish()
            with S.scope():
                self.phase1(self.i_xfull, self.hT_all, NTB if "small" not in self.dbg else 8)
                self.phase1(self.i_xown, self.hT_own, NJ)
            if self.stop == "p1":
                return self.finish()
            with S.scope():
                self.proj_kv()
            with S.scope():
                self.proj_q()
            if self.stop == "p2":
                return self.finish()
            self.attention_and_rest()
            return self.finish()

    def inject(self):
        S = self.S
        self.kT_all = Buf(self.din("kT_in", [NKCH, 128, T], BF16), Tok("kT_in"))
        self.v_all = Buf(self.din("v_in", [3, 4, 128, NTB, 128], BF16), Tok("v_in"))
        self.qT_all = Buf(self.din("qT_in", [NQCH, 128, NOWN], BF16), Tok("qT_in"))
        S.dma("sp", self.wis[:], self.din("wis_in", [128, NJ, 32]).ap()[:, :, :], w=[self.wis])
        S.dma("sp", self.gbs[:], self.din("gbs_in", [128, NJ, 48]).ap()[:, :, :], w=[self.gbs])

    def attention_and_rest(self):
        S = self.S
        self.yT_all = S.dram("yT_all", [32, 128, NOWN], BF16)
        if "yinj" in self.dbg:
            self.yT_all = Buf(self.din("yT_in", [32, 128, NOWN], BF16), Tok("yT_in"))
        else:
            self.prep_tables()
        cum = "cum" in self.dbg
        lvl = ["dsa", "nsa", "merge", "all"].index(self.stop) if (cum and self.stop in ("dsa", "nsa", "merge", "all")) else -1
        if self.stop in ("dsa", "all") or lvl >= 0:
            self.dsa_index()
            self.dsa_attn()
        if self.stop in ("nsa", "all") or lvl >= 1:
            self.nsa_compress()
            self.nsa_attn()
        if self.stop in ("merge", "mp", "all") or lvl >= 2:
            self.merge()
        if self.stop == "peer" and "inj" in self.dbg:
            self.x1_d = Buf(self.din("x1_in", [NOWN, D]), Tok("x1_in"))
            self.hT2_own = S.dram("hT2_own", [4, 128, KC, 512], BF16)
            self.phase1(self.x1_d.t, self.hT2_own, NJ, AB_off=64, src_tok=self.x1_d)
        if self.stop in ("peer", "mp", "all"):
            self.dout("out", [NOWN, D])
            self.peer()
        if "y" in self.dbg:
            o = self.dout("d_yT", [32, 128, 256], BF16)
            S.dma("sp", o.ap()[:, :, :], self.yT_all.t.ap()[:, :, 0:256], r=[self.yT_all],
                  w=[self.outs["d_yT"][1]])

    def finish(self):
        S = self.S
        toks = [tok for (_, tok) in self.outs.values()]
        deps = [t.lw for t in toks if t.lw is not None]
        S._wait("sp", deps)
        S.barrier()
        return self.nc

    def consts(self):
        S = self.S
        self.identf = S.sb("identf", [128, 128], F32)
        self.ident = S.sb("ident", [128, 128], BF16)
        self.ones_bf = S.sb("ones_bf", [128, 128], BF16)
        self.modc = S.sb("modc", [128, 192], F32)
        self.AB = S.sb("AB", [128, 128], F32)
        self.gn = S.sb("gn", [128, 64], F32)
        self.gcol = S.sb("gcol", [128, 6], F32)
        S.op("pool", "memset", self.identf[:], 1.0, w=[self.identf])
        S.op("pool", "affine_select", self.identf[:], self.identf[:], pattern=[[-1, 128]],
             compare_op=ALU.is_equal, fill=0.0, base=0, channel_multiplier=1,
             r=[self.identf], w=[self.identf])
        S.op("dve", "tensor_copy", self.ident[:], self.identf[:], r=[self.identf], w=[self.ident])
        S.op("dve", "memset", self.ones_bf[:], 1.0, w=[self.ones_bf])
        self.wis = S.sb("wis", [128, NJ, 32], F32)
        self.gbs = S.sb("gbs", [128, NJ, 48], F32)
        self.eps128 = S.sb("eps128", [128, 1], F32)
        S.op("dve", "memset", self.eps128[:], 128.0 * EPS, w=[self.eps128])
        S.dma("sp", self.gn[:], self.i_gnT.ap()[:, :], w=[self.gn])
        S.dma("sp", self.gcol[:], self.i_gains.ap()[:, :], w=[self.gcol])
        S.op("dve", "tensor_scalar", self.gcol[:], self.gcol[:], float(np.sqrt(128.0)), None, ALU.mult,
             r=[self.gcol], w=[self.gcol])

    def phase0(self):
        S = self.S
        cT = S.sb("cTs", [128, KC], F32)
        bT = S.sb("bTs", [128, 192], F32)
        S.dma("sp", cT[:], self.i_cT.ap()[:, :], w=[cT])
        S.dma("sp", bT[:], self.i_b_adaT.ap()[:, :], w=[bT])
        ps = S.ps("ps_mod", [128, 192], F32)
        prow = S.psring("ps_row", [1, 512], F32, 2)
        rows = S.ring("modrow", [1, 512], F32, 2)
        one = S.sb("one11", [1, 1], F32)
        S.op("dve", "memset", one[:], 1.0, w=[one])
        wr = S.ring("wada", [128, KC, 512], F32, 2)
        wa = self.i_w_ada.ap().rearrange("(kc p) n -> p kc n", p=128)
        qs = ["sp", "pool"]
        for ct in range(48):
            wt = wr.next()
            for hh in range(2):
                S.dma(qs[hh], wt[:, hh * 16:(hh + 1) * 16, :], wa[:, hh * 16:(hh + 1) * 16, ct * 512:(ct + 1) * 512],
                      w=[wt])
            pr = prow.next()
            for kc in range(KC):
                S.op("pe", "matmul", pr[:], lhsT=cT[:, kc:kc + 1], rhs=wt[:, kc, :],
                     start=(kc == 0), stop=(kc == KC - 1), r=[wt, cT], w=[pr])
            row = rows.next()
            S.op("act", "activation", row[:], pr[:], AF.Identity, r=[pr], w=[row])
            for i in range(4):
                jc = ct * 4 + i
                S.op("pe", "matmul", ps[:, jc:jc + 1], lhsT=row[0:1, i * 128:(i + 1) * 128], rhs=one[0:1, 0:1],
                     start=True, stop=True, r=[row, one], w=[ps])
        S.op("dve", "tensor_tensor", self.modc[:], ps[:], bT[:], ALU.add, r=[ps, bT], w=[self.modc])
        self.mod_to_AB()
        if "modc" in self.dbg:
            o = self.dout("d_modc", [128, 192])
            S.dma("sp", o.ap()[:, :], self.modc[:], r=[self.modc], w=[self.outs["d_modc"][1]])

    def mod_to_AB(self):
        S = self.S
        m, AB, gn = self.modc, self.AB, self.gn
        S.op("dve", "scalar_tensor_tensor", AB[:, 0:32], m[:, 32:64], 1.0, gn[:, 0:32], ALU.add, ALU.mult,
             r=[m, gn], w=[AB])
        S.op("dve", "tensor_copy", AB[:, 32:64], m[:, 0:32], r=[m], w=[AB])
        S.op("dve", "scalar_tensor_tensor", AB[:, 64:96], m[:, 128:160], 1.0, gn[:, 32:64], ALU.add, ALU.mult,
             r=[m, gn], w=[AB])
        S.op("dve", "tensor_copy", AB[:, 96:128], m[:, 96:128], r=[m], w=[AB])

    def norm_hT(self, xt, AB_off, hT, col0, ps_ring, xn_ring, junk, small):
        S = self.S
        ss, rstd = small
        S.op("act", "activation", junk[:], xt[:], AF.Square, accum_out=ss[:], r=[xt], w=[junk, ss])
        S.op("dve", "tensor_scalar", rstd[:], ss[:], 1.0 / D, EPS, ALU.mult, ALU.add, r=[ss], w=[rstd])
        S.op("act", "activation", rstd[:], rstd[:], AF.Sqrt, r=[rstd], w=[rstd])
        S.op("dve", "reciprocal", rstd[:], rstd[:], r=[rstd], w=[rstd])
        xn = xn_ring.next()
        S.op("dve", "tensor_scalar", xn[:], xt[:], rstd[:, 0:1], None, ALU.mult, r=[xt, rstd], w=[xn])
        for k4 in range(KC // 8):
            pt = ps_ring.next()
            for i in range(8):
                kc = k4 * 8 + i
                S.op("pe", "transpose", pt[:, i * 128:(i + 1) * 128], xn[:, kc * 128:(kc + 1) * 128],
                     self.ident[:], r=[xn, self.ident], w=[pt])
            for i in range(8):
                kc = k4 * 8 + i
                A = self.AB[:, AB_off + kc:AB_off + kc + 1]
                B = self.AB[:, AB_off + 32 + kc:AB_off + 32 + kc + 1]
                if i % 2 == 0:
                    S.op("act", "activation", hT[:, kc, col0:col0 + 128], pt[:, i * 128:(i + 1) * 128],
                         AF.Identity, bias=B, scale=A, r=[pt, self.AB], w=[hT])
                else:
                    S.op("dve", "tensor_scalar", hT[:, kc, col0:col0 + 128], pt[:, i * 128:(i + 1) * 128],
                         A, B, ALU.mult, ALU.add, r=[pt, self.AB], w=[hT])

    def phase1(self, xsrc, hdst, nblk, AB_off=0, src_tok=None):
        S = self.S
        with S.scope():
            xr = S.ring("p1x", [128, D], F32, 2)
            xnr = S.ring("p1xn", [128, D], BF16, 2)
            junk = S.sb("p1junk", [128, D], BF16)
            hr = S.ring("p1h", [128, KC, 512], BF16, 2)
            psr = S.psring("p1ps", [128, 1024], BF16, 3)
            smalls = [(S.sb(f"p1ss{i}", [128, 1], F32), S.sb(f"p1rs{i}", [128, 1], F32)) for i in range(2)]
            xa = xsrc.ap()
            for tb in range(nblk):
                if tb % 4 == 0:
                    hT = hr.next()
                xt = xr.next()
                S.dma("sp", xt[:], xa[tb * 128:(tb + 1) * 128, :], r=([src_tok] if src_tok is not None else []),
                      w=[xt])
                self.norm_hT(xt, AB_off, hT, (tb % 4) * 128, psr, xnr, junk, smalls[tb % 2])
                if tb % 4 == 3:
                    S.dma("pool", hdst.t.ap()[tb // 4], hT[:], r=[hT], w=[hdst])

    def load_w(self, wsrc, col0, ncols, wt, q="pool"):
        S = self.S
        wa = wsrc.ap()
        for kc in range(KC):
            S.dma(q, wt[:, kc, 0:ncols], wa[kc * 128:(kc + 1) * 128, col0:col0 + ncols], w=[wt])

    def evac_T(self, kind, ps, ot, n, tmp):
        S = self.S
        if kind == "c":
            S.op("act", "activation", ot[:, 0:n], ps[:, 0:n], AF.Identity, r=[ps], w=[ot])
        elif kind == "s":
            S.op("act", "activation", ot[:, 0:n], ps[:, 0:n], AF.Sigmoid, r=[ps], w=[ot])
        else:
            gi = int(kind[1:])
            sq, ps2, rr = tmp
            S.op("act", "activation", sq[:, 0:n], ps[:, 0:n], AF.Square, r=[ps], w=[sq])
            S.op("pe", "matmul", ps2[:, 0:n], lhsT=self.ones_bf[:], rhs=sq[:, 0:n], start=True, stop=True,
                 r=[sq, self.ones_bf], w=[ps2])
            S.op("act", "activation", rr[:, 0:n], ps2[:, 0:n], AF.Sqrt, bias=self.eps128[:, 0:1],
                 r=[ps2, self.eps128], w=[rr])
            S.op("dve", "reciprocal", rr[:, 0:n], rr[:, 0:n], r=[rr], w=[rr])
            S.op("dve", "scalar_tensor_tensor", ot[:, 0:n], ps[:, 0:n], self.gcol[:, gi:gi + 1], rr[:, 0:n],
                 ALU.mult, ALU.mult, r=[ps, self.gcol, rr], w=[ot])

    def proj_kv(self):
        S = self.S
        ntile = 32 if "small" not in self.dbg else 2
        hr = S.ring("kvh", [128, KC, 512], BF16, 2)
        wk = S.sb("kvwk", [128, KC, 7 * 128], BF16)
        wv = S.sb("kvwv", [128, KC, 512], BF16)
        psr = S.psring("kvps", [128, 512], F32, 4)
        ps2r = S.psring("kvps2", [128, 512], F32, 2)
        otr = S.ring("kvot", [128, 512], BF16, 4)
        sqr = S.ring("kvsq", [128, 512], BF16, 2)
        rrr = S.ring("kvrr", [128, 512], F32, 2)
        kTa = self.kT_all.t.ap()
        va = self.v_all.t.ap()
        for p in range(3):
            self.load_w(self.i_w_kT, p * 7 * 128, 7 * 128, wk)
            self.load_w(self.i_w_v, p * 512, 512, wv)
            for tt in range(ntile):
                hT = hr.next()
                S.dma("sp", hT[:], self.hT_all.t.ap()[tt], r=[self.hT_all], w=[hT])
                for ci in range(7):
                    ch = p * 7 + ci
                    ps = psr.next()
                    for kc in range(KC):
                        S.op("pe", "matmul", ps[:], lhsT=wk[:, kc, ci * 128:(ci + 1) * 128], rhs=hT[:, kc, :],
                             start=(kc == 0), stop=(kc == KC - 1), r=[wk, hT], w=[ps])
                    ot = otr.next()
                    self.evac_T(KCH[ch], ps, ot, 512, (sqr.next(), ps2r.next(), rrr.next()))
                    S.dma("sp", kTa[ch, :, tt * 512:(tt + 1) * 512], ot[:], r=[ot], w=[self.kT_all])
                for tb in range(4):
                    ps = psr.next()
                    for kc in range(KC):
                        S.op("pe", "matmul", ps[:], lhsT=hT[:, kc, tb * 128:(tb + 1) * 128], rhs=wv[:, kc, :],
                             start=(kc == 0), stop=(kc == KC - 1), r=[wv, hT], w=[ps])
                    ot = otr.next()
                    S.op("dve", "tensor_copy", ot[:], ps[:], r=[ps], w=[ot])
                    blk = tt * 4 + tb
                    S.dma("sp", va[p, :, :, blk, :].rearrange("g p d -> p g d"),
                          ot[:].rearrange("p (g d) -> p g d", g=4), r=[ot], w=[self.v_all])
        if "kv" in self.dbg:
            o = self.dout("d_kT", [NKCH, 128, 1024], BF16)
            S.dma("sp", o.ap()[:, :, :], kTa[:, :, 0:1024], r=[self.kT_all], w=[self.outs["d_kT"][1]])
            o = self.dout("d_v", [3, 4, 128, 8, 128], BF16)
            for i3 in range(3):
                S.dma("sp", o.ap()[i3], va[i3, :, :, 0:8, :], r=[self.v_all], w=[self.outs["d_v"][1]])

    def proj_q(self):
        S = self.S
        hT4 = S.sb("qh", [128, 4, KC, 512], BF16) if False else None
        hts = [S.sb(f"qh{i}", [128, KC, 512], BF16) for i in range(2)]
        G = 4
        wr = S.ring("qw", [128, KC, G * 128], BF16, 2)
        psr = S.psring("qps", [128, 512], F32, 4)
        ps2r = S.psring("qps2", [128, 512], F32, 2)
        otr = S.ring("qot", [128, 512], BF16, 4)
        sqr = S.ring("qsq", [128, 512], BF16, 2)
        rrr = S.ring("qrr", [128, 512], F32, 2)
        qTa = self.qT_all.t.ap()
        ngrp = NQCH // G if "small" not in self.dbg else 2
        wqs = S.sb("qwqs", [128, KC, 80], BF16)
        self.load_w(self.i_w_qs, 0, 80, wqs)
        for half in range(2):
            for i in range(2):
                S.dma("sp", hts[i][:], self.hT_own.t.ap()[half * 2 + i], r=[self.hT_own], w=[hts[i]])
            for i in range(2):
                for tb in range(4):
                    if "noqs" in self.dbg:
                        continue
                    j = (half * 2 + i) * 4 + tb
                    ps = psr.next()
                    for kc in range(KC):
                        S.op("pe", "matmul", ps[:, 0:80], lhsT=hts[i][:, kc, tb * 128:(tb + 1) * 128],
                             rhs=wqs[:, kc, :], start=(kc == 0), stop=(kc == KC - 1), r=[wqs, hts[i]], w=[ps])
                    S.op("act", "activation", self.wis[:, j, :], ps[:, 0:32], AF.Identity,
                         scale=float(1.0 / (8.0 * np.sqrt(32.0))), r=[ps], w=[self.wis])
                    S.op("act", "activation", self.gbs[:, j, :], ps[:, 32:80], AF.Sigmoid, r=[ps], w=[self.gbs])
            for g in range(ngrp):
                wt = wr.next()
                self.load_w(self.i_w_qT, g * G * 128, G * 128, wt)
                for i in range(2):
                    tt = half * 2 + i
                    for ci in range(G):
                        ch = g * G + ci
                        ps = psr.next()
                        for kc in range(KC):
                            S.op("pe", "matmul", ps[:], lhsT=wt[:, kc, ci * 128:(ci + 1) * 128],
                                 rhs=hts[i][:, kc, :], start=(kc == 0), stop=(kc == KC - 1),
                                 r=[wt, hts[i]], w=[ps])
                        ot = otr.next()
                        self.evac_T(QCH[ch], ps, ot, 512, (sqr.next(), ps2r.next(), rrr.next()))
                        S.dma("sp", qTa[ch, :, tt * 512:(tt + 1) * 512], ot[:], r=[ot], w=[self.qT_all])
        if "q" in self.dbg:
            o = self.dout("d_qT", [NQCH, 128, 512], BF16)
            S.dma("sp", o.ap()[:, :, :], qTa[:, :, 0:512], r=[self.qT_all], w=[self.outs["d_qT"][1]])


    def prep_tables(self):
        S = self.S
        self.tabs = {}
        with S.scope():
            raw = S.ring("tbraw", [128, 4, 512], F32, 2)
            et = S.ring("tbe", [128, 4, 512], BF16, 2)
            for name, nt in (("dsa", 21), ("slc", 21), ("win", 12), ("cmp", 5)):
                src = self.din("tab_" + name, [nt, 4, 128, 512])
                dst = S.dram("tabe_" + name, [nt, 4, 128, 512], BF16)
                self.tabs[name] = dst
                for t in range(nt):
                    r = raw.next()
                    S.dma("sp", r[:], src.ap()[t].rearrange("g p n -> p g n"), w=[r])
                    e = et.next()
                    S.op("act", "activation", e[:], r[:], AF.Exp, r=[r], w=[e])
                    S.dma("pool", dst.t.ap()[t].rearrange("g p n -> p g n"), e[:], r=[e], w=[dst])

    def load_tab(self, name, g, tab, nt):
        S = self.S
        src = self.tabs[name]
        S.dma("pool", tab[:, 0:nt, :], src.t.ap()[:, g].rearrange("o p n -> p o n"), r=[src], w=[tab])

    def dsa_index(self):
        S = self.S
        nj = NJ if "small" not in self.dbg else 2
        self.maskT_d = S.dram("maskT_d", [NJ, 128, 128, 128], BF16)
        with S.scope():
            score = S.sb("ix_score", [128, T], F32)
            junk = S.sb("ix_junk", [128, 4096], BF16)
            mst = S.sb("ix_mst", [128, 128, 128], BF16)
            qi = S.ring("ix_qi", [128, 16, 128], BF16, 2)
            dh = S.ring("ix_dh", [128, 32, 128], BF16, 2)
            kir = S.ring("ix_ki", [128, 2048], BF16, 2)
            rl = S.ring("ix_rl", [128, 512], BF16, 4)
            cm = S.sb("ix_cm", [128, 1024], F32)
            jf = S.sb("ix_jf", [128, 1024], F32)
            half = S.sb("ix_half", [128, 1], F32)
            sm = {k: S.sb("ix_" + k, [128, 1], F32) for k in ("lo", "hi", "mid", "cnt", "pred", "d1", "d2", "t1", "t2")}
            ps_s = S.psring("ix_pss", [128, 512], F32, 3)
            ps_c = S.psring("ix_psc", [128, 512], F32, 2)
            ps_t = S.psring("ix_pst", [128, 1024], BF16, 2)
            S.dma("sp", cm[:], self.din("cmask", [128, 1024]).ap()[:, :], w=[cm])
            S.op("dve", "memset", half[:], 0.5, w=[half])
            qTa = self.qT_all.t.ap()
            kia = self.kT_all.t.ap()[4]
            for j in range(nj):
                nkb = 8 * j + 8
                Tk = nkb * 128
                q = qi.next()
                S.dma("sp", q[:], qTa[16:32, :, j * 128:(j + 1) * 128].rearrange("c p q -> p c q"),
                      r=[self.qT_all], w=[q])
                d = dh.next()
                for h in range(32):
                    S.op("dve", "tensor_scalar", d[:, h, :], self.ident[:],
                         self.wis[:, j, h:h + 1], None, ALU.mult, r=[self.ident, self.wis], w=[d])
                for kt in range(nkb // 4):
                    if kt % 4 == 0:
                        ki = kir.next()
                        n = min(2048, Tk - kt * 512)
                        S.dma("sp", ki[:, 0:n], kia[:, kt * 512:kt * 512 + n], r=[self.kT_all], w=[ki])
                    ko = (kt % 4) * 512
                    pc = ps_c.next()
                    for h in range(32):
                        pb = (h % 2) * 64
                        ps = ps_s.next()
                        S.op("pe", "matmul", ps[:], lhsT=q[pb:pb + 64, h // 2, :], rhs=ki[pb:pb + 64, ko:ko + 512],
                             start=True, stop=True, r=[q, ki], w=[ps])
                        r = rl.next()
                        if h % 2 == 0:
                            S.op("act", "activation", r[:], ps[:], AF.Relu, r=[ps], w=[r])
                        else:
                            S.op("dve", "tensor_scalar", r[:], ps[:], 0.0, None, ALU.max, r=[ps], w=[r])
                        S.op("pe", "matmul", pc[:], lhsT=d[:, h, :], rhs=r[:], start=(h == 0), stop=(h == 31),
                             r=[d, r], w=[pc])
                    if kt >= nkb // 4 - 2:
                        co = (kt - (nkb // 4 - 2)) * 512
                        S.op("dve", "tensor_tensor", score[:, kt * 512:(kt + 1) * 512], pc[:], cm[:, co:co + 512],
                             ALU.add, r=[pc, cm], w=[score])
                    else:
                        S.op("act", "activation", score[:, kt * 512:(kt + 1) * 512], pc[:], AF.Identity,
                             r=[pc], w=[score])
                lo, hi, mid, cnt, pred = sm["lo"], sm["hi"], sm["mid"], sm["cnt"], sm["pred"]
                d1, d2, t1, t2 = sm["d1"], sm["d2"], sm["t1"], sm["t2"]
                S.op("dve", "tensor_tensor", jf[:], score[:, Tk - 1024:Tk], cm[:], ALU.subtract,
                     r=[score, cm], w=[jf])
                S.op("dve", "tensor_reduce", t1[:], jf[:], AX.X, ALU.min, r=[jf], w=[t1])
                if Tk > 1024:
                    S.op("dve", "tensor_reduce", t2[:], score[:, 0:Tk - 1024], AX.X, ALU.min, r=[score], w=[t2])
                    S.op("dve", "tensor_tensor", lo[:], t1[:], t2[:], ALU.min, r=[t1, t2], w=[lo])
                else:
                    S.op("dve", "tensor_copy", lo[:], t1[:], r=[t1], w=[lo])
                S.op("dve", "tensor_reduce", hi[:], score[:, 0:Tk], AX.X, ALU.max, r=[score], w=[hi])
                S.op("dve", "tensor_scalar", hi[:], hi[:], 1.0, None, ALU.add, r=[hi], w=[hi])
                for it in range(30):
                    S.op("dve", "scalar_tensor_tensor", mid[:], lo[:], hi[:, 0:1], half[:], ALU.add, ALU.mult,
                         r=[lo, hi, half], w=[mid])
                    nch = (Tk + 4095) // 4096
                    for ci in range(nch):
                        c0 = ci * 4096
                        n = min(4096, Tk - c0)
                        init = 0.0 if ci == 0 else cnt[:, 0:1]
                        S.op("dve", "tensor_scalar", junk[:, 0:n], score[:, c0:c0 + n], mid[:, 0:1], init,
                             ALU.is_ge, ALU.add, accum_out=cnt[:], r=[score, mid, cnt], w=[junk, cnt])
                    S.op("dve", "tensor_scalar", pred[:], cnt[:], 255.5, None, ALU.is_ge, r=[cnt], w=[pred])
                    S.op("dve", "tensor_tensor", d1[:], mid[:], lo[:], ALU.subtract, r=[mid, lo], w=[d1])
                    S.op("dve", "tensor_tensor", d2[:], hi[:], mid[:], ALU.subtract, r=[mid, hi], w=[d2])
                    S.op("dve", "scalar_tensor_tensor", lo[:], d1[:], pred[:, 0:1], lo[:], ALU.mult, ALU.add,
                         r=[d1, pred, lo], w=[lo])
                    S.op("dve", "scalar_tensor_tensor", hi[:], d2[:], pred[:, 0:1], mid[:], ALU.mult, ALU.add,
                         r=[d2, pred, mid], w=[hi])
                for c8 in range(nkb // 8):
                    S.op("dve", "tensor_scalar", junk[:, 0:1024], score[:, c8 * 1024:(c8 + 1) * 1024], lo[:, 0:1], None,
                         ALU.is_ge, r=[score, lo], w=[junk])
                    pt = ps_t.next()
                    for i in range(8):
                        S.op("pe", "transpose", pt[:, i * 128:(i + 1) * 128], junk[:, i * 128:(i + 1) * 128],
                             self.ident[:], r=[junk, self.ident], w=[pt])
                    S.op("act", "activation", mst[:, c8 * 8:(c8 + 1) * 8, :].rearrange("p a b -> p (a b)"), pt[:],
                         AF.Identity, r=[pt], w=[mst])
                S.dma("sp", self.maskT_d.t.ap()[j, :, 0:nkb, :], mst[:, 0:nkb, :], r=[mst], w=[self.maskT_d])
            if "ix" in self.dbg:
                o = self.dout("d_score", [128, 2048])
                S.dma("sp", o.ap()[:, :], score[:, 0:2048], r=[score], w=[self.outs["d_score"][1]])
                o = self.dout("d_lo", [128, 1])
                S.dma("sp", o.ap()[:, :], sm["lo"][:], r=[sm["lo"]], w=[self.outs["d_lo"][1]])

    def attn(self, P, qT, qtok, kT_dram, ksrc, v_dram, vsrc, kb0, kb1, tab, tid_fn, mask_fn, keep=None):
        S = self.S
        oT = P["oT"].next()
        den = P["den"].next()
        scale = float(128.0 ** -0.5)
        first = True
        for c0 in range(kb0, kb1, 16):
            nb = min(16, kb1 - c0)
            kt = P["kt"].next()
            vt = P["vt"].next()
            S.dma("sp", kt[:, 0:nb * 128], kT_dram[:, c0 * 128:(c0 + nb) * 128], r=[ksrc], w=[kt])
            S.dma("sp", vt[:, 0:nb, :], v_dram[:, c0:c0 + nb, :], r=[vsrc], w=[vt])
            for i in range(nb):
                kb = c0 + i
                psl = P["psl"].next()
                S.op("pe", "matmul", psl[:], lhsT=kt[:, i * 128:(i + 1) * 128], rhs=qT, start=True, stop=True,
                     r=[kt, qtok], w=[psl])
                e = P["e"].next()
                S.op("act", "activation", e[:], psl[:], AF.Exp, scale=scale, r=[psl], w=[e])
                p = (keep if keep is not None else P["p"]).next()
                S.op("dve", "tensor_tensor", p[:], e[:], tab[:, tid_fn(kb), :], ALU.mult, r=[e, tab], w=[p])
                if mask_fn is not None:
                    map_, mtok = mask_fn(kb)
                    p2 = P["p"].next()
                    S.op("pool", "tensor_tensor", p2[:].rearrange("p (h q) -> p h q", h=4),
                         p[:].rearrange("p (h q) -> p h q", h=4),
                         map_.unsqueeze(1).broadcast_to([128, 4, 128]), ALU.mult, r=[p, mtok], w=[p2])
                    p = p2
                last = (kb == kb1 - 1)
                S.op("pe", "matmul", oT[:], lhsT=vt[:, i, :], rhs=p[:], start=first, stop=last, r=[vt, p], w=[oT])
                S.op("pe", "matmul", den[:], lhsT=self.ones_bf[:], rhs=p[:], start=first, stop=last,
                     r=[p, self.ones_bf], w=[den])
                first = False
        return oT, den

    def attn_rings(self, pre):
        S = self.S
        return {
            "kt": S.ring(pre + "kt", [128, 2048], BF16, 2),
            "vt": S.ring(pre + "vt", [128, 16, 128], BF16, 2),
            "psl": S.psring(pre + "psl", [128, 512], F32, 2),
            "e": S.ring(pre + "e", [128, 512], BF16, 3),
            "p": S.ring(pre + "p", [128, 512], BF16, 4),
            "oT": S.psring(pre + "oT", [128, 512], F32, 2),
            "den": S.psring(pre + "den", [128, 512], F32, 2),
        }

    def load_qT(self, qr, base, g, j):
        S = self.S
        qt = qr.next()
        S.dma("sp", qt[:], self.qT_all.t.ap()[base + 4 * g:base + 4 * g + 4, :, j * 128:(j + 1) * 128]
              .rearrange("h p q -> p h q"), r=[self.qT_all], w=[qt])
        return qt

    def store_y(self, y, hbase, g, j):
        S = self.S
        S.dma("sp", self.yT_all.t.ap()[hbase + 4 * g:hbase + 4 * g + 4, :, j * 128:(j + 1) * 128]
              .rearrange("h p q -> p h q"), y[:].rearrange("p (h q) -> p h q", h=4), r=[y], w=[self.yT_all])

    def dsa_attn(self):
        S = self.S
        nj = NJ if "small" not in self.dbg else 2
        with S.scope():
            P = self.attn_rings("da_")
            tab = S.sb("da_tab", [128, 21, 512], BF16)
            mT = S.ring("da_mT", [128, 128, 128], BF16, 1)
            qr = S.ring("da_q", [128, 4, 128], BF16, 2)
            rdr = S.ring("da_rd", [128, 512], F32, 2)
            yr = S.ring("da_y", [128, 512], BF16, 2)
            for g in range(4):
                self.load_tab("dsa", g, tab, 21)
                for j in range(nj):
                    nkb = 8 * j + 8
                    m = mT.next()
                    S.dma("pool", m[:, 0:nkb, :], self.maskT_d.t.ap()[j, :, 0:nkb, :], r=[self.maskT_d], w=[m])
                    qt = self.load_qT(qr, 0, g, j)
                    oT, den = self.attn(P, qt[:].rearrange("p h q -> p (h q)"), qt,
                                        self.kT_all.t.ap()[g], self.kT_all, self.v_all.t.ap()[0, g], self.v_all,
                                        0, nkb, tab, lambda kb, j=j: min(8 * j + 7 - kb, 20),
                                        lambda kb, m=m: (m[:, kb, :], m.tok))
                    rd = rdr.next()
                    S.op("dve", "tensor_scalar", rd[:], den[:], 1e-30, None, ALU.max, r=[den], w=[rd])
                    S.op("dve", "reciprocal", rd[:], rd[:], r=[rd], w=[rd])
                    y = yr.next()
                    S.op("dve", "tensor_tensor", y[:], oT[:], rd[:], ALU.mult, r=[oT, rd], w=[y])
                    self.store_y(y, 0, g, j)


    def gelu_tanh(self, out, x, n, tmp):
        S = self.S
        S.op("dve", "tensor_tensor", tmp[:, 0:n], x[:, 0:n], x[:, 0:n], ALU.mult, r=[x], w=[tmp])
        S.op("dve", "tensor_scalar", tmp[:, 0:n], tmp[:, 0:n], 0.044715, 1.0, ALU.mult, ALU.add, r=[tmp], w=[tmp])
        S.op("dve", "tensor_tensor", tmp[:, 0:n], tmp[:, 0:n], x[:, 0:n], ALU.mult, r=[tmp, x], w=[tmp])
        S.op("act", "activation", tmp[:, 0:n], tmp[:, 0:n], AF.Sigmoid, scale=1.5957691216057308, r=[tmp], w=[tmp])
        S.op("dve", "tensor_tensor", out[:, 0:n], tmp[:, 0:n], x[:, 0:n], ALU.mult, r=[tmp, x], w=[out])

    def nsa_compress(self):
        S = self.S
        self.kcT_d = S.dram("kcT_d", [4, 128, 1024], BF16)
        self.vc_d = S.dram("vc_d", [4, 128, 8, 128], BF16)
        with S.scope():
            w1 = S.sb("cp_w1", [128, 32, 256], BF16)
            w2 = S.sb("cp_w2", [128, 2, 128], BF16)
            peT = S.sb("cp_pe", [128, 32], BF16)
            pb = S.sb("cp_pb", [128, 2], F32)
            xr = S.ring("cp_x", [128, 8208], BF16, 2)
            hx = S.ring("cp_hx", [128, 512], F32, 2)
            tmpr = S.ring("cp_tmp", [128, 512], F32, 2)
            hid = [S.sb(f"cp_hid{i}", [128, 512], BF16) for i in range(2)]
            otr = S.ring("cp_ot", [128, 512], BF16, 2)
            sqr = S.ring("cp_sq", [128, 512], BF16, 1)
            rrr = S.ring("cp_rr", [128, 512], F32, 1)
            psr = S.psring("cp_ps", [128, 512], F32, 3)
            ps2r = S.psring("cp_ps2", [128, 512], F32, 1)
            psb = S.ps("cp_psb", [128, 2], F32)
            for kv in range(2):
                sfx = "k" if kv == 0 else "v"
                w1src = self.din("cmp_w1_" + sfx, [32, 128, 256])
                w2src = self.din("cmp_w2_" + sfx, [256, 128])
                pesrc = self.din("cmp_peT_" + sfx, [128, 32])
                S.dma("pool", w1[:], w1src.ap().rearrange("l d e -> d l e"), w=[w1])
                S.dma("pool", w2[:], w2src.ap().rearrange("(c e) d -> e c d", c=2), w=[w2])
                S.dma("pool", peT[:], pesrc.ap()[:, :], w=[peT])
                for ec in range(2):
                    for l in range(32):
                        S.op("pe", "matmul", psb[:, ec:ec + 1], lhsT=w1[:, l, ec * 128:(ec + 1) * 128],
                             rhs=peT[:, l:l + 1], start=(l == 0), stop=(l == 31), r=[w1, peT], w=[psb])
                S.op("dve", "tensor_copy", pb[:], psb[:], r=[psb], w=[pb])
                for g in range(4):
                    ch = (5 if kv == 0 else 9) + g
                    for nt in range(2):
                        n0 = nt * 512
                        nn = 512 if nt == 0 else 511
                        xt = xr.next()
                        S.dma("sp", xt[:, 0:16 * nn + 16], self.kT_all.t.ap()[ch, :, 16 * n0:16 * n0 + 16 * nn + 16],
                              r=[self.kT_all], w=[xt])
                        xv = xt[:, 0:8208].rearrange("p (n s) -> p n s", s=16)
                        for ec in range(2):
                            ps = psr.next()
                            for l in range(32):
                                S.op("pe", "matmul", ps[:, 0:nn], lhsT=w1[:, l, ec * 128:(ec + 1) * 128],
                                     rhs=xv[:, l // 16:l // 16 + nn, l % 16], start=(l == 0), stop=(l == 31),
                                     r=[w1, xt], w=[ps])
                            x32 = hx.next()
                            S.op("act", "activation", x32[:, 0:nn], ps[:, 0:nn], AF.Identity, bias=pb[:, ec:ec + 1],
                                 r=[ps, pb], w=[x32])
                            self.gelu_tanh(hid[ec], x32, nn, tmpr.next())
                        if kv == 0:
                            ps = psr.next()
                            for ec in range(2):
                                S.op("pe", "matmul", ps[:, 0:nn], lhsT=w2[:, ec, :], rhs=hid[ec][:, 0:nn],
                                     start=(ec == 0), stop=(ec == 1), r=[w2, hid[ec]], w=[ps])
                            ot = otr.next()
                            S.op("pool", "memset", ot[:], 0.0, w=[ot])
                            self.evac_T("n3", ps, ot, nn, (sqr.next(), ps2r.next(), rrr.next()))
                            S.dma("sp", self.kcT_d.t.ap()[g, :, n0:n0 + 512], ot[:], r=[ot], w=[self.kcT_d])
                        else:
                            ot = otr.next()
                            S.op("pool", "memset", ot[:], 0.0, w=[ot])
                            ps = psr.next()
                            for nb in range(4):
                                m = min(128, nn - nb * 128)
                                for ec in range(2):
                                    S.op("pe", "matmul", ps[0:m, nb * 128:(nb + 1) * 128],
                                         lhsT=hid[ec][:, nb * 128:nb * 128 + m], rhs=w2[:, ec, :],
                                         start=(ec == 0), stop=(ec == 1), r=[w2, hid[ec]], w=[ps])
                            for nb in range(4):
                                m = min(128, nn - nb * 128)
                                S.op("act", "activation", ot[0:m, nb * 128:(nb + 1) * 128],
                                     ps[0:m, nb * 128:(nb + 1) * 128], AF.Identity, r=[ps], w=[ot])
                            S.dma("sp", self.vc_d.t.ap()[g, :, nt * 4:(nt + 1) * 4, :],
                                  ot[:].rearrange("p (b d) -> p b d", b=4), r=[ot], w=[self.vc_d])
            if "cmp" in self.dbg:
                o = self.dout("d_kcT", [4, 128, 1024], BF16)
                S.dma("sp", o.ap()[:, :, :], self.kcT_d.t.ap()[:, :, :], r=[self.kcT_d], w=[self.outs["d_kcT"][1]])
                o = self.dout("d_vc", [4, 128, 8, 128], BF16)
                S.dma("sp", o.ap()[:, :, :, :], self.vc_d.t.ap()[:, :, :, :], r=[self.vc_d], w=[self.outs["d_vc"][1]])

    def nsa_attn(self):
        S = self.S
        nj = NJ if "small" not in self.dbg else 2
        with S.scope():
            P = self.attn_rings("na_")
            keep = S.ring("na_keep", [128, 512], BF16, 9)
            tab_s = S.sb("na_tabs", [128, 21, 512], BF16)
            tab_w = S.sb("na_tabw", [128, 12, 512], BF16)
            tab_c = S.sb("na_tabc", [128, 5, 512], BF16)
            qr = S.ring("na_q", [128, 4, 128], BF16, 2)
            ftab = S.sb("na_ftab", [128, NJ, 256], F32)
            ov = S.sb("na_ov", [128, 8, 256], BF16)
            yexp = S.sb("na_yexp", [128, 8192], BF16)
            S.dma("sp", ftab[:], self.din("ftab", [NJ, 128, 256]).ap().rearrange("j p m -> p j m"), w=[ftab])
            S.dma("pool", ov[:], self.din("ov", [1024, 256]).ap().rearrange("(c p) m -> p c m", p=128), w=[ov])
            S.dma("pool", yexp[:], self.din("yexp", [128, 8192]).ap()[:, :], w=[yexp])
            rdr = S.ring("na_rd", [128, 512], F32, 2)
            Rr = S.ring("na_R", [128, 512], F32, 2)
            yacc = S.sb("na_yacc", [128, 512], F32)
            ytmp = S.sb("na_ytmp", [128, 512], F32)
            yr = S.ring("na_y", [128, 512], BF16, 2)
            pnr = S.ring("na_pn", [128, 512], BF16, 2)
            rg = S.ring("na_rg", [128, 512], BF16, 2)
            imp = S.sb("na_imp", [128, 256], F32)
            imp3 = S.sb("na_imp3", [128, 256], F32)
            m8 = S.sb("na_m8", [128, 16], F32)
            mb = S.sb("na_mb", [128, 256], BF16)
            mbT = S.sb("na_mbT", [128, 2, 128], BF16)
            mkr = S.ring("na_mk", [128, 128], BF16, 3)
            psm = S.psring("na_psm", [128, 512], F32, 1)
            pst = S.ps("na_pst", [128, 1024], BF16)

            def finish_branch(br, oT, den, g, j):
                r = rg.next()
                for h in range(4):
                    col = (4 * g + h) * 3 + br
                    S.op("dve", "tensor_scalar", r[:, h * 128:(h + 1) * 128], self.ident[:],
                         self.gbs[:, j, col:col + 1], None, ALU.mult, r=[self.ident, self.gbs], w=[r])
                pg = psm.next()
                S.op("pe", "matmul", pg[:], lhsT=self.ones_bf[:], rhs=r[:], start=True, stop=True,
                     r=[r, self.ones_bf], w=[pg])
                rd = rdr.next()
                S.op("dve", "tensor_scalar", rd[:], den[:], 1e-30, None, ALU.max, r=[den], w=[rd])
                S.op("dve", "reciprocal", rd[:], rd[:], r=[rd], w=[rd])
                R = Rr.next()
                S.op("dve", "tensor_tensor", R[:], rd[:], pg[:], ALU.mult, r=[rd, pg], w=[R])
                if br == 0:
                    S.op("dve", "tensor_tensor", yacc[:], oT[:], R[:], ALU.mult, r=[oT, R], w=[yacc])
                else:
                    S.op("dve", "tensor_tensor", ytmp[:], oT[:], R[:], ALU.mult, r=[oT, R], w=[ytmp])
                    S.op("dve", "tensor_tensor", yacc[:], yacc[:], ytmp[:], ALU.add, r=[yacc, ytmp], w=[yacc])
                return rd

            for g in range(4):
                self.load_tab("slc", g, tab_s, 21)
                self.load_tab("win", g, tab_w, 12)
                self.load_tab("cmp", g, tab_c, 5)
                for j in range(nj):
                    qt = self.load_qT(qr, 32, g, j)
                    qv = qt[:].rearrange("p h q -> p (h q)")
                    ncc = j // 2 + 1
                    keep.i = 0
                    oT, den = self.attn(P, qv, qt, self.kcT_d.t.ap()[g], self.kcT_d, self.vc_d.t.ap()[g], self.vc_d,
                                        0, ncc, tab_c, lambda kb, j=j: min(j - 2 * kb, 4), None, keep=keep)
                    rd = finish_branch(0, oT, den, g, j)
                    pi = psm.next()
                    for cc in range(ncc):
                        pn = pnr.next()
                        S.op("dve", "tensor_tensor", pn[:], keep.bufs[cc][:], rd[:], ALU.mult,
                             r=[keep.bufs[cc], rd], w=[pn])
                        for h in range(4):
                            S.op("pe", "matmul", pi[:, 0:256], lhsT=pn[:, h * 128:(h + 1) * 128], rhs=ov[:, cc, :],
                                 start=(cc == 0 and h == 0), stop=(cc == ncc - 1 and h == 3), r=[pn, ov], w=[pi])
                    S.op("dve", "tensor_tensor", imp[:], pi[:, 0:256], ftab[:, j, :], ALU.add, r=[pi, ftab], w=[imp])
                    S.op("dve", "max", m8[:, 0:8], imp[:], r=[imp], w=[m8])
                    S.op("dve", "match_replace", imp3[:], m8[:, 0:8], imp[:], -3.0e38, r=[m8, imp], w=[imp3])
                    S.op("dve", "max", m8[:, 8:16], imp3[:], r=[imp3], w=[m8])
                    S.op("dve", "tensor_scalar", mb[:], imp[:], m8[:, 15:16], None, ALU.is_ge, r=[imp, m8], w=[mb])
                    for hh in range(2):
                        S.op("pe", "transpose", pst[:, hh * 128:(hh + 1) * 128], mb[:, hh * 128:(hh + 1) * 128],
                             self.ident[:], r=[mb, self.ident], w=[pst])
                    S.op("act", "activation", mbT[:].rearrange("p a b -> p (a b)"), pst[:, 0:256], AF.Identity,
                         r=[pst], w=[mbT])

                    def slc_mask(kb):
                        pm = psm.next()
                        S.op("pe", "matmul", pm[:, 0:128], lhsT=yexp[:, (kb % 64) * 128:(kb % 64 + 1) * 128],
                             rhs=mbT[:, kb // 64, :], start=True, stop=True, r=[yexp, mbT], w=[pm])
                        mk = mkr.next()
                        S.op("act", "activation", mk[:], pm[:, 0:128], AF.Identity, r=[pm], w=[mk])
                        return mk[:], mk.tok

                    nkb = 8 * j + 8
                    oT, den = self.attn(P, qv, qt, self.kT_all.t.ap()[13 + g], self.kT_all,
                                        self.v_all.t.ap()[1, g], self.v_all, 0, nkb, tab_s,
                                        lambda kb, j=j: min(8 * j + 7 - kb, 20), slc_mask)
                    finish_branch(1, oT, den, g, j)
                    kb0 = max(0, 8 * j - 4)
                    oT, den = self.attn(P, qv, qt, self.kT_all.t.ap()[17 + g], self.kT_all,
                                        self.v_all.t.ap()[2, g], self.v_all, kb0, nkb, tab_w,
                                        lambda kb, j=j: kb - (8 * j - 4), None)
                    finish_branch(2, oT, den, g, j)
                    y = yr.next()
                    S.op("act", "activation", y[:], yacc[:], AF.Identity, r=[yacc], w=[y])
                    self.store_y(y, 16, g, j)


    def row_bcast(self, dst, col0):
        S = self.S
        with S.scope():
            tl = S.ring("rb_l", [128, 128], F32, 2)
            ps = S.psring("rb_ps", [128, 512], F32, 2)
            onesf = S.sb("rb_ones", [128, 128], F32)
            S.op("dve", "memset", onesf[:], 1.0, w=[onesf])
            for k4 in range(8):
                p = ps.next()
                for i in range(4):
                    kc = k4 * 4 + i
                    t = tl.next()
                    S.op("dve", "tensor_scalar", t[:], onesf[:], self.modc[:, col0 + kc:col0 + kc + 1], None, ALU.mult,
                         r=[onesf, self.modc], w=[t])
                    S.op("pe", "matmul", p[:, i * 128:(i + 1) * 128], lhsT=t[:], rhs=self.identf[:], start=True,
                         stop=True, r=[t, self.identf], w=[p])
                S.op("act", "activation", dst[:, k4 * 512:(k4 + 1) * 512], p[:], AF.Identity, r=[p], w=[dst])

    def merge(self):
        S = self.S
        self.x1_d = S.dram("x1_d", [NOWN, D], F32)
        self.mT_d = S.dram("mT_d", [32, 128, NOWN], BF16)
        self.hT2_own = S.dram("hT2_own", [4, 128, KC, 512], BF16)
        wa_src = self.din("w_branch_a", [2048, D]).ap().rearrange("(h p) n -> p h n", p=128)
        wb_src = self.din("w_branch_b", [2048, D]).ap().rearrange("(h p) n -> p h n", p=128)
        ya = self.yT_all.t.ap()
        qTa = self.qT_all.t.ap()
        with S.scope():
            wa = S.sb("mg_wa", [128, 16, 1024], BF16)
            wb = S.sb("mg_wb", [128, 16, 1024], BF16)
            yar = S.ring("mg_ya", [128, 16, 512], BF16, 2)
            ybr = S.ring("mg_yb", [128, 16, 512], BF16, 2)
            gr = S.ring("mg_g", [128, 2, 512], BF16, 3)
            t1r = S.ring("mg_t1", [128, 512], F32, 2)
            t2r = S.ring("mg_t2", [128, 512], F32, 2)
            mr = S.ring("mg_m", [128, 512], BF16, 3)
            psr = S.psring("mg_ps", [128, 512], F32, 4)
            for ccg in range(4):
                for h in range(16):
                    S.dma("pool", wa[:, h, :], wa_src[:, h, ccg * 1024:(ccg + 1) * 1024], w=[wa])
                    S.dma("pool", wb[:, h, :], wb_src[:, h, ccg * 1024:(ccg + 1) * 1024], w=[wb])
                for tt in range(4):
                    yat = yar.next()
                    ybt = ybr.next()
                    S.dma("sp", yat[:], ya[0:16, :, tt * 512:(tt + 1) * 512].rearrange("h p q -> p h q"),
                          r=[self.yT_all], w=[yat])
                    S.dma("sp", ybt[:], ya[16:32, :, tt * 512:(tt + 1) * 512].rearrange("h p q -> p h q"),
                          r=[self.yT_all], w=[ybt])
                    for ci in range(8):
                        cc = ccg * 8 + ci
                        gt = gr.next()
                        S.dma("sp", gt[:, 0, :], qTa[48 + cc, :, tt * 512:(tt + 1) * 512], r=[self.qT_all], w=[gt])
                        S.dma("sp", gt[:, 1, :], qTa[80 + cc, :, tt * 512:(tt + 1) * 512], r=[self.qT_all], w=[gt])
                        pA = psr.next()
                        for h in range(16):
                            S.op("pe", "matmul", pA[:], lhsT=wa[:, h, ci * 128:(ci + 1) * 128], rhs=yat[:, h, :],
                                 start=(h == 0), stop=(h == 15), r=[wa, yat], w=[pA])
                        pB = psr.next()
                        for h in range(16):
                            S.op("pe", "matmul", pB[:], lhsT=wb[:, h, ci * 128:(ci + 1) * 128], rhs=ybt[:, h, :],
                                 start=(h == 0), stop=(h == 15), r=[wb, ybt], w=[pB])
                        t1 = t1r.next()
                        t2 = t2r.next()
                        S.op("dve", "tensor_tensor", t1[:], pA[:], gt[:, 0, :], ALU.mult, r=[pA, gt], w=[t1])
                        S.op("dve", "tensor_tensor", t2[:], pB[:], gt[:, 1, :], ALU.mult, r=[pB, gt], w=[t2])
                        m = mr.next()
                        S.op("pool", "tensor_tensor", m[:], t1[:], t2[:], ALU.add, r=[t1, t2], w=[m])
                        S.dma("sp", self.mT_d.t.ap()[cc, :, tt * 512:(tt + 1) * 512], m[:], r=[m], w=[self.mT_d])
        with S.scope():
            gtB = S.sb("mo_gtB", [128, D], F32)
            self.row_bcast(gtB, 64)
            wo_src = self.din("w_out", [D, D]).ap().rearrange("(kc p) n -> p kc n", p=128)
            mt = S.sb("mo_mt", [128, KC, 512], BF16)
            wo = S.sb("mo_wo", [128, KC, 512], BF16)
            xts = [S.sb(f"mo_x{i}", [128, D], F32) for i in range(4)]
            tmpr = S.ring("mo_tmp", [128, 512], F32, 2)
            psr = S.psring("mo_ps", [128, 512], F32, 3)
            with S.scope():
                pass
            for tg in range(4):
                S.dma("sp", mt[:], self.mT_d.t.ap()[:, :, tg * 512:(tg + 1) * 512].rearrange("c p q -> p c q"),
                      r=[self.mT_d], w=[mt])
                for tb in range(4):
                    r0 = (tg * 4 + tb) * 128
                    S.dma("sp", xts[tb][:], self.i_xown.ap()[r0:r0 + 128, :], w=[xts[tb]])
                for ct in range(8):
                    for kc in range(KC):
                        S.dma("pool", wo[:, kc, :], wo_src[:, kc, ct * 512:(ct + 1) * 512], w=[wo])
                    for tb in range(4):
                        ps = psr.next()
                        for kc in range(KC):
                            S.op("pe", "matmul", ps[:], lhsT=mt[:, kc, tb * 128:(tb + 1) * 128], rhs=wo[:, kc, :],
                                 start=(kc == 0), stop=(kc == KC - 1), r=[mt, wo], w=[ps])
                        tmp = tmpr.next()
                        sl = slice(ct * 512, (ct + 1) * 512)
                        S.op("dve", "tensor_tensor", tmp[:], ps[:], gtB[:, sl], ALU.mult, r=[ps, gtB], w=[tmp])
                        S.op("pool", "tensor_tensor", xts[tb][:, sl], xts[tb][:, sl], tmp[:], ALU.add,
                             r=[xts[tb], tmp], w=[xts[tb]])
                for tb in range(4):
                    r0 = (tg * 4 + tb) * 128
                    S.dma("sp", self.x1_d.t.ap()[r0:r0 + 128, :], xts[tb][:], r=[xts[tb]], w=[self.x1_d])
        self.phase1(self.x1_d.t, self.hT2_own, NJ, AB_off=64, src_tok=self.x1_d)


    def peer(self):
        S = self.S
        NE = 16384
        self.uT_d = S.dram("uT_d", [128, 128, KC, 128], BF16)
        self.qpT_d = S.dram("qpT_d", [16, 128, NOWN], BF16)
        u_src = self.din("peer_u", [NE, D]).ap()
        v_src = self.din("peer_v", [NE, D]).ap()
        with S.scope():
            ur = S.ring("pu_u", [128, D], BF16, 2)
            utr = S.ring("pu_ut", [128, KC, 128], BF16, 2)
            psr = S.psring("pu_ps", [128, 1024], BF16, 3)
            nec = 128
            for ec in range(nec):
                ut = ur.next()
                S.dma("pool", ut[:], u_src[ec * 128:(ec + 1) * 128, :], w=[ut])
                utt = utr.next()
                for k4 in range(4):
                    pt = psr.next()
                    for i in range(8):
                        kc = k4 * 8 + i
                        S.op("pe", "transpose", pt[:, i * 128:(i + 1) * 128], ut[:, kc * 128:(kc + 1) * 128],
                             self.ident[:], r=[ut, self.ident], w=[pt])
                    dst = utt[:, k4 * 8:(k4 + 1) * 8, :].rearrange("p a b -> p (a b)")
                    if k4 % 2 == 0:
                        S.op("act", "activation", dst, pt[:], AF.Identity, r=[pt], w=[utt])
                    else:
                        S.op("dve", "tensor_copy", dst, pt[:], r=[pt], w=[utt])
                S.dma("sp", self.uT_d.t.ap()[ec], utt[:], r=[utt], w=[self.uT_d])
        with S.scope():
            hts = [S.sb(f"pq_h{i}", [128, KC, 512], BF16) for i in range(2)]
            wr = S.ring("pq_w", [128, KC, 512], BF16, 2)
            psr = S.psring("pq_ps", [128, 512], F32, 3)
            otr = S.ring("pq_ot", [128, 512], BF16, 3)
            for half in range(2):
                for i in range(2):
                    S.dma("sp", hts[i][:], self.hT2_own.t.ap()[half * 2 + i], r=[self.hT2_own], w=[hts[i]])
                for gq in range(4):
                    wt = wr.next()
                    self.load_w(self.din("w_peer_q", [D, 2048]) if "w_peer_q" not in self.inp else self.inp["w_peer_q"],
                                gq * 512, 512, wt)
                    for i in range(2):
                        tt = half * 2 + i
                        for ci in range(4):
                            ch = gq * 4 + ci
                            ps = psr.next()
                            for kc in range(KC):
                                S.op("pe", "matmul", ps[:], lhsT=wt[:, kc, ci * 128:(ci + 1) * 128],
                                     rhs=hts[i][:, kc, :], start=(kc == 0), stop=(kc == KC - 1),
                                     r=[wt, hts[i]], w=[ps])
                            ot = otr.next()
                            S.op("act", "activation", ot[:], ps[:], AF.Identity, r=[ps], w=[ot])
                            S.dma("sp", self.qpT_d.t.ap()[ch, :, tt * 512:(tt + 1) * 512], ot[:], r=[ot],
                                  w=[self.qpT_d])
        with S.scope():
            keysT = S.sb("pe_keys", [128, 16, 128], BF16)
            S.dma("pool", keysT[:], self.din("peer_keysT", [16, 128, 128]).ap().rearrange("c d n -> d c n"),
                  w=[keysT])
            ngrp = 8 if "small" not in self.dbg else 1
            nec = 128
            for tg in range(ngrp):
                with S.scope():
                    actT = S.sb("pe_actT", [128, 128, 256], BF16)
                    with S.scope():
                        W = [S.sb(f"pe_W{i}", [128, NE], BF16) for i in range(2)]
                        with S.scope():
                            qp = S.sb("pe_qp", [128, 16, 256], BF16)
                            S.dma("sp", qp[:], self.qpT_d.t.ap()[:, :, tg * 256:(tg + 1) * 256]
                                  .rearrange("c p q -> p c q"), r=[self.qpT_d], w=[qp])
                            ssb = S.sb("pe_s", [128, 16, 128], F32)
                            pss = S.psring("pe_pss", [128, 512], F32, 2)
                            S4 = S.ring("pe_S4", [128, 1024], F32, 2)
                            E4 = S.ring("pe_E4", [128, 1024], BF16, 3)
                            sm = {k: S.sb("pe_" + k, [128, n], F32) for k, n in
                                  (("t1", 16), ("t2", 16), ("sr", 128), ("cand", 256), ("cr", 256), ("tc", 16),
                                   ("e16", 16), ("den", 1), ("nthr", 1), ("bias", 1), ("thr", 1))}
                            for tb in range(2):
                                for c4 in range(4):
                                    ps = pss.next()
                                    for i in range(4):
                                        ch = c4 * 4 + i
                                        S.op("pe", "matmul", ps[:, i * 128:(i + 1) * 128],
                                             lhsT=qp[:, ch, tb * 128:(tb + 1) * 128], rhs=keysT[:, ch, :],
                                             start=True, stop=True, r=[qp, keysT], w=[ps])
                                    S.op("act", "activation", ssb[:, c4 * 4:(c4 + 1) * 4, :].rearrange("p a b -> p (a b)"),
                                         ps[:], AF.Identity, r=[ps], w=[ssb])
                                for h in range(8):
                                    s1 = ssb[:, 2 * h, :]
                                    s2 = ssb[:, 2 * h + 1, :]
                                    for (sx, tx) in ((s1, sm["t1"]), (s2, sm["t2"])):
                                        S.op("dve", "max", tx[:, 0:8], sx, r=[ssb], w=[tx])
                                        S.op("dve", "match_replace", sm["sr"][:], tx[:, 0:8], sx, -3.0e38,
                                             r=[ssb, tx], w=[sm["sr"]])
                                        S.op("dve", "max", tx[:, 8:16], sm["sr"][:], r=[sm["sr"]], w=[tx])
                                    cand = sm["cand"]
                                    S.op("dve", "tensor_tensor", cand[:].rearrange("p (a b) -> p a b", a=16),
                                         sm["t1"][:].unsqueeze(2).broadcast_to([128, 16, 16]),
                                         sm["t2"][:].unsqueeze(1).broadcast_to([128, 16, 16]), ALU.add,
                                         r=[sm["t1"], sm["t2"]], w=[cand])
                                    tc_ = sm["tc"]
                                    S.op("dve", "max", tc_[:, 0:8], cand[:], r=[cand], w=[tc_])
                                    S.op("dve", "match_replace", sm["cr"][:], tc_[:, 0:8], cand[:], -3.0e38,
                                         r=[cand, tc_], w=[sm["cr"]])
                                    S.op("dve", "max", tc_[:, 8:16], sm["cr"][:], r=[sm["cr"]], w=[tc_])
                                    thr, nthr, den, bias = sm["thr"], sm["nthr"], sm["den"], sm["bias"]
                                    S.op("dve", "tensor_copy", thr[:], tc_[:, 15:16], r=[tc_], w=[thr])
                                    S.op("dve", "tensor_scalar", nthr[:], tc_[:, 15:16], -1.0, None, ALU.mult,
                                         r=[tc_], w=[nthr])
                                    S.op("act", "activation", sm["e16"][:], tc_[:], AF.Exp, bias=nthr[:, 0:1],
                                         accum_out=den[:], r=[tc_, nthr], w=[sm["e16"], den])
                                    S.op("act", "activation", den[:], den[:], AF.Ln, r=[den], w=[den])
                                    S.op("dve", "tensor_tensor", bias[:], nthr[:], den[:], ALU.subtract,
                                         r=[nthr, den], w=[bias])
                                    for q8 in range(16):
                                        i0 = q8 * 8
                                        s4 = S4.next()
                                        S.op("pool", "tensor_tensor", s4[:].rearrange("p (a b) -> p a b", a=8),
                                             s1[:, i0:i0 + 8].unsqueeze(2).broadcast_to([128, 8, 128]),
                                             s2.unsqueeze(1).broadcast_to([128, 8, 128]), ALU.add,
                                             r=[ssb], w=[s4])
                                        e4 = E4.next()
                                        S.op("act", "activation", e4[:], s4[:], AF.Exp, bias=bias[:, 0:1],
                                             r=[s4, bias], w=[e4])
                                        wsl = W[tb][:, q8 * 1024:(q8 + 1) * 1024]
                                        if h == 0:
                                            S.op("dve", "scalar_tensor_tensor", wsl, s4[:], thr[:, 0:1], e4[:],
                                                 ALU.is_ge, ALU.mult, r=[s4, thr, e4], w=[W[tb]])
                                        else:
                                            S.op("dve", "scalar_tensor_tensor", e4[:], s4[:], thr[:, 0:1], e4[:],
                                                 ALU.is_ge, ALU.mult, r=[s4, thr, e4], w=[e4])
                                            S.op("pool", "tensor_tensor", wsl, wsl, e4[:], ALU.add,
                                                 r=[W[tb], e4], w=[W[tb]])
                        with S.scope():
                            h2 = S.sb("pe_h2", [128, KC, 256], BF16)
                            S.dma("sp", h2[:], self.hT2_own.t.ap()[tg // 2, :, :, (tg % 2) * 256:(tg % 2 + 1) * 256],
                                  r=[self.hT2_own], w=[h2])
                            uTr = S.ring("pe_uT", [128, KC, 128], BF16, 2)
                            psa = S.psring("pe_psa", [128, 256], F32, 2)
                            pst = S.psring("pe_pst", [128, 256], BF16, 2)
                            xg = S.ring("pe_xg", [128, 256], F32, 2)
                            tg_ = S.ring("pe_tg", [128, 256], F32, 2)
                            gl = S.ring("pe_gl", [128, 256], F32, 2)
                            for ec in range(nec):
                                uT = uTr.next()
                                S.dma("sp" if ec % 2 == 0 else "pool", uT[:], self.uT_d.t.ap()[ec],
                                      r=[self.uT_d], w=[uT])
                                pa = psa.next()
                                for kc in range(KC):
                                    S.op("pe", "matmul", pa[:], lhsT=uT[:, kc, :], rhs=h2[:, kc, :],
                                         start=(kc == 0), stop=(kc == KC - 1), r=[uT, h2], w=[pa])
                                pt = pst.next()
                                for tb in range(2):
                                    S.op("pe", "transpose", pt[:, tb * 128:(tb + 1) * 128],
                                         W[tb][:, ec * 128:(ec + 1) * 128], self.ident[:],
                                         r=[W[tb], self.ident], w=[pt])
                                x = xg.next()
                                S.op("act", "activation", x[:], pa[:], AF.Identity, r=[pa], w=[x])
                                g_ = gl.next()
                                self.gelu_tanh(g_, x, 256, tg_.next())
                                S.op("dve", "tensor_tensor", actT[:, ec, :], g_[:], pt[:], ALU.mult,
                                     r=[g_, pt], w=[actT])
                    with S.scope():
                        gtB = S.sb("pe_gtB", [128, D], F32)
                        self.row_bcast(gtB, 160)
                        vr = S.ring("pe_v", [128, 1024], BF16, 3)
                        x1 = [S.sb(f"pe_x1{i}", [128, D], F32) for i in range(2)]
                        pso = [S.ps(f"pe_pso{i}", [128, 512], F32) for i in range(4)]
                        tmpr = S.ring("pe_tmp", [128, 512], F32, 2)
                        for tb in range(2):
                            r0 = tg * 256 + tb * 128
                            S.dma("sp", x1[tb][:], self.x1_d.t.ap()[r0:r0 + 128, :], r=[self.x1_d], w=[x1[tb]])
                        for dq in range(4):
                            for ec in range(nec):
                                vt = vr.next()
                                S.dma("pool", vt[:], v_src[ec * 128:(ec + 1) * 128, dq * 1024:(dq + 1) * 1024], w=[vt])
                                for tb in range(2):
                                    for d2 in range(2):
                                        S.op("pe", "matmul", pso[tb * 2 + d2][:],
                                             lhsT=actT[:, ec, tb * 128:(tb + 1) * 128],
                                             rhs=vt[:, d2 * 512:(d2 + 1) * 512], start=(ec == 0), stop=(ec == nec - 1),
                                             r=[actT, vt], w=[pso[tb * 2 + d2]])
                            for tb in range(2):
                                for d2 in range(2):
                                    sl = slice(dq * 1024 + d2 * 512, dq * 1024 + (d2 + 1) * 512)
                                    tmp = tmpr.next()
                                    S.op("dve", "tensor_tensor", tmp[:], pso[tb * 2 + d2][:], gtB[:, sl], ALU.mult,
                                         r=[pso[tb * 2 + d2], gtB], w=[tmp])
                                    S.op("pool", "tensor_tensor", x1[tb][:, sl], x1[tb][:, sl], tmp[:], ALU.add,
                                         r=[x1[tb], tmp], w=[x1[tb]])
                        out = self.outs["out"]
                        for tb in range(2):
                            r0 = tg * 256 + tb * 128
                            S.dma("sp", out[0].ap()[r0:r0 + 128, :], x1[tb][:], r=[x1[tb]], w=[out[1]])


def rel_bucket_np(d):
    n = np.maximum(d, 0)
    nf = np.maximum(n, 1).astype(np.float32)
    lb = 16 + (np.log(nf / np.float32(16)) / np.float32(np.log(2048 / 16)) * np.float32(16)).astype(np.int32)
    return np.where(n < 16, n, np.minimum(lb, 31))


def _bias_tile(rel_bias, heads0, dist, valid):
    bk = rel_bucket_np(dist)
    vals = rel_bias[bk][:, :, heads0:heads0 + 16]
    vals = np.where(valid[:, :, None], vals, np.float32(-30000.0))
    return np.ascontiguousarray(vals.reshape(128, 128, 4, 4).transpose(2, 0, 3, 1).reshape(4, 128, 512)).astype(np.float32)


def make_tables(rel_bias, c):
    k = np.arange(128)[:, None]
    q = np.arange(128)[None, :]
    out = {}
    for name, h0 in (("dsa", 0), ("slc", 16)):
        tiles = []
        for op in range(21):
            dist = 128 * (op + c - 7) + q - k
            tiles.append(_bias_tile(rel_bias, h0, dist, dist >= 0))
        out["tab_" + name] = np.stack(tiles)
    tiles = []
    for w in range(12):
        dist = 128 * (c + 4 - w) + q - k
        tiles.append(_bias_tile(rel_bias, 16, dist, (dist >= 0) & (dist < 512)))
    out["tab_win"] = np.stack(tiles)
    tiles = []
    for e8 in range(5):
        e = 8 * e8 if e8 < 4 else 64
        dist = 128 * (e + c) + q - (16 * k + 31)
        tiles.append(_bias_tile(rel_bias, 16, dist, dist >= 0))
    out["tab_cmp"] = np.stack(tiles)
    ft = np.zeros((NJ, 128, 256), np.float32)
    mm = np.arange(256)[None, :]
    for j in range(NJ):
        tq = 128 * (8 * j + c) + np.arange(128)[:, None]
        cur = tq // 64
        forced = (mm == 0) | (mm == cur) | (mm == cur - 1)
        adm = mm * 64 <= tq
        ft[j] = np.where(adm, np.where(forced, 1e9, 0.0), -1e30)
    out["ftab"] = ft
    kk = np.arange(1024)[None, :]
    qq = np.arange(128)[:, None]
    out["cmask"] = np.where(kk <= 128 * c + qq, 0.0, -1e30).astype(np.float32)
    return out

def make_consts():
    n = np.arange(1024)[:, None]
    m = np.arange(256)[None, :]
    ov = ((16 * n < 64 * m + 64) & (16 * n + 32 > 64 * m) & (n < 1023)).astype(np.float32)
    cidx = np.arange(8192)[None, :]
    yexp = (np.arange(128)[:, None] == cidx // 64).astype(np.float32)
    return {"ov": ov, "yexp": yexp}


IN_SPLITS = (2048, 512, 512, 2048, 64, 32, 2048, 3072, 48, 8192)


def prep_inputs(inp):
    offs = np.concatenate([[0], np.cumsum(IN_SPLITS)])
    w_in = inp["w_in"][0]
    seg = lambda i: w_in[:, offs[i]:offs[i + 1]]
    qa, ka, va, qi, ki, wi, qb, kvb, gb, gm = [seg(i) for i in range(10)]
    kv = lambda i: kvb[:, i * 512:(i + 1) * 512]
    w_kT = np.ascontiguousarray(np.concatenate([ka, ki, ki, kv(0), kv(1), kv(2), kv(4)], axis=1))
    w_v = np.ascontiguousarray(np.concatenate([va, kv(3), kv(5)], axis=1))
    w_qT = np.ascontiguousarray(np.concatenate([qa, qi, qb, gm], axis=1))
    w_qs = np.ascontiguousarray(np.concatenate([wi, gb], axis=1))
    colT = lambda v: np.ascontiguousarray(v.reshape(-1, 128).T)
    x = inp["x"][0]
    shared = {
        "xfull": x,
        "cT": colT(inp["c"][0]),
        "w_ada": inp["w_ada"][0],
        "b_adaT": colT(inp["b_ada"][0]),
        "gnT": np.ascontiguousarray(np.concatenate([colT(inp["g_mix"][0]), colT(inp["g_ffn"][0])], axis=1)),
        "gains": np.ascontiguousarray(np.stack([inp[k][0] for k in
                                                ("gq_a", "gk_a", "gq_b", "gk_cmp", "gk_slc", "gk_win")], axis=1)),
        "w_kT": w_kT, "w_v": w_v, "w_qT": w_qT, "w_qs": w_qs,
        "cmp_w1_k": inp["cmp_w1_k"][0], "cmp_w1_v": inp["cmp_w1_v"][0],
        "cmp_w2_k": inp["cmp_w2_k"][0], "cmp_w2_v": inp["cmp_w2_v"][0],
        "cmp_peT_k": np.ascontiguousarray(inp["cmp_pe_k"][0].T), "cmp_peT_v": np.ascontiguousarray(inp["cmp_pe_v"][0].T),
        "w_branch_a": inp["w_branch_a"][0], "w_branch_b": inp["w_branch_b"][0], "w_out": inp["w_out"][0],
        "w_peer_q": inp["w_peer_q"][0],
        "peer_keysT": np.ascontiguousarray(inp["peer_sub_keys"][0].reshape(16, 128, 128).transpose(0, 2, 1)),
        "peer_u": inp["peer_u"][0], "peer_v": inp["peer_v"][0],
    }
    shared.update(make_consts())
    maps = []
    xb = x.reshape(NTB, 128, D)
    for c in range(NCORES):
        m = dict(shared)
        m["xown"] = np.ascontiguousarray(xb[c::8].reshape(NOWN, D))
        m.update(make_tables(inp["rel_bias"], c))
        maps.append(m)
    return maps


_CACHE = {}


def kernel(**inputs):
    inputs = {k: np.asarray(v) for k, v in inputs.items()}
    if "prog" not in _CACHE:
        p = Prog()
        p.build()
        _CACHE["prog"] = p
    p = _CACHE["prog"]
    maps = prep_inputs(inputs)
    maps = [{k: np.ascontiguousarray(v, dtype=np.float32) for k, v in m.items() if k in p.inp} for m in maps]
    res = run_bass_kernel_spmd(p.nc, maps, core_ids=list(range(NCORES)))
    out = np.zeros((1, T, D), np.float32)
    ob = out[0].reshape(NTB, 128, D)
    for c in range(NCORES):
        ob[c::8] = res.results[c]["out"].reshape(NJ, 128, D)
    return out
```

```python
import contextlib
import numpy as np
import ml_dtypes
import concourse.bass as bass
import concourse.mybir as mybir
from concourse.bass_utils import run_bass_kernel_spmd

F32 = mybir.dt.float32
BF16 = mybir.dt.bfloat16
AF = mybir.ActivationFunctionType
ALU = mybir.AluOpType
AX = mybir.AxisListType

NCORES = 8
T = 16384
D = 4096
NTB = T // 128
NOWN = 2048
NJ = 16
EPS = 1e-6
KC = 32


class Tok:
    __slots__ = ("lw", "rd", "dsem", "dcnt", "name")

    def __init__(self, name=""):
        self.lw = None
        self.rd = {}
        self.dsem = None
        self.dcnt = 0
        self.name = name


class Buf:
    def __init__(self, t, tok):
        self.t = t
        self.tok = tok

    def __getitem__(self, k):
        return self.t[k]


class Ring:
    def __init__(self, bufs):
        self.bufs = bufs
        self.i = 0

    def next(self):
        b = self.bufs[self.i % len(self.bufs)]
        self.i += 1
        return b


class Sched:
    def __init__(self, nc):
        self.nc = nc
        self.E = {"pe": nc.tensor, "act": nc.scalar, "dve": nc.vector, "pool": nc.gpsimd, "sp": nc.sync}
        self.sem = {k: nc.semaphore("se_" + k).__enter__() for k in self.E}
        self.cnt = {k: 0 for k in self.E}
        self.seen = {k: {} for k in self.E}
        self.dsems = []
        self.free_dsems = []
        self.dsem_cnt = {}
        self.scope_toks = [[]]
        self.ninst = 0
        self.stack = None

    def scope(self):
        return _Scope(self)

    def _enter(self, cm):
        return self.stack.enter_context(cm)

    def _nm(self, name):
        self.nuid = getattr(self, "nuid", 0) + 1
        return f"{name}_{self.nuid}"

    def sb(self, name, shape, dtype):
        return Buf(self._enter(self.nc.sbuf_tensor(self._nm(name), list(shape), dtype)), Tok(name))

    def ps(self, name, shape, dtype):
        return Buf(self._enter(self.nc.psum_tensor(self._nm(name), list(shape), dtype)), Tok(name))

    def dram(self, name, shape, dtype):
        return Buf(self.nc.dram_tensor(name, list(shape), dtype, kind="Internal"), Tok(name))

    def ring(self, name, shape, dtype, n):
        return Ring([self.sb(f"{name}{i}", shape, dtype) for i in range(n)])

    def psring(self, name, shape, dtype, n):
        return Ring([self.ps(f"{name}{i}", shape, dtype) for i in range(n)])

    @staticmethod
    def _toks(xs):
        return [x.tok if isinstance(x, Buf) else x for x in xs]

    @staticmethod
    def _deps(r, w):
        deps = []
        for t in r:
            if t.lw is not None:
                deps.append(t.lw)
        for t in w:
            if t.lw is not None:
                deps.append(t.lw)
            deps.extend(t.rd.values())
        return deps

    def _wait(self, eng, deps, skip_own=False):
        seen = self.seen[eng]
        own = self.sem[eng]
        for sem, val in deps:
            if skip_own and sem is own:
                continue
            k = id(sem)
            if seen.get(k, 0) < val:
                self.E[eng].wait_ge(sem, val)
                seen[k] = val

    @staticmethod
    def _commit(me, r, w):
        k = id(me[0])
        for t in r:
            t.rd[k] = me
        for t in w:
            t.lw = me
            t.rd = {}

    def op(self, eng, meth, *args, r=(), w=(), **kw):
        r = self._toks(r)
        w = self._toks(w)
        self._wait(eng, self._deps(r, w), skip_own=(eng == "pe"))
        inst = getattr(self.E[eng], meth)(*args, **kw)
        self.cnt[eng] += 1
        inst.then_inc(self.sem[eng], 1)
        self._commit((self.sem[eng], self.cnt[eng]), r, w)
        self.ninst += 1
        return inst

    def dma(self, q, out, in_, r=(), w=(), **kw):
        r = self._toks(r)
        w = self._toks(w)
        self._wait(q, self._deps(r, w))
        inst = self.E[q].dma_start(out=out, in_=in_, **kw)
        t = w[0]
        if t.dsem is None:
            if self.free_dsems:
                t.dsem, t.dcnt = self.free_dsems.pop()
            else:
                t.dsem = self.nc.semaphore(f"sd{len(self.dsems)}").__enter__()
                t.dcnt = 0
                self.dsems.append(t.dsem)
            self.scope_toks[-1].append(t)
        t.dcnt += 16
        inst.then_inc(t.dsem, 16)
        self.dsem_cnt[id(t.dsem)] = (t.dsem, t.dcnt)
        self._commit((t.dsem, t.dcnt), r, w)
        self.ninst += 1
        return inst

    def barrier(self):
        deps = [(self.sem[k], self.cnt[k]) for k in self.E if self.cnt[k] > 0]
        deps += list(self.dsem_cnt.values())
        for eng in self.E:
            self._wait(eng, deps)

    def release_scope_sems(self, toks):
        for t in toks:
            if t.dsem is not None:
                self.free_dsems.append((t.dsem, t.dcnt))
                t.dsem = None
                t.dcnt = 0


class _Scope:
    def __init__(self, S):
        self.S = S

    def __enter__(self):
        self.prev = self.S.stack
        self.es = contextlib.ExitStack()
        self.es.__enter__()
        self.S.stack = self.es
        self.S.scope_toks.append([])
        return self

    def __exit__(self, *a):
        self.S.barrier()
        self.S.release_scope_sems(self.S.scope_toks.pop())
        self.S.stack = self.prev
        return self.es.__exit__(*a)


KCH = (["n1"] * 4) + ["c"] + (["c"] * 8) + (["n4"] * 4) + (["n5"] * 4)
NKCH = len(KCH)
QCH = (["n0"] * 16) + (["c"] * 16) + (["n2"] * 16) + (["s"] * 64)
NQCH = len(QCH)


class Prog:
    def __init__(self, stop="all", dbg=()):
        self.stop = stop
        self.dbg = set(dbg)
        nc = bass.Bass("TRN2", target_bir_lowering=False)
        self.nc = nc
        self.S = Sched(nc)
        self.inp = {}
        self.outs = {}

    def din(self, name, shape, dtype=F32):
        t = self.nc.dram_tensor(name, list(shape), dtype, kind="ExternalInput")
        self.inp[name] = t
        return t

    INSHAPES = {
        "xfull": [T, D], "xown": [NOWN, D], "cT": [128, KC], "w_ada": [D, 6 * D], "b_adaT": [128, 192],
        "gnT": [128, 64], "gains": [128, 6], "w_kT": [D, 21 * 128], "w_v": [D, 1536],
        "w_qT": [D, 112 * 128], "w_qs": [D, 80],
    }

    def __getattr__(self, name):
        if name.startswith("i_") and name[2:] in self.INSHAPES:
            nm = name[2:]
            if nm not in self.inp:
                shp = list(self.INSHAPES[nm])
                if "small" in self.dbg and nm == "xfull":
                    shp[0] = 1024
                if "small" in self.dbg and nm == "w_qT":
                    shp[1] = 1024
                self.din(nm, shp)
            return self.inp[nm]
        raise AttributeError(name)

    def dout(self, name, shape, dtype=F32):
        t = self.nc.dram_tensor(name, list(shape), dtype, kind="ExternalOutput")
        self.outs[name] = (t, Tok(name))
        return t

    def build(self):
        nc, S = self.nc, self.S
        self.hT_all = S.dram("hT_all", [32, 128, KC, 512], BF16)
        self.hT_own = S.dram("hT_own", [4, 128, KC, 512], BF16)
        self.kT_all = S.dram("kT_all", [NKCH, 128, T], BF16)
        self.v_all = S.dram("v_all", [3, 4, 128, NTB, 128], BF16)
        self.qT_all = S.dram("qT_all", [NQCH, 128, NOWN], BF16)

        with S.scope():
            self.consts()
            with S.scope():
                if "nomod" in self.dbg:
                    self.din("modc_in", [128, 192])
                    S.dma("sp", self.modc[:], self.inp["modc_in"].ap()[:, :], w=[self.modc])
                    self.mod_to_AB()
                else:
                    self.phase0()
            if self.stop == "p0":
                return self.finish()
            if "inj" in self.dbg:
                self.inject()
                self.attention_and_rest()
                return self.finish()
            with S.scope():
                self.phase1(self.i_xfull, self.hT_all, NTB if "small" not in self.dbg else 8)
                self.phase1(self.i_xown, self.hT_own, NJ)
            if self.stop == "p1":
                return self.finish()
            with S.scope():
                self.proj_kv()
            with S.scope():
                self.proj_q()
            if self.stop == "p2":
                return self.finish()
            self.attention_and_rest()
            return self.finish()

    def inject(self):
        S = self.S
        self.kT_all = Buf(self.din("kT_in", [NKCH, 128, T], BF16), Tok("kT_in"))
        self.v_all = Buf(self.din("v_in", [3, 4, 128, NTB, 128], BF16), Tok("v_in"))
        self.qT_all = Buf(self.din("qT_in", [NQCH, 128, NOWN], BF16), Tok("qT_in"))
        S.dma("sp", self.wis[:], self.din("wis_in", [128, NJ, 32]).ap()[:, :, :], w=[self.wis])
        S.dma("sp", self.gbs[:], self.din("gbs_in", [128, NJ, 48]).ap()[:, :, :], w=[self.gbs])

    def attention_and_rest(self):
        S = self.S
        self.yT_all = S.dram("yT_all", [32, 128, NOWN], BF16)
        if "yinj" in self.dbg:
            self.yT_all = Buf(self.din("yT_in", [32, 128, NOWN], BF16), Tok("yT_in"))
        else:
            self.prep_tables()
        cum = "cum" in self.dbg
        lvl = ["dsa", "nsa", "merge", "all"].index(self.stop) if (cum and self.stop in ("dsa", "nsa", "merge", "all")) else -1
        if self.stop in ("dsa", "all") or lvl >= 0:
            self.dsa_index()
            self.dsa_attn()
        if self.stop in ("nsa", "all") or lvl >= 1:
            self.nsa_compress()
            self.nsa_attn()
        if self.stop in ("merge", "mp", "all") or lvl >= 2:
            self.merge()
        if self.stop == "peer" and "inj" in self.dbg:
            self.x1_d = Buf(self.din("x1_in", [NOWN, D]), Tok("x1_in"))
            self.hT2_own = S.dram("hT2_own", [4, 128, KC, 512], BF16)
            self.phase1(self.x1_d.t, self.hT2_own, NJ, AB_off=64, src_tok=self.x1_d)
        if self.stop in ("peer", "mp", "all"):
            self.dout("out", [NOWN, D])
            self.peer()
        if "y" in self.dbg:
            o = self.dout("d_yT", [32, 128, 256], BF16)
            S.dma("sp", o.ap()[:, :, :], self.yT_all.t.ap()[:, :, 0:256], r=[self.yT_all],
                  w=[self.outs["d_yT"][1]])

    def finish(self):
        S = self.S
        toks = [tok for (_, tok) in self.outs.values()]
        deps = [t.lw for t in toks if t.lw is not None]
        S._wait("sp", deps)
        S.barrier()
        return self.nc

    def consts(self):
        S = self.S
        self.identf = S.sb("identf", [128, 128], F32)
        self.ident = S.sb("ident", [128, 128], BF16)
        self.ones_bf = S.sb("ones_bf", [128, 128], BF16)
        self.modc = S.sb("modc", [128, 192], F32)
        self.AB = S.sb("AB", [128, 128], F32)
        self.gn = S.sb("gn", [128, 64], F32)
        self.gcol = S.sb("gcol", [128, 6], F32)
        S.op("pool", "memset", self.identf[:], 1.0, w=[self.identf])
        S.op("pool", "affine_select", self.identf[:], self.identf[:], pattern=[[-1, 128]],
             compare_op=ALU.is_equal, fill=0.0, base=0, channel_multiplier=1,
             r=[self.identf], w=[self.identf])
        S.op("dve", "tensor_copy", self.ident[:], self.identf[:], r=[self.identf], w=[self.ident])
        S.op("dve", "memset", self.ones_bf[:], 1.0, w=[self.ones_bf])
        self.wis = S.sb("wis", [128, NJ, 32], F32)
        self.gbs = S.sb("gbs", [128, NJ, 48], F32)
        self.eps128 = S.sb("eps128", [128, 1], F32)
        S.op("dve", "memset", self.eps128[:], 128.0 * EPS, w=[self.eps128])
        S.dma("sp", self.gn[:], self.i_gnT.ap()[:, :], w=[self.gn])
        S.dma("sp", self.gcol[:], self.i_gains.ap()[:, :], w=[self.gcol])
        S.op("dve", "tensor_scalar", self.gcol[:], self.gcol[:], float(np.sqrt(128.0)), None, ALU.mult,
             r=[self.gcol], w=[self.gcol])

    def phase0(self):
        S = self.S
        cT = S.sb("cTs", [128, KC], F32)
        bT = S.sb("bTs", [128, 192], F32)
        S.dma("sp", cT[:], self.i_cT.ap()[:, :], w=[cT])
        S.dma("sp", bT[:], self.i_b_adaT.ap()[:, :], w=[bT])
        ps = S.ps("ps_mod", [128, 192], F32)
        prow = S.psring("ps_row", [1, 512], F32, 2)
        rows = S.ring("modrow", [1, 512], F32, 2)
        one = S.sb("one11", [1, 1], F32)
        S.op("dve", "memset", one[:], 1.0, w=[one])
        wr = S.ring("wada", [128, KC, 512], F32, 2)
        wa = self.i_w_ada.ap().rearrange("(kc p) n -> p kc n", p=128)
        qs = ["sp", "pool"]
        for ct in range(48):
            wt = wr.next()
            for hh in range(2):
                S.dma(qs[hh], wt[:, hh * 16:(hh + 1) * 16, :], wa[:, hh * 16:(hh + 1) * 16, ct * 512:(ct + 1) * 512],
                      w=[wt])
            pr = prow.next()
            for kc in range(KC):
                S.op("pe", "matmul", pr[:], lhsT=cT[:, kc:kc + 1], rhs=wt[:, kc, :],
                     start=(kc == 0), stop=(kc == KC - 1), r=[wt, cT], w=[pr])
            row = rows.next()
            S.op("act", "activation", row[:], pr[:], AF.Identity, r=[pr], w=[row])
            for i in range(4):
                jc = ct * 4 + i
                S.op("pe", "matmul", ps[:, jc:jc + 1], lhsT=row[0:1, i * 128:(i + 1) * 128], rhs=one[0:1, 0:1],
                     start=True, stop=True, r=[row, one], w=[ps])
        S.op("dve", "tensor_tensor", self.modc[:], ps[:], bT[:], ALU.add, r=[ps, bT], w=[self.modc])
        self.mod_to_AB()
        if "modc" in self.dbg:
            o = self.dout("d_modc", [128, 192])
            S.dma("sp", o.ap()[:, :], self.modc[:], r=[self.modc], w=[self.outs["d_modc"][1]])

    def mod_to_AB(self):
        S = self.S
        m, AB, gn = self.modc, self.AB, self.gn
        S.op("dve", "scalar_tensor_tensor", AB[:, 0:32], m[:, 32:64], 1.0, gn[:, 0:32], ALU.add, ALU.mult,
             r=[m, gn], w=[AB])
        S.op("dve", "tensor_copy", AB[:, 32:64], m[:, 0:32], r=[m], w=[AB])
        S.op("dve", "scalar_tensor_tensor", AB[:, 64:96], m[:, 128:160], 1.0, gn[:, 32:64], ALU.add, ALU.mult,
             r=[m, gn], w=[AB])
        S.op("dve", "tensor_copy", AB[:, 96:128], m[:, 96:128], r=[m], w=[AB])

    def norm_hT(self, xt, AB_off, hT, col0, ps_ring, xn_ring, junk, small):
        S = self.S
        ss, rstd = small
        S.op("act", "activation", junk[:], xt[:], AF.Square, accum_out=ss[:], r=[xt], w=[junk, ss])
        S.op("dve", "tensor_scalar", rstd[:], ss[:], 1.0 / D, EPS, ALU.mult, ALU.add, r=[ss], w=[rstd])
        S.op("act", "activation", rstd[:], rstd[:], AF.Sqrt, r=[rstd], w=[rstd])
        S.op("dve", "reciprocal", rstd[:], rstd[:], r=[rstd], w=[rstd])
        xn = xn_ring.next()
        S.op("dve", "tensor_scalar", xn[:], xt[:], rstd[:, 0:1], None, ALU.mult, r=[xt, rstd], w=[xn])
        for k4 in range(KC // 8):
            pt = ps_ring.next()
            for i in range(8):
                kc = k4 * 8 + i
                S.op("pe", "transpose", pt[:, i * 128:(i + 1) * 128], xn[:, kc * 128:(kc + 1) * 128],
                     self.ident[:], r=[xn, self.ident], w=[pt])
            for i in range(8):
                kc = k4 * 8 + i
                A = self.AB[:, AB_off + kc:AB_off + kc + 1]
                B = self.AB[:, AB_off + 32 + kc:AB_off + 32 + kc + 1]
                if i % 2 == 0:
                    S.op("act", "activation", hT[:, kc, col0:col0 + 128], pt[:, i * 128:(i + 1) * 128],
                         AF.Identity, bias=B, scale=A, r=[pt, self.AB], w=[hT])
                else:
                    S.op("dve", "tensor_scalar", hT[:, kc, col0:col0 + 128], pt[:, i * 128:(i + 1) * 128],
                         A, B, ALU.mult, ALU.add, r=[pt, self.AB], w=[hT])

    def phase1(self, xsrc, hdst, nblk, AB_off=0, src_tok=None):
        S = self.S
        with S.scope():
            xr = S.ring("p1x", [128, D], F32, 2)
            xnr = S.ring("p1xn", [128, D], BF16, 2)
            junk = S.sb("p1junk", [128, D], BF16)
            hr = S.ring("p1h", [128, KC, 512], BF16, 2)
            psr = S.psring("p1ps", [128, 1024], BF16, 3)
            smalls = [(S.sb(f"p1ss{i}", [128, 1], F32), S.sb(f"p1rs{i}", [128, 1], F32)) for i in range(2)]
            xa = xsrc.ap()
            for tb in range(nblk):
                if tb % 4 == 0:
                    hT = hr.next()
                xt = xr.next()
                S.dma("sp", xt[:], xa[tb * 128:(tb + 1) * 128, :], r=([src_tok] if src_tok is not None else []),
                      w=[xt])
                self.norm_hT(xt, AB_off, hT, (tb % 4) * 128, psr, xnr, junk, smalls[tb % 2])
                if tb % 4 == 3:
                    S.dma("pool", hdst.t.ap()[tb // 4], hT[:], r=[hT], w=[hdst])

    def load_w(self, wsrc, col0, ncols, wt, q="pool"):
        S = self.S
        wa = wsrc.ap()
        for kc in range(KC):
            S.dma(q, wt[:, kc, 0:ncols], wa[kc * 128:(kc + 1) * 128, col0:col0 + ncols], w=[wt])

    def evac_T(self, kind, ps, ot, n, tmp):
        S = self.S
        if kind == "c":
            S.op("act", "activation", ot[:, 0:n], ps[:, 0:n], AF.Identity, r=[ps], w=[ot])
        elif kind == "s":
            S.op("act", "activation", ot[:, 0:n], ps[:, 0:n], AF.Sigmoid, r=[ps], w=[ot])
        else:
            gi = int(kind[1:])
            sq, ps2, rr = tmp
            S.op("act", "activation", sq[:, 0:n], ps[:, 0:n], AF.Square, r=[ps], w=[sq])
            S.op("pe", "matmul", ps2[:, 0:n], lhsT=self.ones_bf[:], rhs=sq[:, 0:n], start=True, stop=True,
                 r=[sq, self.ones_bf], w=[ps2])
            S.op("act", "activation", rr[:, 0:n], ps2[:, 0:n], AF.Sqrt, bias=self.eps128[:, 0:1],
                 r=[ps2, self.eps128], w=[rr])
            S.op("dve", "reciprocal", rr[:, 0:n], rr[:, 0:n], r=[rr], w=[rr])
            S.op("dve", "scalar_tensor_tensor", ot[:, 0:n], ps[:, 0:n], self.gcol[:, gi:gi + 1], rr[:, 0:n],
                 ALU.mult, ALU.mult, r=[ps, self.gcol, rr], w=[ot])

    def proj_kv(self):
        S = self.S
        ntile = 32 if "small" not in self.dbg else 2
        hr = S.ring("kvh", [128, KC, 512], BF16, 2)
        wk = S.sb("kvwk", [128, KC, 7 * 128], BF16)
        wv = S.sb("kvwv", [128, KC, 512], BF16)
        psr = S.psring("kvps", [128, 512], F32, 4)
        ps2r = S.psring("kvps2", [128, 512], F32, 2)
        otr = S.ring("kvot", [128, 512], BF16, 4)
        sqr = S.ring("kvsq", [128, 512], BF16, 2)
        rrr = S.ring("kvrr", [128, 512], F32, 2)
        kTa = self.kT_all.t.ap()
        va = self.v_all.t.ap()
        for p in range(3):
            self.load_w(self.i_w_kT, p * 7 * 128, 7 * 128, wk)
            self.load_w(self.i_w_v, p * 512, 512, wv)
            for tt in range(ntile):
                hT = hr.next()
                S.dma("sp", hT[:], self.hT_all.t.ap()[tt], r=[self.hT_all], w=[hT])
                for ci in range(7):
                    ch = p * 7 + ci
                    ps = psr.next()
                    for kc in range(KC):
                        S.op("pe", "matmul", ps[:], lhsT=wk[:, kc, ci * 128:(ci + 1) * 128], rhs=hT[:, kc, :],
                             start=(kc == 0), stop=(kc == KC - 1), r=[wk, hT], w=[ps])
                    ot = otr.next()
                    self.evac_T(KCH[ch], ps, ot, 512, (sqr.next(), ps2r.next(), rrr.next()))
                    S.dma("sp", kTa[ch, :, tt * 512:(tt + 1) * 512], ot[:], r=[ot], w=[self.kT_all])
                for tb in range(4):
                    ps = psr.next()
                    for kc in range(KC):
                        S.op("pe", "matmul", ps[:], lhsT=hT[:, kc, tb * 128:(tb + 1) * 128], rhs=wv[:, kc, :],
                             start=(kc == 0), stop=(kc == KC - 1), r=[wv, hT], w=[ps])
                    ot = otr.next()
                    S.op("dve", "tensor_copy", ot[:], ps[:], r=[ps], w=[ot])
                    blk = tt * 4 + tb
                    S.dma("sp", va[p, :, :, blk, :].rearrange("g p d -> p g d"),
                          ot[:].rearrange("p (g d) -> p g d", g=4), r=[ot], w=[self.v_all])
        if "kv" in self.dbg:
            o = self.dout("d_kT", [NKCH, 128, 1024], BF16)
            S.dma("sp", o.ap()[:, :, :], kTa[:, :, 0:1024], r=[self.kT_all], w=[self.outs["d_kT"][1]])
            o = self.dout("d_v", [3, 4, 128, 8, 128], BF16)
            for i3 in range(3):
                S.dma("sp", o.ap()[i3], va[i3, :, :, 0:8, :], r=[self.v_all], w=[self.outs["d_v"][1]])

    def proj_q(self):
        S = self.S
        hT4 = S.sb("qh", [128, 4, KC, 512], BF16) if False else None
        hts = [S.sb(f"qh{i}", [128, KC, 512], BF16) for i in range(2)]
        G = 4
        wr = S.ring("qw", [128, KC, G * 128], BF16, 2)
        psr = S.psring("qps", [128, 512], F32, 4)
        ps2r = S.psring("qps2", [128, 512], F32, 2)
        otr = S.ring("qot", [128, 512], BF16, 4)
        sqr = S.ring("qsq", [128, 512], BF16, 2)
        rrr = S.ring("qrr", [128, 512], F32, 2)
        qTa = self.qT_all.t.ap()
        ngrp = NQCH // G if "small" not in self.dbg else 2
        wqs = S.sb("qwqs", [128, KC, 80], BF16)
        self.load_w(self.i_w_qs, 0, 80, wqs)
        for half in range(2):
            for i in range(2):
                S.dma("sp", hts[i][:], self.hT_own.t.ap()[half * 2 + i], r=[self.hT_own], w=[hts[i]])
            for i in range(2):
                for tb in range(4):
                    if "noqs" in self.dbg:
                        continue
                    j = (half * 2 + i) * 4 + tb
                    ps = psr.next()
                    for kc in range(KC):
                        S.op("pe", "matmul", ps[:, 0:80], lhsT=hts[i][:, kc, tb * 128:(tb + 1) * 128],
                             rhs=wqs[:, kc, :], start=(kc == 0), stop=(kc == KC - 1), r=[wqs, hts[i]], w=[ps])
                    S.op("act", "activation", self.wis[:, j, :], ps[:, 0:32], AF.Identity,
                         scale=float(1.0 / (8.0 * np.sqrt(32.0))), r=[ps], w=[self.wis])
                    S.op("act", "activation", self.gbs[:, j, :], ps[:, 32:80], AF.Sigmoid, r=[ps], w=[self.gbs])
            for g in range(ngrp):
                wt = wr.next()
                self.load_w(self.i_w_qT, g * G * 128, G * 128, wt)
                for i in range(2):
                    tt = half * 2 + i
                    for ci in range(G):
                        ch = g * G + ci
                        ps = psr.next()
                        for kc in range(KC):
                            S.op("pe", "matmul", ps[:], lhsT=wt[:, kc, ci * 128:(ci + 1) * 128],
                                 rhs=hts[i][:, kc, :], start=(kc == 0), stop=(kc == KC - 1),
                                 r=[wt, hts[i]], w=[ps])
                        ot = otr.next()
                        self.evac_T(QCH[ch], ps, ot, 512, (sqr.next(), ps2r.next(), rrr.next()))
                        S.dma("sp", qTa[ch, :, tt * 512:(tt + 1) * 512], ot[:], r=[ot], w=[self.qT_all])
        if "q" in self.dbg:
            o = self.dout("d_qT", [NQCH, 128, 512], BF16)
            S.dma("sp", o.ap()[:, :, :], qTa[:, :, 0:512], r=[self.qT_all], w=[self.outs["d_qT"][1]])


    def prep_tables(self):
        S = self.S
        self.tabs = {}
        with S.scope():
            raw = S.ring("tbraw", [128, 4, 512], F32, 2)
            et = S.ring("tbe", [128, 4, 512], BF16, 2)
            for name, nt in (("dsa", 21), ("slc", 21), ("win", 12), ("cmp", 5)):
                src = self.din("tab_" + name, [nt, 4, 128, 512])
                dst = S.dram("tabe_" + name, [nt, 4, 128, 512], BF16)
                self.tabs[name] = dst
                for t in range(nt):
                    r = raw.next()
                    S.dma("sp", r[:], src.ap()[t].rearrange("g p n -> p g n"), w=[r])
                    e = et.next()
                    S.op("act", "activation", e[:], r[:], AF.Exp, r=[r], w=[e])
                    S.dma("pool", dst.t.ap()[t].rearrange("g p n -> p g n"), e[:], r=[e], w=[dst])

    def load_tab(self, name, g, tab, nt):
        S = self.S
        src = self.tabs[name]
        S.dma("pool", tab[:, 0:nt, :], src.t.ap()[:, g].rearrange("o p n -> p o n"), r=[src], w=[tab])

    def dsa_index(self):
        S = self.S
        nj = NJ if "small" not in self.dbg else 2
        self.maskT_d = S.dram("maskT_d", [NJ, 128, 128, 128], BF16)
        with S.scope():
            score = S.sb("ix_score", [128, T], F32)
            junk = S.sb("ix_junk", [128, 4096], BF16)
            mst = S.sb("ix_mst", [128, 128, 128], BF16)
            qi = S.ring("ix_qi", [128, 16, 128], BF16, 2)
            dh = S.ring("ix_dh", [128, 32, 128], BF16, 2)
            kir = S.ring("ix_ki", [128, 2048], BF16, 2)
            rl = S.ring("ix_rl", [128, 512], BF16, 6)
            cm = S.sb("ix_cm", [128, 1024], F32)
            jf = S.sb("ix_jf", [128, 1024], F32)
            half = S.sb("ix_half", [128, 1], F32)
            sm = {k: S.sb("ix_" + k, [128, 1], F32) for k in ("lo", "hi", "mid", "cnt", "pred", "d1", "d2", "t1", "t2")}
            ps_s = S.psring("ix_pss", [128, 512], F32, 4)
            ps_c = S.psring("ix_psc", [128, 512], F32, 2)
            ps_t = S.psring("ix_pst", [128, 1024], BF16, 2)
            S.dma("sp", cm[:], self.din("cmask", [128, 1024]).ap()[:, :], w=[cm])
            S.op("dve", "memset", half[:], 0.5, w=[half])
            qTa = self.qT_all.t.ap()
            kia = self.kT_all.t.ap()[4]
            for j in range(nj):
                nkb = 8 * j + 8
                Tk = nkb * 128
                q = qi.next()
                S.dma("sp", q[:], qTa[16:32, :, j * 128:(j + 1) * 128].rearrange("c p q -> p c q"),
                      r=[self.qT_all], w=[q])
                d = dh.next()
                for h in range(32):
                    S.op("act", "activation", d[:, h, :], self.ident[:], AF.Identity,
                         scale=self.wis[:, j, h:h + 1], r=[self.ident, self.wis], w=[d])
                items = [(kt, h) for kt in range(nkb // 4) for h in range(32)]
                kis = {}
                pcs = {}
                rls = {}

                def ix_a(kt, h):
                    if h == 0 and kt % 4 == 0:
                        ki = kir.next()
                        n = min(2048, Tk - kt * 512)
                        S.dma("sp", ki[:, 0:n], kia[:, kt * 512:kt * 512 + n], r=[self.kT_all], w=[ki])
                        kis[kt // 4] = ki
                    ki = kis[kt // 4]
                    ko = (kt % 4) * 512
                    pb = (h % 2) * 64
                    ps = ps_s.next()
                    S.op("pe", "matmul", ps[:], lhsT=q[pb:pb + 64, h // 2, :], rhs=ki[pb:pb + 64, ko:ko + 512],
                         start=True, stop=True, r=[q, ki], w=[ps])
                    r = rl.next()
                    S.op("act", "activation", r[:], ps[:], AF.Relu, r=[ps], w=[r])
                    rls[(kt, h)] = r

                def ix_b(kt, h):
                    if h == 0:
                        pcs[kt] = ps_c.next()
                    pc = pcs[kt]
                    r = rls.pop((kt, h))
                    S.op("pe", "matmul", pc[:], lhsT=d[:, h, :], rhs=r[:], start=(h == 0), stop=(h == 31),
                         r=[d, r], w=[pc])
                    if h == 31:
                        if kt >= nkb // 4 - 2:
                            co = (kt - (nkb // 4 - 2)) * 512
                            S.op("dve", "tensor_tensor", score[:, kt * 512:(kt + 1) * 512], pc[:], cm[:, co:co + 512],
                                 ALU.add, r=[pc, cm], w=[score])
                        else:
                            S.op("dve", "tensor_copy", score[:, kt * 512:(kt + 1) * 512], pc[:],
                                 r=[pc], w=[score])

                LAI = 3
                for n in range(len(items) + LAI):
                    if n < len(items):
                        ix_a(*items[n])
                    if n >= LAI:
                        ix_b(*items[n - LAI])
                lo, hi, mid, cnt, pred = sm["lo"], sm["hi"], sm["mid"], sm["cnt"], sm["pred"]
                d1, d2, t1, t2 = sm["d1"], sm["d2"], sm["t1"], sm["t2"]
                S.op("dve", "tensor_tensor", jf[:], score[:, Tk - 1024:Tk], cm[:], ALU.subtract,
                     r=[score, cm], w=[jf])
                S.op("dve", "tensor_reduce", t1[:], jf[:], AX.X, ALU.min, r=[jf], w=[t1])
                if Tk > 1024:
                    S.op("dve", "tensor_reduce", t2[:], score[:, 0:Tk - 1024], AX.X, ALU.min, r=[score], w=[t2])
                    S.op("dve", "tensor_tensor", lo[:], t1[:], t2[:], ALU.min, r=[t1, t2], w=[lo])
                else:
                    S.op("dve", "tensor_copy", lo[:], t1[:], r=[t1], w=[lo])
                S.op("dve", "tensor_reduce", hi[:], score[:, 0:Tk], AX.X, ALU.max, r=[score], w=[hi])
                S.op("dve", "tensor_scalar", hi[:], hi[:], 1.0, None, ALU.add, r=[hi], w=[hi])
                for it in range(26):
                    S.op("dve", "scalar_tensor_tensor", mid[:], lo[:], hi[:, 0:1], half[:], ALU.add, ALU.mult,
                         r=[lo, hi, half], w=[mid])
                    nch = (Tk + 4095) // 4096
                    for ci in range(nch):
                        c0 = ci * 4096
                        n = min(4096, Tk - c0)
                        init = 0.0 if ci == 0 else cnt[:, 0:1]
                        S.op("dve", "tensor_scalar", junk[:, 0:n], score[:, c0:c0 + n], mid[:, 0:1], init,
                             ALU.is_ge, ALU.add, accum_out=cnt[:], r=[score, mid, cnt], w=[junk, cnt])
                    S.op("dve", "tensor_scalar", pred[:], cnt[:], 255.5, None, ALU.is_ge, r=[cnt], w=[pred])
                    S.op("dve", "tensor_tensor", d1[:], mid[:], lo[:], ALU.subtract, r=[mid, lo], w=[d1])
                    S.op("dve", "tensor_tensor", d2[:], hi[:], mid[:], ALU.subtract, r=[mid, hi], w=[d2])
                    S.op("dve", "scalar_tensor_tensor", lo[:], d1[:], pred[:, 0:1], lo[:], ALU.mult, ALU.add,
                         r=[d1, pred, lo], w=[lo])
                    S.op("dve", "scalar_tensor_tensor", hi[:], d2[:], pred[:, 0:1], mid[:], ALU.mult, ALU.add,
                         r=[d2, pred, mid], w=[hi])
                for c8 in range(nkb // 8):
                    S.op("dve", "tensor_scalar", junk[:, 0:1024], score[:, c8 * 1024:(c8 + 1) * 1024], lo[:, 0:1], None,
                         ALU.is_ge, r=[score, lo], w=[junk])
                    pt = ps_t.next()
                    for i in range(8):
                        S.op("pe", "transpose", pt[:, i * 128:(i + 1) * 128], junk[:, i * 128:(i + 1) * 128],
                             self.ident[:], r=[junk, self.ident], w=[pt])
                    S.op("act", "activation", mst[:, c8 * 8:(c8 + 1) * 8, :].rearrange("p a b -> p (a b)"), pt[:],
                         AF.Identity, r=[pt], w=[mst])
                S.dma("sp", self.maskT_d.t.ap()[j, :, 0:nkb, :], mst[:, 0:nkb, :], r=[mst], w=[self.maskT_d])
            if "ix" in self.dbg:
                o = self.dout("d_score", [128, 2048])
                S.dma("sp", o.ap()[:, :], score[:, 0:2048], r=[score], w=[self.outs["d_score"][1]])
                o = self.dout("d_lo", [128, 1])
                S.dma("sp", o.ap()[:, :], sm["lo"][:], r=[sm["lo"]], w=[self.outs["d_lo"][1]])

    def attn(self, P, qT, qtok, kT_dram, ksrc, v_dram, vsrc, kb0, kb1, tab, tid_fn, mask_fn, keep=None):
        S = self.S
        oT = P["oT"].next()
        den = P["den"].next()
        scale = float(128.0 ** -0.5)
        LA = 2
        nblk = kb1 - kb0
        chunks = {}

        def load_chunk(ci):
            c0 = kb0 + ci * 16
            if c0 >= kb1 or ci in chunks:
                return
            nb = min(16, kb1 - c0)
            kt = P["kt"].next()
            vt = P["vt"].next()
            S.dma("sp", kt[:, 0:nb * 128], kT_dram[:, c0 * 128:(c0 + nb) * 128], r=[ksrc], w=[kt])
            S.dma("sp", vt[:, 0:nb, :], v_dram[:, c0:c0 + nb, :], r=[vsrc], w=[vt])
            chunks[ci] = (kt, vt)

        ptiles = {}

        def stage_a(n):
            kb = kb0 + n
            ci, i = divmod(n, 16)
            if i == 0:
                load_chunk(ci)
                load_chunk(ci + 1)
            kt, vt = chunks[ci]
            psl = P["psl"].next()
            S.op("pe", "matmul", psl[:], lhsT=kt[:, i * 128:(i + 1) * 128], rhs=qT, start=True, stop=True,
                 r=[kt, qtok], w=[psl])
            e = P["e"].next()
            S.op("act", "activation", e[:], psl[:], AF.Exp, scale=scale, r=[psl], w=[e])
            if mask_fn is None:
                p = (keep if keep is not None else P["p"]).next()
                S.op("dve", "tensor_tensor", p[:], e[:], tab[:, tid_fn(kb), :], ALU.mult, r=[e, tab], w=[p])
            else:
                pa = P["pa"].next()
                S.op("dve", "tensor_tensor", pa[:], e[:], tab[:, tid_fn(kb), :], ALU.mult, r=[e, tab], w=[pa])
                map_, mtok = mask_fn(kb)
                p = P["p"].next()
                S.op("pool", "tensor_tensor", p[:].rearrange("p (h q) -> p h q", h=4),
                     pa[:].rearrange("p (h q) -> p h q", h=4),
                     map_.unsqueeze(1).broadcast_to([128, 4, 128]), ALU.mult, r=[pa, mtok], w=[p])
            ptiles[n] = (p, vt, i)

        def stage_b(n):
            p, vt, i = ptiles.pop(n)
            S.op("pe", "matmul", oT[:], lhsT=vt[:, i, :], rhs=p[:], start=(n == 0), stop=(n == nblk - 1),
                 r=[vt, p], w=[oT])
            S.op("pe", "matmul", den[:], lhsT=self.ones_bf[:], rhs=p[:], start=(n == 0), stop=(n == nblk - 1),
                 r=[p, self.ones_bf], w=[den])

        for n in range(nblk + LA):
            if n < nblk:
                stage_a(n)
            if n >= LA:
                stage_b(n - LA)
        return oT, den

    def attn_rings(self, pre):
        S = self.S
        return {
            "kt": S.ring(pre + "kt", [128, 2048], BF16, 3),
            "vt": S.ring(pre + "vt", [128, 16, 128], BF16, 3),
            "psl": S.psring(pre + "psl", [128, 512], F32, 3),
            "e": S.ring(pre + "e", [128, 512], BF16, 3),
            "pa": S.ring(pre + "pa", [128, 512], BF16, 3),
            "p": S.ring(pre + "p", [128, 512], BF16, 5),
            "oT": S.psring(pre + "oT", [128, 512], F32, 2),
            "den": S.psring(pre + "den", [128, 512], F32, 1),
        }

    def load_qT(self, qr, base, g, j):
        S = self.S
        qt = qr.next()
        S.dma("sp", qt[:], self.qT_all.t.ap()[base + 4 * g:base + 4 * g + 4, :, j * 128:(j + 1) * 128]
              .rearrange("h p q -> p h q"), r=[self.qT_all], w=[qt])
        return qt

    def store_y(self, y, hbase, g, j):
        S = self.S
        S.dma("sp", self.yT_all.t.ap()[hbase + 4 * g:hbase + 4 * g + 4, :, j * 128:(j + 1) * 128]
              .rearrange("h p q -> p h q"), y[:].rearrange("p (h q) -> p h q", h=4), r=[y], w=[self.yT_all])

    def dsa_attn(self):
        S = self.S
        nj = NJ if "small" not in self.dbg else 2
        with S.scope():
            P = self.attn_rings("da_")
            tab = S.sb("da_tab", [128, 21, 512], BF16)
            mT = S.ring("da_mT", [128, 128, 128], BF16, 1)
            qr = S.ring("da_q", [128, 4, 128], BF16, 2)
            rdr = S.ring("da_rd", [128, 512], F32, 2)
            yr = S.ring("da_y", [128, 512], BF16, 2)
            for g in range(4):
                self.load_tab("dsa", g, tab, 21)
                for j in range(nj):
                    nkb = 8 * j + 8
                    m = mT.next()
                    S.dma("pool", m[:, 0:nkb, :], self.maskT_d.t.ap()[j, :, 0:nkb, :], r=[self.maskT_d], w=[m])
                    qt = self.load_qT(qr, 0, g, j)
                    oT, den = self.attn(P, qt[:].rearrange("p h q -> p (h q)"), qt,
                                        self.kT_all.t.ap()[g], self.kT_all, self.v_all.t.ap()[0, g], self.v_all,
                                        0, nkb, tab, lambda kb, j=j: min(8 * j + 7 - kb, 20),
                                        lambda kb, m=m: (m[:, kb, :], m.tok))
                    rd = rdr.next()
                    S.op("dve", "tensor_scalar", rd[:], den[:], 1e-30, None, ALU.max, r=[den], w=[rd])
                    S.op("dve", "reciprocal", rd[:], rd[:], r=[rd], w=[rd])
                    y = yr.next()
                    S.op("dve", "tensor_tensor", y[:], oT[:], rd[:], ALU.mult, r=[oT, rd], w=[y])
                    self.store_y(y, 0, g, j)


    def gelu_tanh(self, out, x, n, tmp):
        S = self.S
        S.op("dve", "tensor_tensor", tmp[:, 0:n], x[:, 0:n], x[:, 0:n], ALU.mult, r=[x], w=[tmp])
        S.op("dve", "tensor_scalar", tmp[:, 0:n], tmp[:, 0:n], 0.044715, 1.0, ALU.mult, ALU.add, r=[tmp], w=[tmp])
        S.op("dve", "tensor_tensor", tmp[:, 0:n], tmp[:, 0:n], x[:, 0:n], ALU.mult, r=[tmp, x], w=[tmp])
        S.op("act", "activation", tmp[:, 0:n], tmp[:, 0:n], AF.Sigmoid, scale=1.5957691216057308, r=[tmp], w=[tmp])
        S.op("dve", "tensor_tensor", out[:, 0:n], tmp[:, 0:n], x[:, 0:n], ALU.mult, r=[tmp, x], w=[out])

    def nsa_compress(self):
        S = self.S
        self.kcT_d = S.dram("kcT_d", [4, 128, 1024], BF16)
        self.vc_d = S.dram("vc_d", [4, 128, 8, 128], BF16)
        with S.scope():
            w1 = S.sb("cp_w1", [128, 32, 256], BF16)
            w2 = S.sb("cp_w2", [128, 2, 128], BF16)
            peT = S.sb("cp_pe", [128, 32], BF16)
            pb = S.sb("cp_pb", [128, 2], F32)
            xr = S.ring("cp_x", [128, 8208], BF16, 2)
            hx = S.ring("cp_hx", [128, 512], F32, 2)
            tmpr = S.ring("cp_tmp", [128, 512], F32, 2)
            hid = [S.sb(f"cp_hid{i}", [128, 512], BF16) for i in range(2)]
            otr = S.ring("cp_ot", [128, 512], BF16, 2)
            sqr = S.ring("cp_sq", [128, 512], BF16, 1)
            rrr = S.ring("cp_rr", [128, 512], F32, 1)
            psr = S.psring("cp_ps", [128, 512], F32, 3)
            ps2r = S.psring("cp_ps2", [128, 512], F32, 1)
            psb = S.ps("cp_psb", [128, 2], F32)
            for kv in range(2):
                sfx = "k" if kv == 0 else "v"
                w1src = self.din("cmp_w1_" + sfx, [32, 128, 256])
                w2src = self.din("cmp_w2_" + sfx, [256, 128])
                pesrc = self.din("cmp_peT_" + sfx, [128, 32])
                S.dma("pool", w1[:], w1src.ap().rearrange("l d e -> d l e"), w=[w1])
                S.dma("pool", w2[:], w2src.ap().rearrange("(c e) d -> e c d", c=2), w=[w2])
                S.dma("pool", peT[:], pesrc.ap()[:, :], w=[peT])
                for ec in range(2):
                    for l in range(32):
                        S.op("pe", "matmul", psb[:, ec:ec + 1], lhsT=w1[:, l, ec * 128:(ec + 1) * 128],
                             rhs=peT[:, l:l + 1], start=(l == 0), stop=(l == 31), r=[w1, peT], w=[psb])
                S.op("dve", "tensor_copy", pb[:], psb[:], r=[psb], w=[pb])
                for g in range(4):
                    ch = (5 if kv == 0 else 9) + g
                    for nt in range(2):
                        n0 = nt * 512
                        nn = 512 if nt == 0 else 511
                        xt = xr.next()
                        S.dma("sp", xt[:, 0:16 * nn + 16], self.kT_all.t.ap()[ch, :, 16 * n0:16 * n0 + 16 * nn + 16],
                              r=[self.kT_all], w=[xt])
                        xv = xt[:, 0:8208].rearrange("p (n s) -> p n s", s=16)
                        for ec in range(2):
                            ps = psr.next()
                            for l in range(32):
                                S.op("pe", "matmul", ps[:, 0:nn], lhsT=w1[:, l, ec * 128:(ec + 1) * 128],
                                     rhs=xv[:, l // 16:l // 16 + nn, l % 16], start=(l == 0), stop=(l == 31),
                                     r=[w1, xt], w=[ps])
                            x32 = hx.next()
                            S.op("act", "activation", x32[:, 0:nn], ps[:, 0:nn], AF.Identity, bias=pb[:, ec:ec + 1],
                                 r=[ps, pb], w=[x32])
                            self.gelu_tanh(hid[ec], x32, nn, tmpr.next())
                        if kv == 0:
                            ps = psr.next()
                            for ec in range(2):
                                S.op("pe", "matmul", ps[:, 0:nn], lhsT=w2[:, ec, :], rhs=hid[ec][:, 0:nn],
                                     start=(ec == 0), stop=(ec == 1), r=[w2, hid[ec]], w=[ps])
                            ot = otr.next()
                            S.op("pool", "memset", ot[:], 0.0, w=[ot])
                            self.evac_T("n3", ps, ot, nn, (sqr.next(), ps2r.next(), rrr.next()))
                            S.dma("sp", self.kcT_d.t.ap()[g, :, n0:n0 + 512], ot[:], r=[ot], w=[self.kcT_d])
                        else:
                            ot = otr.next()
                            S.op("pool", "memset", ot[:], 0.0, w=[ot])
                            ps = psr.next()
                            for nb in range(4):
                                m = min(128, nn - nb * 128)
                                for ec in range(2):
                                    S.op("pe", "matmul", ps[0:m, nb * 128:(nb + 1) * 128],
                                         lhsT=hid[ec][:, nb * 128:nb * 128 + m], rhs=w2[:, ec, :],
                                         start=(ec == 0), stop=(ec == 1), r=[w2, hid[ec]], w=[ps])
                            for nb in range(4):
                                m = min(128, nn - nb * 128)
                                S.op("act", "activation", ot[0:m, nb * 128:(nb + 1) * 128],
                                     ps[0:m, nb * 128:(nb + 1) * 128], AF.Identity, r=[ps], w=[ot])
                            S.dma("sp", self.vc_d.t.ap()[g, :, nt * 4:(nt + 1) * 4, :],
                                  ot[:].rearrange("p (b d) -> p b d", b=4), r=[ot], w=[self.vc_d])
            if "cmp" in self.dbg:
                o = self.dout("d_kcT", [4, 128, 1024], BF16)
                S.dma("sp", o.ap()[:, :, :], self.kcT_d.t.ap()[:, :, :], r=[self.kcT_d], w=[self.outs["d_kcT"][1]])
                o = self.dout("d_vc", [4, 128, 8, 128], BF16)
                S.dma("sp", o.ap()[:, :, :, :], self.vc_d.t.ap()[:, :, :, :], r=[self.vc_d], w=[self.outs["d_vc"][1]])

    def nsa_attn(self):
        S = self.S
        nj = NJ if "small" not in self.dbg else 2
        with S.scope():
            P = self.attn_rings("na_")
            keep = S.ring("na_keep", [128, 512], BF16, 9)
            tab_s = S.sb("na_tabs", [128, 21, 512], BF16)
            tab_w = S.sb("na_tabw", [128, 12, 512], BF16)
            tab_c = S.sb("na_tabc", [128, 5, 512], BF16)
            qr = S.ring("na_q", [128, 4, 128], BF16, 2)
            ftab = S.sb("na_ftab", [128, NJ, 256], F32)
            ov = S.sb("na_ov", [128, 8, 256], BF16)
            yexp = S.sb("na_yexp", [128, 8192], BF16)
            S.dma("sp", ftab[:], self.din("ftab", [NJ, 128, 256]).ap().rearrange("j p m -> p j m"), w=[ftab])
            S.dma("pool", ov[:], self.din("ov", [1024, 256]).ap().rearrange("(c p) m -> p c m", p=128), w=[ov])
            S.dma("pool", yexp[:], self.din("yexp", [128, 8192]).ap()[:, :], w=[yexp])
            rdr = S.ring("na_rd", [128, 512], F32, 2)
            Rr = S.ring("na_R", [128, 512], F32, 2)
            yacc = S.sb("na_yacc", [128, 512], F32)
            ytmp = S.sb("na_ytmp", [128, 512], F32)
            yr = S.ring("na_y", [128, 512], BF16, 2)
            pnr = S.ring("na_pn", [128, 512], BF16, 2)
            rg = S.ring("na_rg", [128, 512], BF16, 2)
            imp = S.sb("na_imp", [128, 256], F32)
            imp3 = S.sb("na_imp3", [128, 256], F32)
            m8 = S.sb("na_m8", [128, 16], F32)
            mb = S.sb("na_mb", [128, 256], BF16)
            mbT = S.sb("na_mbT", [128, 2, 128], BF16)
            mkr = S.ring("na_mk", [128, 128], BF16, 3)
            psm = S.psring("na_psm", [128, 512], F32, 1)
            pst = S.ps("na_pst", [128, 1024], BF16)

            def finish_branch(br, oT, den, g, j):
                r = rg.next()
                for h in range(4):
                    col = (4 * g + h) * 3 + br
                    S.op("dve", "tensor_scalar", r[:, h * 128:(h + 1) * 128], self.ident[:],
                         self.gbs[:, j, col:col + 1], None, ALU.mult, r=[self.ident, self.gbs], w=[r])
                pg = psm.next()
                S.op("pe", "matmul", pg[:], lhsT=self.ones_bf[:], rhs=r[:], start=True, stop=True,
                     r=[r, self.ones_bf], w=[pg])
                rd = rdr.next()
                S.op("dve", "tensor_scalar", rd[:], den[:], 1e-30, None, ALU.max, r=[den], w=[rd])
                S.op("dve", "reciprocal", rd[:], rd[:], r=[rd], w=[rd])
                R = Rr.next()
                S.op("dve", "tensor_tensor", R[:], rd[:], pg[:], ALU.mult, r=[rd, pg], w=[R])
                if br == 0:
                    S.op("dve", "tensor_tensor", yacc[:], oT[:], R[:], ALU.mult, r=[oT, R], w=[yacc])
                else:
                    S.op("dve", "tensor_tensor", ytmp[:], oT[:], R[:], ALU.mult, r=[oT, R], w=[ytmp])
                    S.op("dve", "tensor_tensor", yacc[:], yacc[:], ytmp[:], ALU.add, r=[yacc, ytmp], w=[yacc])
                return rd

            for g in range(4):
                self.load_tab("slc", g, tab_s, 21)
                self.load_tab("win", g, tab_w, 12)
                self.load_tab("cmp", g, tab_c, 5)
                for j in range(nj):
                    qt = self.load_qT(qr, 32, g, j)
                    qv = qt[:].rearrange("p h q -> p (h q)")
                    ncc = j // 2 + 1
                    keep.i = 0
                    oT, den = self.attn(P, qv, qt, self.kcT_d.t.ap()[g], self.kcT_d, self.vc_d.t.ap()[g], self.vc_d,
                                        0, ncc, tab_c, lambda kb, j=j: min(j - 2 * kb, 4), None, keep=keep)
                    rd = finish_branch(0, oT, den, g, j)
                    pi = psm.next()
                    for cc in range(ncc):
                        pn = pnr.next()
                        S.op("dve", "tensor_tensor", pn[:], keep.bufs[cc][:], rd[:], ALU.mult,
                             r=[keep.bufs[cc], rd], w=[pn])
                        for h in range(4):
                            S.op("pe", "matmul", pi[:, 0:256], lhsT=pn[:, h * 128:(h + 1) * 128], rhs=ov[:, cc, :],
                                 start=(cc == 0 and h == 0), stop=(cc == ncc - 1 and h == 3), r=[pn, ov], w=[pi])
                    S.op("dve", "tensor_tensor", imp[:], pi[:, 0:256], ftab[:, j, :], ALU.add, r=[pi, ftab], w=[imp])
                    S.op("dve", "max", m8[:, 0:8], imp[:], r=[imp], w=[m8])
                    S.op("dve", "match_replace", imp3[:], m8[:, 0:8], imp[:], -3.0e38, r=[m8, imp], w=[imp3])
                    S.op("dve", "max", m8[:, 8:16], imp3[:], r=[imp3], w=[m8])
                    S.op("dve", "tensor_scalar", mb[:], imp[:], m8[:, 15:16], None, ALU.is_ge, r=[imp, m8], w=[mb])
                    for hh in range(2):
                        S.op("pe", "transpose", pst[:, hh * 128:(hh + 1) * 128], mb[:, hh * 128:(hh + 1) * 128],
                             self.ident[:], r=[mb, self.ident], w=[pst])
                    S.op("act", "activation", mbT[:].rearrange("p a b -> p (a b)"), pst[:, 0:256], AF.Identity,
                         r=[pst], w=[mbT])

                    def slc_mask(kb):
                        pm = psm.next()
                        S.op("pe", "matmul", pm[:, 0:128], lhsT=yexp[:, (kb % 64) * 128:(kb % 64 + 1) * 128],
                             rhs=mbT[:, kb // 64, :], start=True, stop=True, r=[yexp, mbT], w=[pm])
                        mk = mkr.next()
                        S.op("act", "activation", mk[:], pm[:, 0:128], AF.Identity, r=[pm], w=[mk])
                        return mk[:], mk.tok

                    nkb = 8 * j + 8
                    oT, den = self.attn(P, qv, qt, self.kT_all.t.ap()[13 + g], self.kT_all,
                                        self.v_all.t.ap()[1, g], self.v_all, 0, nkb, tab_s,
                                        lambda kb, j=j: min(8 * j + 7 - kb, 20), slc_mask)
                    finish_branch(1, oT, den, g, j)
                    kb0 = max(0, 8 * j - 4)
                    oT, den = self.attn(P, qv, qt, self.kT_all.t.ap()[17 + g], self.kT_all,
                                        self.v_all.t.ap()[2, g], self.v_all, kb0, nkb, tab_w,
                                        lambda kb, j=j: kb - (8 * j - 4), None)
                    finish_branch(2, oT, den, g, j)
                    y = yr.next()
                    S.op("act", "activation", y[:], yacc[:], AF.Identity, r=[yacc], w=[y])
                    self.store_y(y, 16, g, j)


    def row_bcast(self, dst, col0):
        S = self.S
        with S.scope():
            tl = S.ring("rb_l", [128, 128], F32, 2)
            ps = S.psring("rb_ps", [128, 512], F32, 2)
            onesf = S.sb("rb_ones", [128, 128], F32)
            S.op("dve", "memset", onesf[:], 1.0, w=[onesf])
            for k4 in range(8):
                p = ps.next()
                for i in range(4):
                    kc = k4 * 4 + i
                    t = tl.next()
                    S.op("dve", "tensor_scalar", t[:], onesf[:], self.modc[:, col0 + kc:col0 + kc + 1], None, ALU.mult,
                         r=[onesf, self.modc], w=[t])
                    S.op("pe", "matmul", p[:, i * 128:(i + 1) * 128], lhsT=t[:], rhs=self.identf[:], start=True,
                         stop=True, r=[t, self.identf], w=[p])
                S.op("act", "activation", dst[:, k4 * 512:(k4 + 1) * 512], p[:], AF.Identity, r=[p], w=[dst])

    def merge(self):
        S = self.S
        self.x1_d = S.dram("x1_d", [NOWN, D], F32)
        self.mT_d = S.dram("mT_d", [32, 128, NOWN], BF16)
        self.hT2_own = S.dram("hT2_own", [4, 128, KC, 512], BF16)
        wa_src = self.din("w_branch_a", [2048, D]).ap().rearrange("(h p) n -> p h n", p=128)
        wb_src = self.din("w_branch_b", [2048, D]).ap().rearrange("(h p) n -> p h n", p=128)
        ya = self.yT_all.t.ap()
        qTa = self.qT_all.t.ap()
        with S.scope():
            wa = S.sb("mg_wa", [128, 16, 1024], BF16)
            wb = S.sb("mg_wb", [128, 16, 1024], BF16)
            yar = S.ring("mg_ya", [128, 16, 512], BF16, 2)
            ybr = S.ring("mg_yb", [128, 16, 512], BF16, 2)
            gr = S.ring("mg_g", [128, 2, 512], BF16, 3)
            t1r = S.ring("mg_t1", [128, 512], F32, 2)
            t2r = S.ring("mg_t2", [128, 512], F32, 2)
            mr = S.ring("mg_m", [128, 512], BF16, 3)
            psr = S.psring("mg_ps", [128, 512], F32, 4)
            for ccg in range(4):
                for h in range(16):
                    S.dma("pool", wa[:, h, :], wa_src[:, h, ccg * 1024:(ccg + 1) * 1024], w=[wa])
                    S.dma("pool", wb[:, h, :], wb_src[:, h, ccg * 1024:(ccg + 1) * 1024], w=[wb])
                for tt in range(4):
                    yat = yar.next()
                    ybt = ybr.next()
                    S.dma("sp", yat[:], ya[0:16, :, tt * 512:(tt + 1) * 512].rearrange("h p q -> p h q"),
                          r=[self.yT_all], w=[yat])
                    S.dma("sp", ybt[:], ya[16:32, :, tt * 512:(tt + 1) * 512].rearrange("h p q -> p h q"),
                          r=[self.yT_all], w=[ybt])
                    for ci in range(8):
                        cc = ccg * 8 + ci
                        gt = gr.next()
                        S.dma("sp", gt[:, 0, :], qTa[48 + cc, :, tt * 512:(tt + 1) * 512], r=[self.qT_all], w=[gt])
                        S.dma("sp", gt[:, 1, :], qTa[80 + cc, :, tt * 512:(tt + 1) * 512], r=[self.qT_all], w=[gt])
                        pA = psr.next()
                        for h in range(16):
                            S.op("pe", "matmul", pA[:], lhsT=wa[:, h, ci * 128:(ci + 1) * 128], rhs=yat[:, h, :],
                                 start=(h == 0), stop=(h == 15), r=[wa, yat], w=[pA])
                        pB = psr.next()
                        for h in range(16):
                            S.op("pe", "matmul", pB[:], lhsT=wb[:, h, ci * 128:(ci + 1) * 128], rhs=ybt[:, h, :],
                                 start=(h == 0), stop=(h == 15), r=[wb, ybt], w=[pB])
                        t1 = t1r.next()
                        t2 = t2r.next()
                        S.op("dve", "tensor_tensor", t1[:], pA[:], gt[:, 0, :], ALU.mult, r=[pA, gt], w=[t1])
                        S.op("dve", "tensor_tensor", t2[:], pB[:], gt[:, 1, :], ALU.mult, r=[pB, gt], w=[t2])
                        m = mr.next()
                        S.op("pool", "tensor_tensor", m[:], t1[:], t2[:], ALU.add, r=[t1, t2], w=[m])
                        S.dma("sp", self.mT_d.t.ap()[cc, :, tt * 512:(tt + 1) * 512], m[:], r=[m], w=[self.mT_d])
        with S.scope():
            gtB = S.sb("mo_gtB", [128, D], F32)
            self.row_bcast(gtB, 64)
            wo_src = self.din("w_out", [D, D]).ap().rearrange("(kc p) n -> p kc n", p=128)
            mt = S.sb("mo_mt", [128, KC, 512], BF16)
            wo = S.sb("mo_wo", [128, KC, 512], BF16)
            xts = [S.sb(f"mo_x{i}", [128, D], F32) for i in range(4)]
            tmpr = S.ring("mo_tmp", [128, 512], F32, 2)
            psr = S.psring("mo_ps", [128, 512], F32, 3)
            with S.scope():
                pass
            for tg in range(4):
                S.dma("sp", mt[:], self.mT_d.t.ap()[:, :, tg * 512:(tg + 1) * 512].rearrange("c p q -> p c q"),
                      r=[self.mT_d], w=[mt])
                for tb in range(4):
                    r0 = (tg * 4 + tb) * 128
                    S.dma("sp", xts[tb][:], self.i_xown.ap()[r0:r0 + 128, :], w=[xts[tb]])
                for ct in range(8):
                    for kc in range(KC):
                        S.dma("pool", wo[:, kc, :], wo_src[:, kc, ct * 512:(ct + 1) * 512], w=[wo])
                    for tb in range(4):
                        ps = psr.next()
                        for kc in range(KC):
                            S.op("pe", "matmul", ps[:], lhsT=mt[:, kc, tb * 128:(tb + 1) * 128], rhs=wo[:, kc, :],
                                 start=(kc == 0), stop=(kc == KC - 1), r=[mt, wo], w=[ps])
                        tmp = tmpr.next()
                        sl = slice(ct * 512, (ct + 1) * 512)
                        S.op("dve", "tensor_tensor", tmp[:], ps[:], gtB[:, sl], ALU.mult, r=[ps, gtB], w=[tmp])
                        S.op("pool", "tensor_tensor", xts[tb][:, sl], xts[tb][:, sl], tmp[:], ALU.add,
                             r=[xts[tb], tmp], w=[xts[tb]])
                for tb in range(4):
                    r0 = (tg * 4 + tb) * 128
                    S.dma("sp", self.x1_d.t.ap()[r0:r0 + 128, :], xts[tb][:], r=[xts[tb]], w=[self.x1_d])
        self.phase1(self.x1_d.t, self.hT2_own, NJ, AB_off=64, src_tok=self.x1_d)


    def peer(self):
        S = self.S
        NE = 16384
        self.uT_d = S.dram("uT_d", [128, 128, KC, 128], BF16)
        self.qpT_d = S.dram("qpT_d", [16, 128, NOWN], BF16)
        u_src = self.din("peer_u", [NE, D]).ap()
        v_src = self.din("peer_v", [NE, D]).ap()
        with S.scope():
            ur = S.ring("pu_u", [128, D], BF16, 2)
            utr = S.ring("pu_ut", [128, KC, 128], BF16, 2)
            psr = S.psring("pu_ps", [128, 1024], BF16, 3)
            nec = 128
            for ec in range(nec):
                ut = ur.next()
                S.dma("pool", ut[:], u_src[ec * 128:(ec + 1) * 128, :], w=[ut])
                utt = utr.next()
                for k4 in range(4):
                    pt = psr.next()
                    for i in range(8):
                        kc = k4 * 8 + i
                        S.op("pe", "transpose", pt[:, i * 128:(i + 1) * 128], ut[:, kc * 128:(kc + 1) * 128],
                             self.ident[:], r=[ut, self.ident], w=[pt])
                    dst = utt[:, k4 * 8:(k4 + 1) * 8, :].rearrange("p a b -> p (a b)")
                    if k4 % 2 == 0:
                        S.op("act", "activation", dst, pt[:], AF.Identity, r=[pt], w=[utt])
                    else:
                        S.op("dve", "tensor_copy", dst, pt[:], r=[pt], w=[utt])
                S.dma("sp", self.uT_d.t.ap()[ec], utt[:], r=[utt], w=[self.uT_d])
        with S.scope():
            hts = [S.sb(f"pq_h{i}", [128, KC, 512], BF16) for i in range(2)]
            wr = S.ring("pq_w", [128, KC, 512], BF16, 2)
            psr = S.psring("pq_ps", [128, 512], F32, 3)
            otr = S.ring("pq_ot", [128, 512], BF16, 3)
            for half in range(2):
                for i in range(2):
                    S.dma("sp", hts[i][:], self.hT2_own.t.ap()[half * 2 + i], r=[self.hT2_own], w=[hts[i]])
                for gq in range(4):
                    wt = wr.next()
                    self.load_w(self.din("w_peer_q", [D, 2048]) if "w_peer_q" not in self.inp else self.inp["w_peer_q"],
                                gq * 512, 512, wt)
                    for i in range(2):
                        tt = half * 2 + i
                        for ci in range(4):
                            ch = gq * 4 + ci
                            ps = psr.next()
                            for kc in range(KC):
                                S.op("pe", "matmul", ps[:], lhsT=wt[:, kc, ci * 128:(ci + 1) * 128],
                                     rhs=hts[i][:, kc, :], start=(kc == 0), stop=(kc == KC - 1),
                                     r=[wt, hts[i]], w=[ps])
                            ot = otr.next()
                            S.op("act", "activation", ot[:], ps[:], AF.Identity, r=[ps], w=[ot])
                            S.dma("sp", self.qpT_d.t.ap()[ch, :, tt * 512:(tt + 1) * 512], ot[:], r=[ot],
                                  w=[self.qpT_d])
        with S.scope():
            keysT = S.sb("pe_keys", [128, 16, 128], BF16)
            S.dma("pool", keysT[:], self.din("peer_keysT", [16, 128, 128]).ap().rearrange("c d n -> d c n"),
                  w=[keysT])
            ngrp = 8 if "small" not in self.dbg else 1
            nec = 128
            for tg in range(ngrp):
                with S.scope():
                    actT = S.sb("pe_actT", [128, 128, 256], BF16)
                    with S.scope():
                        W = [S.sb(f"pe_W{i}", [128, NE], BF16) for i in range(2)]
                        with S.scope():
                            qp = S.sb("pe_qp", [128, 16, 256], BF16)
                            S.dma("sp", qp[:], self.qpT_d.t.ap()[:, :, tg * 256:(tg + 1) * 256]
                                  .rearrange("c p q -> p c q"), r=[self.qpT_d], w=[qp])
                            ssb = S.sb("pe_s", [128, 16, 128], F32)
                            pss = S.psring("pe_pss", [128, 512], F32, 2)
                            S4 = S.ring("pe_S4", [128, 1024], F32, 3)
                            E4 = S.ring("pe_E4", [128, 1024], BF16, 5)
                            sm = {k: S.sb("pe_" + k, [128, n], F32) for k, n in
                                  (("t1", 16), ("t2", 16), ("sr", 128), ("cand", 256), ("cr", 256), ("tc", 16),
                                   ("e16", 16), ("den", 1), ("nthr", 1), ("bias", 1), ("thr", 1))}
                            for tb in range(2):
                                for c4 in range(4):
                                    ps = pss.next()
                                    for i in range(4):
                                        ch = c4 * 4 + i
                                        S.op("pe", "matmul", ps[:, i * 128:(i + 1) * 128],
                                             lhsT=qp[:, ch, tb * 128:(tb + 1) * 128], rhs=keysT[:, ch, :],
                                             start=True, stop=True, r=[qp, keysT], w=[ps])
                                    S.op("act", "activation", ssb[:, c4 * 4:(c4 + 1) * 4, :].rearrange("p a b -> p (a b)"),
                                         ps[:], AF.Identity, r=[ps], w=[ssb])
                                for h in range(8):
                                    s1 = ssb[:, 2 * h, :]
                                    s2 = ssb[:, 2 * h + 1, :]
                                    for (sx, tx) in ((s1, sm["t1"]), (s2, sm["t2"])):
                                        S.op("dve", "max", tx[:, 0:8], sx, r=[ssb], w=[tx])
                                        S.op("dve", "match_replace", sm["sr"][:], tx[:, 0:8], sx, -3.0e38,
                                             r=[ssb, tx], w=[sm["sr"]])
                                        S.op("dve", "max", tx[:, 8:16], sm["sr"][:], r=[sm["sr"]], w=[tx])
                                    cand = sm["cand"]
                                    S.op("dve", "tensor_tensor", cand[:].rearrange("p (a b) -> p a b", a=16),
                                         sm["t1"][:].unsqueeze(2).broadcast_to([128, 16, 16]),
                                         sm["t2"][:].unsqueeze(1).broadcast_to([128, 16, 16]), ALU.add,
                                         r=[sm["t1"], sm["t2"]], w=[cand])
                                    tc_ = sm["tc"]
                                    S.op("dve", "max", tc_[:, 0:8], cand[:], r=[cand], w=[tc_])
                                    S.op("dve", "match_replace", sm["cr"][:], tc_[:, 0:8], cand[:], -3.0e38,
                                         r=[cand, tc_], w=[sm["cr"]])
                                    S.op("dve", "max", tc_[:, 8:16], sm["cr"][:], r=[sm["cr"]], w=[tc_])
                                    thr, nthr, den, bias = sm["thr"], sm["nthr"], sm["den"], sm["bias"]
                                    S.op("dve", "tensor_copy", thr[:], tc_[:, 15:16], r=[tc_], w=[thr])
                                    S.op("dve", "tensor_scalar", nthr[:], tc_[:, 15:16], -1.0, None, ALU.mult,
                                         r=[tc_], w=[nthr])
                                    S.op("act", "activation", sm["e16"][:], tc_[:], AF.Exp, bias=nthr[:, 0:1],
                                         accum_out=den[:], r=[tc_, nthr], w=[sm["e16"], den])
                                    S.op("act", "activation", den[:], den[:], AF.Ln, r=[den], w=[den])
                                    S.op("dve", "tensor_tensor", bias[:], nthr[:], den[:], ALU.subtract,
                                         r=[nthr, den], w=[bias])
                                    pend = {}

                                    def gate_a(q8, h=h, tb=tb, s1=s1, s2=s2, bias=bias, thr=thr, pend=pend):
                                        i0 = q8 * 8
                                        s4 = S4.next()
                                        S.op("pool", "tensor_tensor", s4[:].rearrange("p (a b) -> p a b", a=8),
                                             s1[:, i0:i0 + 8].unsqueeze(2).broadcast_to([128, 8, 128]),
                                             s2.unsqueeze(1).broadcast_to([128, 8, 128]), ALU.add,
                                             r=[ssb], w=[s4])
                                        e4 = E4.next()
                                        S.op("act", "activation", e4[:], s4[:], AF.Exp, bias=bias[:, 0:1],
                                             r=[s4, bias], w=[e4])
                                        wsl = W[tb][:, q8 * 1024:(q8 + 1) * 1024]
                                        if h == 0:
                                            S.op("dve", "scalar_tensor_tensor", wsl, s4[:], thr[:, 0:1], e4[:],
                                                 ALU.is_ge, ALU.mult, r=[s4, thr, e4], w=[W[tb]])
                                        else:
                                            S.op("dve", "scalar_tensor_tensor", e4[:], s4[:], thr[:, 0:1], e4[:],
                                                 ALU.is_ge, ALU.mult, r=[s4, thr, e4], w=[e4])
                                            pend[q8] = e4

                                    def gate_b(q8, tb=tb, pend=pend):
                                        e4 = pend.pop(q8)
                                        wsl = W[tb][:, q8 * 1024:(q8 + 1) * 1024]
                                        S.op("pool", "tensor_tensor", wsl, wsl, e4[:], ALU.add,
                                             r=[W[tb], e4], w=[W[tb]])

                                    for n in range(16 + 2):
                                        if n < 16:
                                            gate_a(n)
                                        if n >= 2 and h > 0:
                                            gate_b(n - 2)
                        with S.scope():
                            h2 = S.sb("pe_h2", [128, KC, 256], BF16)
                            S.dma("sp", h2[:], self.hT2_own.t.ap()[tg // 2, :, :, (tg % 2) * 256:(tg % 2 + 1) * 256],
                                  r=[self.hT2_own], w=[h2])
                            uTr = S.ring("pe_uT", [128, KC, 128], BF16, 2)
                            psa = S.psring("pe_psa", [128, 256], F32, 2)
                            pst = S.psring("pe_pst", [128, 256], BF16, 2)
                            xg = S.ring("pe_xg", [128, 256], F32, 2)
                            tg_ = S.ring("pe_tg", [128, 256], F32, 2)
                            gl = S.ring("pe_gl", [128, 256], F32, 2)
                            for ec in range(nec):
                                uT = uTr.next()
                                S.dma("sp" if ec % 2 == 0 else "pool", uT[:], self.uT_d.t.ap()[ec],
                                      r=[self.uT_d], w=[uT])
                                pa = psa.next()
                                for kc in range(KC):
                                    S.op("pe", "matmul", pa[:], lhsT=uT[:, kc, :], rhs=h2[:, kc, :],
                                         start=(kc == 0), stop=(kc == KC - 1), r=[uT, h2], w=[pa])
                                pt = pst.next()
                                for tb in range(2):
                                    S.op("pe", "transpose", pt[:, tb * 128:(tb + 1) * 128],
                                         W[tb][:, ec * 128:(ec + 1) * 128], self.ident[:],
                                         r=[W[tb], self.ident], w=[pt])
                                x = xg.next()
                                S.op("act", "activation", x[:], pa[:], AF.Identity, r=[pa], w=[x])
                                g_ = gl.next()
                                self.gelu_tanh(g_, x, 256, tg_.next())
                                S.op("dve", "tensor_tensor", actT[:, ec, :], g_[:], pt[:], ALU.mult,
                                     r=[g_, pt], w=[actT])
                    with S.scope():
                        gtB = S.sb("pe_gtB", [128, D], F32)
                        self.row_bcast(gtB, 160)
                        vr = S.ring("pe_v", [128, 1024], BF16, 3)
                        x1 = [S.sb(f"pe_x1{i}", [128, D], F32) for i in range(2)]
                        pso = [S.ps(f"pe_pso{i}", [128, 512], F32) for i in range(4)]
                        tmpr = S.ring("pe_tmp", [128, 512], F32, 2)
                        for tb in range(2):
                            r0 = tg * 256 + tb * 128
                            S.dma("sp", x1[tb][:], self.x1_d.t.ap()[r0:r0 + 128, :], r=[self.x1_d], w=[x1[tb]])
                        for dq in range(4):
                            for ec in range(nec):
                                vt = vr.next()
                                S.dma("pool", vt[:], v_src[ec * 128:(ec + 1) * 128, dq * 1024:(dq + 1) * 1024], w=[vt])
                                for tb in range(2):
                                    for d2 in range(2):
                                        S.op("pe", "matmul", pso[tb * 2 + d2][:],
                                             lhsT=actT[:, ec, tb * 128:(tb + 1) * 128],
                                             rhs=vt[:, d2 * 512:(d2 + 1) * 512], start=(ec == 0), stop=(ec == nec - 1),
                                             r=[actT, vt], w=[pso[tb * 2 + d2]])
                            for tb in range(2):
                                for d2 in range(2):
                                    sl = slice(dq * 1024 + d2 * 512, dq * 1024 + (d2 + 1) * 512)
                                    tmp = tmpr.next()
                                    S.op("dve", "tensor_tensor", tmp[:], pso[tb * 2 + d2][:], gtB[:, sl], ALU.mult,
                                         r=[pso[tb * 2 + d2], gtB], w=[tmp])
                                    S.op("pool", "tensor_tensor", x1[tb][:, sl], x1[tb][:, sl], tmp[:], ALU.add,
                                         r=[x1[tb], tmp], w=[x1[tb]])
                        out = self.outs["out"]
                        for tb in range(2):
                            r0 = tg * 256 + tb * 128
                            S.dma("sp", out[0].ap()[r0:r0 + 128, :], x1[tb][:], r=[x1[tb]], w=[out[1]])


def rel_bucket_np(d):
    n = np.maximum(d, 0)
    nf = np.maximum(n, 1).astype(np.float32)
    lb = 16 + (np.log(nf / np.float32(16)) / np.float32(np.log(2048 / 16)) * np.float32(16)).astype(np.int32)
    return np.where(n < 16, n, np.minimum(lb, 31))


def _bias_tile(rel_bias, heads0, dist, valid):
    bk = rel_bucket_np(dist)
    vals = rel_bias[bk][:, :, heads0:heads0 + 16]
    vals = np.where(valid[:, :, None], vals, np.float32(-30000.0))
    return np.ascontiguousarray(vals.reshape(128, 128, 4, 4).transpose(2, 0, 3, 1).reshape(4, 128, 512)).astype(np.float32)


def make_tables(rel_bias, c):
    k = np.arange(128)[:, None]
    q = np.arange(128)[None, :]
    out = {}
    for name, h0 in (("dsa", 0), ("slc", 16)):
        tiles = []
        for op in range(21):
            dist = 128 * (op + c - 7) + q - k
            tiles.append(_bias_tile(rel_bias, h0, dist, dist >= 0))
        out["tab_" + name] = np.stack(tiles)
    tiles = []
    for w in range(12):
        dist = 128 * (c + 4 - w) + q - k
        tiles.append(_bias_tile(rel_bias, 16, dist, (dist >= 0) & (dist < 512)))
    out["tab_win"] = np.stack(tiles)
    tiles = []
    for e8 in range(5):
        e = 8 * e8 if e8 < 4 else 64
        dist = 128 * (e + c) + q - (16 * k + 31)
        tiles.append(_bias_tile(rel_bias, 16, dist, dist >= 0))
    out["tab_cmp"] = np.stack(tiles)
    ft = np.zeros((NJ, 128, 256), np.float32)
    mm = np.arange(256)[None, :]
    for j in range(NJ):
        tq = 128 * (8 * j + c) + np.arange(128)[:, None]
        cur = tq // 64
        forced = (mm == 0) | (mm == cur) | (mm == cur - 1)
        adm = mm * 64 <= tq
        ft[j] = np.where(adm, np.where(forced, 1e9, 0.0), -1e30)
    out["ftab"] = ft
    kk = np.arange(1024)[None, :]
    qq = np.arange(128)[:, None]
    out["cmask"] = np.where(kk <= 128 * c + qq, 0.0, -1e30).astype(np.float32)
    return out

def make_consts():
    n = np.arange(1024)[:, None]
    m = np.arange(256)[None, :]
    ov = ((16 * n < 64 * m + 64) & (16 * n + 32 > 64 * m) & (n < 1023)).astype(np.float32)
    cidx = np.arange(8192)[None, :]
    yexp = (np.arange(128)[:, None] == cidx // 64).astype(np.float32)
    return {"ov": ov, "yexp": yexp}


IN_SPLITS = (2048, 512, 512, 2048, 64, 32, 2048, 3072, 48, 8192)


def prep_inputs(inp):
    offs = np.concatenate([[0], np.cumsum(IN_SPLITS)])
    w_in = inp["w_in"][0]
    seg = lambda i: w_in[:, offs[i]:offs[i + 1]]
    qa, ka, va, qi, ki, wi, qb, kvb, gb, gm = [seg(i) for i in range(10)]
    kv = lambda i: kvb[:, i * 512:(i + 1) * 512]
    w_kT = np.ascontiguousarray(np.concatenate([ka, ki, ki, kv(0), kv(1), kv(2), kv(4)], axis=1))
    w_v = np.ascontiguousarray(np.concatenate([va, kv(3), kv(5)], axis=1))
    w_qT = np.ascontiguousarray(np.concatenate([qa, qi, qb, gm], axis=1))
    w_qs = np.ascontiguousarray(np.concatenate([wi, gb], axis=1))
    colT = lambda v: np.ascontiguousarray(v.reshape(-1, 128).T)
    x = inp["x"][0]
    shared = {
        "xfull": x,
        "cT": colT(inp["c"][0]),
        "w_ada": inp["w_ada"][0],
        "b_adaT": colT(inp["b_ada"][0]),
        "gnT": np.ascontiguousarray(np.concatenate([colT(inp["g_mix"][0]), colT(inp["g_ffn"][0])], axis=1)),
        "gains": np.ascontiguousarray(np.stack([inp[k][0] for k in
                                                ("gq_a", "gk_a", "gq_b", "gk_cmp", "gk_slc", "gk_win")], axis=1)),
        "w_kT": w_kT, "w_v": w_v, "w_qT": w_qT, "w_qs": w_qs,
        "cmp_w1_k": inp["cmp_w1_k"][0], "cmp_w1_v": inp["cmp_w1_v"][0],
        "cmp_w2_k": inp["cmp_w2_k"][0], "cmp_w2_v": inp["cmp_w2_v"][0],
        "cmp_peT_k": np.ascontiguousarray(inp["cmp_pe_k"][0].T), "cmp_peT_v": np.ascontiguousarray(inp["cmp_pe_v"][0].T),
        "w_branch_a": inp["w_branch_a"][0], "w_branch_b": inp["w_branch_b"][0], "w_out": inp["w_out"][0],
        "w_peer_q": inp["w_peer_q"][0],
        "peer_keysT": np.ascontiguousarray(inp["peer_sub_keys"][0].reshape(16, 128, 128).transpose(0, 2, 1)),
        "peer_u": inp["peer_u"][0], "peer_v": inp["peer_v"][0],
    }
    shared.update(make_consts())
    maps = []
    xb = x.reshape(NTB, 128, D)
    for c in range(NCORES):
        m = dict(shared)
        m["xown"] = np.ascontiguousarray(xb[c::8].reshape(NOWN, D))
        m.update(make_tables(inp["rel_bias"], c))
        maps.append(m)
    return maps


_CACHE = {}


def kernel(**inputs):
    inputs = {k: np.asarray(v) for k, v in inputs.items()}
    if "prog" not in _CACHE:
        p = Prog()
        p.build()
        _CACHE["prog"] = p
    p = _CACHE["prog"]
    maps = prep_inputs(inputs)
    maps = [{k: np.ascontiguousarray(v, dtype=np.float32) for k, v in m.items() if k in p.inp} for m in maps]
    res = run_bass_kernel_spmd(p.nc, maps, core_ids=list(range(NCORES)))
    out = np.zeros((1, T, D), np.float32)
    ob = out[0].reshape(NTB, 128, D)
    for c in range(NCORES):
        ob[c::8] = res.results[c]["out"].reshape(NJ, 128, D)
    return out
```

```python
import contextlib
import numpy as np
import ml_dtypes
import concourse.bass as bass
import concourse.mybir as mybir
from concourse.bass_utils import run_bass_kernel_spmd

F32 = mybir.dt.float32
BF16 = mybir.dt.bfloat16
AF = mybir.ActivationFunctionType
ALU = mybir.AluOpType
AX = mybir.AxisListType

NCORES = 8
T = 16384
D = 4096
NTB = T // 128
NOWN = 2048
NJ = 16
EPS = 1e-6
KC = 32


class Tok:
    __slots__ = ("lw", "rd", "dsem", "dcnt", "name")

    def __init__(self, name=""):
        self.lw = None
        self.rd = {}
        self.dsem = None
        self.dcnt = 0
        self.name = name


class Buf:
    def __init__(self, t, tok):
        self.t = t
        self.tok = tok

    def __getitem__(self, k):
        return self.t[k]


class Ring:
    def __init__(self, bufs):
        self.bufs = bufs
        self.i = 0

    def next(self):
        b = self.bufs[self.i % len(self.bufs)]
        self.i += 1
        return b


class Sched:
    def __init__(self, nc):
        self.nc = nc
        self.E = {"pe": nc.tensor, "act": nc.scalar, "dve": nc.vector, "pool": nc.gpsimd, "sp": nc.sync}
        self.sem = {k: nc.semaphore("se_" + k).__enter__() for k in self.E}
        self.cnt = {k: 0 for k in self.E}
        self.seen = {k: {} for k in self.E}
        self.dsems = []
        self.free_dsems = []
        self.dsem_cnt = {}
        self.scope_toks = [[]]
        self.ninst = 0
        self.stack = None

    def scope(self):
        return _Scope(self)

    def _enter(self, cm):
        return self.stack.enter_context(cm)

    def _nm(self, name):
        self.nuid = getattr(self, "nuid", 0) + 1
        return f"{name}_{self.nuid}"

    def sb(self, name, shape, dtype):
        return Buf(self._enter(self.nc.sbuf_tensor(self._nm(name), list(shape), dtype)), Tok(name))

    def ps(self, name, shape, dtype):
        return Buf(self._enter(self.nc.psum_tensor(self._nm(name), list(shape), dtype)), Tok(name))

    def dram(self, name, shape, dtype):
        return Buf(self.nc.dram_tensor(name, list(shape), dtype, kind="Internal"), Tok(name))

    def ring(self, name, shape, dtype, n):
        return Ring([self.sb(f"{name}{i}", shape, dtype) for i in range(n)])

    def psring(self, name, shape, dtype, n):
        return Ring([self.ps(f"{name}{i}", shape, dtype) for i in range(n)])

    @staticmethod
    def _toks(xs):
        return [x.tok if isinstance(x, Buf) else x for x in xs]

    @staticmethod
    def _deps(r, w):
        deps = []
        for t in r:
            if t.lw is not None:
                deps.append(t.lw)
        for t in w:
            if t.lw is not None:
                deps.append(t.lw)
            deps.extend(t.rd.values())
        return deps

    def _wait(self, eng, deps, skip_own=False):
        seen = self.seen[eng]
        own = self.sem[eng]
        for sem, val in deps:
            if skip_own and sem is own:
                continue
            k = id(sem)
            if seen.get(k, 0) < val:
                self.E[eng].wait_ge(sem, val)
                seen[k] = val

    @staticmethod
    def _commit(me, r, w):
        k = id(me[0])
        for t in r:
            t.rd[k] = me
        for t in w:
            t.lw = me
            t.rd = {}

    def op(self, eng, meth, *args, r=(), w=(), **kw):
        r = self._toks(r)
        w = self._toks(w)
        self._wait(eng, self._deps(r, w), skip_own=(eng == "pe"))
        inst = getattr(self.E[eng], meth)(*args, **kw)
        self.cnt[eng] += 1
        inst.then_inc(self.sem[eng], 1)
        self._commit((self.sem[eng], self.cnt[eng]), r, w)
        self.ninst += 1
        return inst

    def dma(self, q, out, in_, r=(), w=(), **kw):
        r = self._toks(r)
        w = self._toks(w)
        self._wait(q, self._deps(r, w))
        inst = self.E[q].dma_start(out=out, in_=in_, **kw)
        t = w[0]
        if t.dsem is None:
            if self.free_dsems:
                t.dsem, t.dcnt = self.free_dsems.pop()
            else:
                t.dsem = self.nc.semaphore(f"sd{len(self.dsems)}").__enter__()
                t.dcnt = 0
                self.dsems.append(t.dsem)
            self.scope_toks[-1].append(t)
        t.dcnt += 16
        inst.then_inc(t.dsem, 16)
        self.dsem_cnt[id(t.dsem)] = (t.dsem, t.dcnt)
        self._commit((t.dsem, t.dcnt), r, w)
        self.ninst += 1
        return inst

    def barrier(self):
        deps = [(self.sem[k], self.cnt[k]) for k in self.E if self.cnt[k] > 0]
        deps += list(self.dsem_cnt.values())
        for eng in self.E:
            self._wait(eng, deps)

    def release_scope_sems(self, toks):
        for t in toks:
            if t.dsem is not None:
                self.free_dsems.append((t.dsem, t.dcnt))
                t.dsem = None
                t.dcnt = 0


class _Scope:
    def __init__(self, S):
        self.S = S

    def __enter__(self):
        self.prev = self.S.stack
        self.es = contextlib.ExitStack()
        self.es.__enter__()
        self.S.stack = self.es
        self.S.scope_toks.append([])
        return self

    def __exit__(self, *a):
        self.S.barrier()
        self.S.release_scope_sems(self.S.scope_toks.pop())
        self.S.stack = self.prev
        return self.es.__exit__(*a)


KCH = (["n1"] * 4) + ["c"] + (["c"] * 8) + (["n4"] * 4) + (["n5"] * 4)
NKCH = len(KCH)
QCH = (["n0"] * 16) + (["c"] * 16) + (["n2"] * 16) + (["s"] * 64)
NQCH = len(QCH)


class Prog:
    def __init__(self, stop="all", dbg=()):
        self.stop = stop
        self.dbg = set(dbg)
        nc = bass.Bass("TRN2", target_bir_lowering=False)
        self.nc = nc
        self.S = Sched(nc)
        self.inp = {}
        self.outs = {}

    def din(self, name, shape, dtype=F32):
        t = self.nc.dram_tensor(name, list(shape), dtype, kind="ExternalInput")
        self.inp[name] = t
        return t

    INSHAPES = {
        "xfull": [T, D], "xown": [NOWN, D], "cT": [128, KC], "w_ada": [D, 6 * D], "b_adaT": [128, 192],
        "gnT": [128, 64], "gains": [128, 6], "w_kT": [D, 21 * 128], "w_v": [D, 1536],
        "w_qT": [D, 112 * 128], "w_qs": [D, 80],
    }

    def __getattr__(self, name):
        if name.startswith("i_") and name[2:] in self.INSHAPES:
            nm = name[2:]
            if nm not in self.inp:
                shp = list(self.INSHAPES[nm])
                if "small" in self.dbg and nm == "xfull":
                    shp[0] = 1024
                if "small" in self.dbg and nm == "w_qT":
                    shp[1] = 1024
                self.din(nm, shp)
            return self.inp[nm]
        raise AttributeError(name)

    def dout(self, name, shape, dtype=F32):
        t = self.nc.dram_tensor(name, list(shape), dtype, kind="ExternalOutput")
        self.outs[name] = (t, Tok(name))
        return t

    def build(self):
        nc, S = self.nc, self.S
        self.hT_all = S.dram("hT_all", [32, 128, KC, 512], BF16)
        self.hT_own = S.dram("hT_own", [4, 128, KC, 512], BF16)
        self.kT_all = S.dram("kT_all", [NKCH, 128, T], BF16)
        self.v_all = S.dram("v_all", [3, 4, 128, NTB, 128], BF16)
        self.qT_all = S.dram("qT_all", [NQCH, 128, NOWN], BF16)

        with S.scope():
            self.consts()
            with S.scope():
                if "nomod" in self.dbg:
                    self.din("modc_in", [128, 192])
                    S.dma("sp", self.modc[:], self.inp["modc_in"].ap()[:, :], w=[self.modc])
                    self.mod_to_AB()
                else:
                    self.phase0()
            if self.stop == "p0":
                return self.finish()
            if "inj" in self.dbg:
                self.inject()
                self.attention_and_rest()
                return self.finish()
            with S.scope():
                self.phase1(self.i_xfull, self.hT_all, NTB if "small" not in self.dbg else 8)
                self.phase1(self.i_xown, self.hT_own, NJ)
            if self.stop == "p1":
                return self.finish()
            with S.scope():
                self.proj_kv()
            with S.scope():
                self.proj_q()
            if self.stop == "p2":
                return self.finish()
            self.attention_and_rest()
            return self.finish()

    def inject(self):
        S = self.S
        self.kT_all = Buf(self.din("kT_in", [NKCH, 128, T], BF16), Tok("kT_in"))
        self.v_all = Buf(self.din("v_in", [3, 4, 128, NTB, 128], BF16), Tok("v_in"))
        self.qT_all = Buf(self.din("qT_in", [NQCH, 128, NOWN], BF16), Tok("qT_in"))
        S.dma("sp", self.wis[:], self.din("wis_in", [128, NJ, 32]).ap()[:, :, :], w=[self.wis])
        S.dma("sp", self.gbs[:], self.din("gbs_in", [128, NJ, 48]).ap()[:, :, :], w=[self.gbs])

    def attention_and_rest(self):
        S = self.S
        self.yT_all = S.dram("yT_all", [32, 128, NOWN], BF16)
        if "yinj" in self.dbg:
            self.yT_all = Buf(self.din("yT_in", [32, 128, NOWN], BF16), Tok("yT_in"))
        else:
            self.prep_tables()
        cum = "cum" in self.dbg
        lvl = ["dsa", "nsa", "merge", "all"].index(self.stop) if (cum and self.stop in ("dsa", "nsa", "merge", "all")) else -1
        if self.stop in ("dsa", "all") or lvl >= 0:
            self.dsa_index()
            self.dsa_attn()
        if self.stop in ("nsa", "all") or lvl >= 1:
            self.nsa_compress()
            self.nsa_attn()
        if self.stop in ("merge", "mp", "all") or lvl >= 2:
            self.merge()
        if self.stop == "peer" and "inj" in self.dbg:
            self.x1_d = Buf(self.din("x1_in", [NOWN, D]), Tok("x1_in"))
            self.hT2_own = S.dram("hT2_own", [4, 128, KC, 512], BF16)
            self.phase1(self.x1_d.t, self.hT2_own, NJ, AB_off=64, src_tok=self.x1_d)
        if self.stop in ("peer", "mp", "all"):
            self.dout("out", [NOWN, D])
            self.peer()
        if "y" in self.dbg:
            o = self.dout("d_yT", [32, 128, 256], BF16)
            S.dma("sp", o.ap()[:, :, :], self.yT_all.t.ap()[:, :, 0:256], r=[self.yT_all],
                  w=[self.outs["d_yT"][1]])

    def finish(self):
        S = self.S
        toks = [tok for (_, tok) in self.outs.values()]
        deps = [t.lw for t in toks if t.lw is not None]
        S._wait("sp", deps)
        S.barrier()
        return self.nc

    def consts(self):
        S = self.S
        self.identf = S.sb("identf", [128, 128], F32)
        self.ident = S.sb("ident", [128, 128], BF16)
        self.ones_bf = S.sb("ones_bf", [128, 128], BF16)
        self.modc = S.sb("modc", [128, 192], F32)
        self.AB = S.sb("AB", [128, 128], F32)
        self.gn = S.sb("gn", [128, 64], F32)
        self.gcol = S.sb("gcol", [128, 6], F32)
        S.op("pool", "memset", self.identf[:], 1.0, w=[self.identf])
        S.op("pool", "affine_select", self.identf[:], self.identf[:], pattern=[[-1, 128]],
             compare_op=ALU.is_equal, fill=0.0, base=0, channel_multiplier=1,
             r=[self.identf], w=[self.identf])
        S.op("dve", "tensor_copy", self.ident[:], self.identf[:], r=[self.identf], w=[self.ident])
        S.op("dve", "memset", self.ones_bf[:], 1.0, w=[self.ones_bf])
        self.wis = S.sb("wis", [128, NJ, 32], F32)
        self.gbs = S.sb("gbs", [128, NJ, 48], F32)
        self.eps128 = S.sb("eps128", [128, 1], F32)
        S.op("dve", "memset", self.eps128[:], 128.0 * EPS, w=[self.eps128])
        S.dma("sp", self.gn[:], self.i_gnT.ap()[:, :], w=[self.gn])
        S.dma("sp", self.gcol[:], self.i_gains.ap()[:, :], w=[self.gcol])
        S.op("dve", "tensor_scalar", self.gcol[:], self.gcol[:], float(np.sqrt(128.0)), None, ALU.mult,
             r=[self.gcol], w=[self.gcol])

    def phase0(self):
        S = self.S
        cT = S.sb("cTs", [128, KC], F32)
        bT = S.sb("bTs", [128, 192], F32)
        S.dma("sp", cT[:], self.i_cT.ap()[:, :], w=[cT])
        S.dma("sp", bT[:], self.i_b_adaT.ap()[:, :], w=[bT])
        ps = S.ps("ps_mod", [128, 192], F32)
        prow = S.psring("ps_row", [1, 512], F32, 2)
        rows = S.ring("modrow", [1, 512], F32, 2)
        one = S.sb("one11", [1, 1], F32)
        S.op("dve", "memset", one[:], 1.0, w=[one])
        wr = S.ring("wada", [128, KC, 512], F32, 2)
        wa = self.i_w_ada.ap().rearrange("(kc p) n -> p kc n", p=128)
        qs = ["sp", "pool"]
        for ct in range(48):
            wt = wr.next()
            for hh in range(2):
                S.dma(qs[hh], wt[:, hh * 16:(hh + 1) * 16, :], wa[:, hh * 16:(hh + 1) * 16, ct * 512:(ct + 1) * 512],
                      w=[wt])
            pr = prow.next()
            for kc in range(KC):
                S.op("pe", "matmul", pr[:], lhsT=cT[:, kc:kc + 1], rhs=wt[:, kc, :],
                     start=(kc == 0), stop=(kc == KC - 1), r=[wt, cT], w=[pr])
            row = rows.next()
            S.op("act", "activation", row[:], pr[:], AF.Identity, r=[pr], w=[row])
            for i in range(4):
                jc = ct * 4 + i
                S.op("pe", "matmul", ps[:, jc:jc + 1], lhsT=row[0:1, i * 128:(i + 1) * 128], rhs=one[0:1, 0:1],
                     start=True, stop=True, r=[row, one], w=[ps])
        S.op("dve", "tensor_tensor", self.modc[:], ps[:], bT[:], ALU.add, r=[ps, bT], w=[self.modc])
        self.mod_to_AB()
        if "modc" in self.dbg:
            o = self.dout("d_modc", [128, 192])
            S.dma("sp", o.ap()[:, :], self.modc[:], r=[self.modc], w=[self.outs["d_modc"][1]])

    def mod_to_AB(self):
        S = self.S
        m, AB, gn = self.modc, self.AB, self.gn
        S.op("dve", "scalar_tensor_tensor", AB[:, 0:32], m[:, 32:64], 1.0, gn[:, 0:32], ALU.add, ALU.mult,
             r=[m, gn], w=[AB])
        S.op("dve", "tensor_copy", AB[:, 32:64], m[:, 0:32], r=[m], w=[AB])
        S.op("dve", "scalar_tensor_tensor", AB[:, 64:96], m[:, 128:160], 1.0, gn[:, 32:64], ALU.add, ALU.mult,
             r=[m, gn], w=[AB])
        S.op("dve", "tensor_copy", AB[:, 96:128], m[:, 96:128], r=[m], w=[AB])

    def norm_hT(self, xt, AB_off, hT, col0, ps_ring, xn_ring, junk, small):
        S = self.S
        ss, rstd = small
        S.op("act", "activation", junk[:], xt[:], AF.Square, accum_out=ss[:], r=[xt], w=[junk, ss])
        S.op("dve", "tensor_scalar", rstd[:], ss[:], 1.0 / D, EPS, ALU.mult, ALU.add, r=[ss], w=[rstd])
        S.op("act", "activation", rstd[:], rstd[:], AF.Sqrt, r=[rstd], w=[rstd])
        S.op("dve", "reciprocal", rstd[:], rstd[:], r=[rstd], w=[rstd])
        xn = xn_ring.next()
        S.op("dve", "tensor_scalar", xn[:], xt[:], rstd[:, 0:1], None, ALU.mult, r=[xt, rstd], w=[xn])
        for k4 in range(KC // 8):
            pt = ps_ring.next()
            for i in range(8):
                kc = k4 * 8 + i
                S.op("pe", "transpose", pt[:, i * 128:(i + 1) * 128], xn[:, kc * 128:(kc + 1) * 128],
                     self.ident[:], r=[xn, self.ident], w=[pt])
            for i in range(8):
                kc = k4 * 8 + i
                A = self.AB[:, AB_off + kc:AB_off + kc + 1]
                B = self.AB[:, AB_off + 32 + kc:AB_off + 32 + kc + 1]
                if i % 2 == 0:
                    S.op("act", "activation", hT[:, kc, col0:col0 + 128], pt[:, i * 128:(i + 1) * 128],
                         AF.Identity, bias=B, scale=A, r=[pt, self.AB], w=[hT])
                else:
                    S.op("dve", "tensor_scalar", hT[:, kc, col0:col0 + 128], pt[:, i * 128:(i + 1) * 128],
                         A, B, ALU.mult, ALU.add, r=[pt, self.AB], w=[hT])

    def phase1(self, xsrc, hdst, nblk, AB_off=0, src_tok=None):
        S = self.S
        with S.scope():
            xr = S.ring("p1x", [128, D], F32, 2)
            xnr = S.ring("p1xn", [128, D], BF16, 2)
            junk = S.sb("p1junk", [128, D], BF16)
            hr = S.ring("p1h", [128, KC, 512], BF16, 2)
            psr = S.psring("p1ps", [128, 1024], BF16, 3)
            smalls = [(S.sb(f"p1ss{i}", [128, 1], F32), S.sb(f"p1rs{i}", [128, 1], F32)) for i in range(2)]
            xa = xsrc.ap()
            for tb in range(nblk):
                if tb % 4 == 0:
                    hT = hr.next()
                xt = xr.next()
                S.dma("sp", xt[:], xa[tb * 128:(tb + 1) * 128, :], r=([src_tok] if src_tok is not None else []),
                      w=[xt])
                self.norm_hT(xt, AB_off, hT, (tb % 4) * 128, psr, xnr, junk, smalls[tb % 2])
                if tb % 4 == 3:
                    S.dma("pool", hdst.t.ap()[tb // 4], hT[:], r=[hT], w=[hdst])

    def load_w(self, wsrc, col0, ncols, wt, q="pool"):
        S = self.S
        wa = wsrc.ap()
        for kc in range(KC):
            S.dma(q, wt[:, kc, 0:ncols], wa[kc * 128:(kc + 1) * 128, col0:col0 + ncols], w=[wt])

    def evac_T(self, kind, ps, ot, n, tmp):
        S = self.S
        if kind == "c":
            S.op("act", "activation", ot[:, 0:n], ps[:, 0:n], AF.Identity, r=[ps], w=[ot])
        elif kind == "s":
            S.op("act", "activation", ot[:, 0:n], ps[:, 0:n], AF.Sigmoid, r=[ps], w=[ot])
        else:
            gi = int(kind[1:])
            sq, ps2, rr = tmp
            S.op("act", "activation", sq[:, 0:n], ps[:, 0:n], AF.Square, r=[ps], w=[sq])
            S.op("pe", "matmul", ps2[:, 0:n], lhsT=self.ones_bf[:], rhs=sq[:, 0:n], start=True, stop=True,
                 r=[sq, self.ones_bf], w=[ps2])
            S.op("act", "activation", rr[:, 0:n], ps2[:, 0:n], AF.Sqrt, bias=self.eps128[:, 0:1],
                 r=[ps2, self.eps128], w=[rr])
            S.op("dve", "reciprocal", rr[:, 0:n], rr[:, 0:n], r=[rr], w=[rr])
            S.op("dve", "scalar_tensor_tensor", ot[:, 0:n], ps[:, 0:n], self.gcol[:, gi:gi + 1], rr[:, 0:n],
                 ALU.mult, ALU.mult, r=[ps, self.gcol, rr], w=[ot])

    def proj_kv(self):
        S = self.S
        ntile = 32 if "small" not in self.dbg else 2
        hr = S.ring("kvh", [128, KC, 512], BF16, 2)
        wk = S.sb("kvwk", [128, KC, 7 * 128], BF16)
        wv = S.sb("kvwv", [128, KC, 512], BF16)
        psr = S.psring("kvps", [128, 512], F32, 4)
        ps2r = S.psring("kvps2", [128, 512], F32, 2)
        otr = S.ring("kvot", [128, 512], BF16, 4)
        sqr = S.ring("kvsq", [128, 512], BF16, 2)
        rrr = S.ring("kvrr", [128, 512], F32, 2)
        kTa = self.kT_all.t.ap()
        va = self.v_all.t.ap()
        for p in range(3):
            self.load_w(self.i_w_kT, p * 7 * 128, 7 * 128, wk)
            self.load_w(self.i_w_v, p * 512, 512, wv)
            for tt in range(ntile):
                hT = hr.next()
                S.dma("sp", hT[:], self.hT_all.t.ap()[tt], r=[self.hT_all], w=[hT])
                for ci in range(7):
                    ch = p * 7 + ci
                    ps = psr.next()
                    for kc in range(KC):
                        S.op("pe", "matmul", ps[:], lhsT=wk[:, kc, ci * 128:(ci + 1) * 128], rhs=hT[:, kc, :],
                             start=(kc == 0), stop=(kc == KC - 1), r=[wk, hT], w=[ps])
                    ot = otr.next()
                    self.evac_T(KCH[ch], ps, ot, 512, (sqr.next(), ps2r.next(), rrr.next()))
                    S.dma("sp", kTa[ch, :, tt * 512:(tt + 1) * 512], ot[:], r=[ot], w=[self.kT_all])
                for tb in range(4):
                    ps = psr.next()
                    for kc in range(KC):
                        S.op("pe", "matmul", ps[:], lhsT=hT[:, kc, tb * 128:(tb + 1) * 128], rhs=wv[:, kc, :],
                             start=(kc == 0), stop=(kc == KC - 1), r=[wv, hT], w=[ps])
                    ot = otr.next()
                    S.op("dve", "tensor_copy", ot[:], ps[:], r=[ps], w=[ot])
                    blk = tt * 4 + tb
                    S.dma("sp", va[p, :, :, blk, :].rearrange("g p d -> p g d"),
                          ot[:].rearrange("p (g d) -> p g d", g=4), r=[ot], w=[self.v_all])
        if "kv" in self.dbg:
            o = self.dout("d_kT", [NKCH, 128, 1024], BF16)
            S.dma("sp", o.ap()[:, :, :], kTa[:, :, 0:1024], r=[self.kT_all], w=[self.outs["d_kT"][1]])
            o = self.dout("d_v", [3, 4, 128, 8, 128], BF16)
            for i3 in range(3):
                S.dma("sp", o.ap()[i3], va[i3, :, :, 0:8, :], r=[self.v_all], w=[self.outs["d_v"][1]])

    def proj_q(self):
        S = self.S
        hT4 = S.sb("qh", [128, 4, KC, 512], BF16) if False else None
        hts = [S.sb(f"qh{i}", [128, KC, 512], BF16) for i in range(2)]
        G = 4
        wr = S.ring("qw", [128, KC, G * 128], BF16, 2)
        psr = S.psring("qps", [128, 512], F32, 4)
        ps2r = S.psring("qps2", [128, 512], F32, 2)
        otr = S.ring("qot", [128, 512], BF16, 4)
        sqr = S.ring("qsq", [128, 512], BF16, 2)
        rrr = S.ring("qrr", [128, 512], F32, 2)
        qTa = self.qT_all.t.ap()
        ngrp = NQCH // G if "small" not in self.dbg else 2
        wqs = S.sb("qwqs", [128, KC, 80], BF16)
        self.load_w(self.i_w_qs, 0, 80, wqs)
        for half in range(2):
            for i in range(2):
                S.dma("sp", hts[i][:], self.hT_own.t.ap()[half * 2 + i], r=[self.hT_own], w=[hts[i]])
            for i in range(2):
                for tb in range(4):
                    if "noqs" in self.dbg:
                        continue
                    j = (half * 2 + i) * 4 + tb
                    ps = psr.next()
                    for kc in range(KC):
                        S.op("pe", "matmul", ps[:, 0:80], lhsT=hts[i][:, kc, tb * 128:(tb + 1) * 128],
                             rhs=wqs[:, kc, :], start=(kc == 0), stop=(kc == KC - 1), r=[wqs, hts[i]], w=[ps])
                    S.op("act", "activation", self.wis[:, j, :], ps[:, 0:32], AF.Identity,
                         scale=float(1.0 / (8.0 * np.sqrt(32.0))), r=[ps], w=[self.wis])
                    S.op("act", "activation", self.gbs[:, j, :], ps[:, 32:80], AF.Sigmoid, r=[ps], w=[self.gbs])
            for g in range(ngrp):
                wt = wr.next()
                self.load_w(self.i_w_qT, g * G * 128, G * 128, wt)
                for i in range(2):
                    tt = half * 2 + i
                    for ci in range(G):
                        ch = g * G + ci
                        ps = psr.next()
                        for kc in range(KC):
                            S.op("pe", "matmul", ps[:], lhsT=wt[:, kc, ci * 128:(ci + 1) * 128],
                                 rhs=hts[i][:, kc, :], start=(kc == 0), stop=(kc == KC - 1),
                                 r=[wt, hts[i]], w=[ps])
                        ot = otr.next()
                        self.evac_T(QCH[ch], ps, ot, 512, (sqr.next(), ps2r.next(), rrr.next()))
                        S.dma("sp", qTa[ch, :, tt * 512:(tt + 1) * 512], ot[:], r=[ot], w=[self.qT_all])
        if "q" in self.dbg:
            o = self.dout("d_qT", [NQCH, 128, 512], BF16)
            S.dma("sp", o.ap()[:, :, :], qTa[:, :, 0:512], r=[self.qT_all], w=[self.outs["d_qT"][1]])


    def prep_tables(self):
        S = self.S
        self.tabs = {}
        with S.scope():
            raw = S.ring("tbraw", [128, 4, 512], F32, 2)
            et = S.ring("tbe", [128, 4, 512], BF16, 2)
            for name, nt in (("dsa", 21), ("slc", 21), ("win", 12), ("cmp", 5)):
                src = self.din("tab_" + name, [nt, 4, 128, 512])
                dst = S.dram("tabe_" + name, [nt, 4, 128, 512], BF16)
                self.tabs[name] = dst
                for t in range(nt):
                    r = raw.next()
                    S.dma("sp", r[:], src.ap()[t].rearrange("g p n -> p g n"), w=[r])
                    e = et.next()
                    S.op("act", "activation", e[:], r[:], AF.Exp, r=[r], w=[e])
                    S.dma("pool", dst.t.ap()[t].rearrange("g p n -> p g n"), e[:], r=[e], w=[dst])

    def load_tab(self, name, g, tab, nt):
        S = self.S
        src = self.tabs[name]
        S.dma("pool", tab[:, 0:nt, :], src.t.ap()[:, g].rearrange("o p n -> p o n"), r=[src], w=[tab])

    def dsa_index(self):
        S = self.S
        nj = NJ if "small" not in self.dbg else 2
        self.maskT_d = S.dram("maskT_d", [NJ, 128, 128, 128], BF16)
        with S.scope():
            score = S.sb("ix_score", [128, T], F32)
            junk = S.sb("ix_junk", [128, 4096], BF16)
            mst = S.sb("ix_mst", [128, 128, 128], BF16)
            qi = S.ring("ix_qi", [128, 16, 128], BF16, 2)
            dh = S.ring("ix_dh", [128, 32, 128], BF16, 2)
            kir = S.ring("ix_ki", [128, 2048], BF16, 2)
            rl = S.ring("ix_rl", [128, 512], BF16, 6)
            cm = S.sb("ix_cm", [128, 1024], F32)
            jf = S.sb("ix_jf", [128, 1024], F32)
            half = S.sb("ix_half", [128, 1], F32)
            sm = {k: S.sb("ix_" + k, [128, 1], F32) for k in ("lo", "hi", "mid", "cnt", "pred", "d1", "d2", "t1", "t2")}
            ps_s = S.psring("ix_pss", [128, 512], F32, 4)
            ps_c = S.psring("ix_psc", [128, 512], F32, 2)
            ps_t = S.psring("ix_pst", [128, 1024], BF16, 2)
            S.dma("sp", cm[:], self.din("cmask", [128, 1024]).ap()[:, :], w=[cm])
            S.op("dve", "memset", half[:], 0.5, w=[half])
            qTa = self.qT_all.t.ap()
            kia = self.kT_all.t.ap()[4]
            for j in range(nj):
                nkb = 8 * j + 8
                Tk = nkb * 128
                q = qi.next()
                S.dma("sp", q[:], qTa[16:32, :, j * 128:(j + 1) * 128].rearrange("c p q -> p c q"),
                      r=[self.qT_all], w=[q])
                d = dh.next()
                for h in range(32):
                    S.op("act", "activation", d[:, h, :], self.ident[:], AF.Identity,
                         scale=self.wis[:, j, h:h + 1], r=[self.ident, self.wis], w=[d])
                items = [(kt, h) for kt in range(nkb // 4) for h in range(32)]
                kis = {}
                pcs = {}
                rls = {}

                def ix_a(kt, h):
                    if h == 0 and kt % 4 == 0:
                        ki = kir.next()
                        n = min(2048, Tk - kt * 512)
                        S.dma("sp", ki[:, 0:n], kia[:, kt * 512:kt * 512 + n], r=[self.kT_all], w=[ki])
                        kis[kt // 4] = ki
                    ki = kis[kt // 4]
                    ko = (kt % 4) * 512
                    pb = (h % 2) * 64
                    ps = ps_s.next()
                    S.op("pe", "matmul", ps[:], lhsT=q[pb:pb + 64, h // 2, :], rhs=ki[pb:pb + 64, ko:ko + 512],
                         start=True, stop=True, r=[q, ki], w=[ps])
                    r = rl.next()
                    S.op("act", "activation", r[:], ps[:], AF.Relu, r=[ps], w=[r])
                    rls[(kt, h)] = r

                def ix_b(kt, h):
                    if h == 0:
                        pcs[kt] = ps_c.next()
                    pc = pcs[kt]
                    r = rls.pop((kt, h))
                    S.op("pe", "matmul", pc[:], lhsT=d[:, h, :], rhs=r[:], start=(h == 0), stop=(h == 31),
                         r=[d, r], w=[pc])
                    if h == 31:
                        if kt >= nkb // 4 - 2:
                            co = (kt - (nkb // 4 - 2)) * 512
                            S.op("dve", "tensor_tensor", score[:, kt * 512:(kt + 1) * 512], pc[:], cm[:, co:co + 512],
                                 ALU.add, r=[pc, cm], w=[score])
                        else:
                            S.op("dve", "tensor_copy", score[:, kt * 512:(kt + 1) * 512], pc[:],
                                 r=[pc], w=[score])

                LAI = 3
                for n in range(len(items) + LAI):
                    if n < len(items):
                        ix_a(*items[n])
                    if n >= LAI:
                        ix_b(*items[n - LAI])
                lo, hi, mid, cnt, pred = sm["lo"], sm["hi"], sm["mid"], sm["cnt"], sm["pred"]
                d1, d2, t1, t2 = sm["d1"], sm["d2"], sm["t1"], sm["t2"]
                S.op("dve", "tensor_tensor", jf[:], score[:, Tk - 1024:Tk], cm[:], ALU.subtract,
                     r=[score, cm], w=[jf])
                S.op("dve", "tensor_reduce", t1[:], jf[:], AX.X, ALU.min, r=[jf], w=[t1])
                if Tk > 1024:
                    S.op("dve", "tensor_reduce", t2[:], score[:, 0:Tk - 1024], AX.X, ALU.min, r=[score], w=[t2])
                    S.op("dve", "tensor_tensor", lo[:], t1[:], t2[:], ALU.min, r=[t1, t2], w=[lo])
                else:
                    S.op("dve", "tensor_copy", lo[:], t1[:], r=[t1], w=[lo])
                S.op("dve", "tensor_reduce", hi[:], score[:, 0:Tk], AX.X, ALU.max, r=[score], w=[hi])
                S.op("dve", "tensor_scalar", hi[:], hi[:], 1.0, None, ALU.add, r=[hi], w=[hi])
                for it in range(26):
                    S.op("dve", "scalar_tensor_tensor", mid[:], lo[:], hi[:, 0:1], half[:], ALU.add, ALU.mult,
                         r=[lo, hi, half], w=[mid])
                    nch = (Tk + 4095) // 4096
                    for ci in range(nch):
                        c0 = ci * 4096
                        n = min(4096, Tk - c0)
                        init = 0.0 if ci == 0 else cnt[:, 0:1]
                        S.op("dve", "tensor_scalar", junk[:, 0:n], score[:, c0:c0 + n], mid[:, 0:1], init,
                             ALU.is_ge, ALU.add, accum_out=cnt[:], r=[score, mid, cnt], w=[junk, cnt])
                    S.op("dve", "tensor_scalar", pred[:], cnt[:], 255.5, None, ALU.is_ge, r=[cnt], w=[pred])
                    S.op("dve", "tensor_tensor", d1[:], mid[:], lo[:], ALU.subtract, r=[mid, lo], w=[d1])
                    S.op("dve", "tensor_tensor", d2[:], hi[:], mid[:], ALU.subtract, r=[mid, hi], w=[d2])
                    S.op("dve", "scalar_tensor_tensor", lo[:], d1[:], pred[:, 0:1], lo[:], ALU.mult, ALU.add,
                         r=[d1, pred, lo], w=[lo])
                    S.op("dve", "scalar_tensor_tensor", hi[:], d2[:], pred[:, 0:1], mid[:], ALU.mult, ALU.add,
                         r=[d2, pred, mid], w=[hi])
                for c8 in range(nkb // 8):
                    S.op("dve", "tensor_scalar", junk[:, 0:1024], score[:, c8 * 1024:(c8 + 1) * 1024], lo[:, 0:1],
                         -30000.0, ALU.is_lt, ALU.mult, r=[score, lo], w=[junk])
                    pt = ps_t.next()
                    for i in range(8):
                        S.op("pe", "transpose", pt[:, i * 128:(i + 1) * 128], junk[:, i * 128:(i + 1) * 128],
                             self.ident[:], r=[junk, self.ident], w=[pt])
                    S.op("act", "activation", mst[:, c8 * 8:(c8 + 1) * 8, :].rearrange("p a b -> p (a b)"), pt[:],
                         AF.Identity, r=[pt], w=[mst])
                S.dma("sp", self.maskT_d.t.ap()[j, :, 0:nkb, :], mst[:, 0:nkb, :], r=[mst], w=[self.maskT_d])
            if "ix" in self.dbg:
                o = self.dout("d_score", [128, 2048])
                S.dma("sp", o.ap()[:, :], score[:, 0:2048], r=[score], w=[self.outs["d_score"][1]])
                o = self.dout("d_lo", [128, 1])
                S.dma("sp", o.ap()[:, :], sm["lo"][:], r=[sm["lo"]], w=[self.outs["d_lo"][1]])

    def attn(self, P, qT, qtok, kT_dram, ksrc, v_dram, vsrc, kb0, kb1, tab, tid_fn, mask_fn, keep=None):
        S = self.S
        oT = P["oT"].next()
        den = P["den"].next()
        scale = float(128.0 ** -0.5)
        LA = 2
        nblk = kb1 - kb0
        chunks = {}

        def load_chunk(ci):
            c0 = kb0 + ci * 16
            if c0 >= kb1 or ci in chunks:
                return
            nb = min(16, kb1 - c0)
            kt = P["kt"].next()
            vt = P["vt"].next()
            S.dma("sp", kt[:, 0:nb * 128], kT_dram[:, c0 * 128:(c0 + nb) * 128], r=[ksrc], w=[kt])
            S.dma("sp", vt[:, 0:nb, :], v_dram[:, c0:c0 + nb, :], r=[vsrc], w=[vt])
            chunks[ci] = (kt, vt)

        ptiles = {}

        def stage_a(n):
            kb = kb0 + n
            ci, i = divmod(n, 16)
            if i == 0:
                load_chunk(ci)
                load_chunk(ci + 1)
            kt, vt = chunks[ci]
            psl = P["psl"].next()
            S.op("pe", "matmul", psl[:], lhsT=kt[:, i * 128:(i + 1) * 128], rhs=qT, start=True,
                 stop=(mask_fn is None), r=[kt, qtok], w=[psl])
            if mask_fn is not None:
                mlhs, mrhs, mtoks = mask_fn(kb)
                S.op("pe", "matmul", psl[:].rearrange("p (h q) -> p h q", h=4), lhsT=mlhs,
                     rhs=mrhs.unsqueeze(1).broadcast_to([128, 4, 128]), start=False, stop=True,
                     r=mtoks, w=[psl])
            e = P["e"].next()
            S.op("act", "activation", e[:], psl[:], AF.Exp, scale=scale, r=[psl], w=[e])
            p = (keep if keep is not None else P["p"]).next()
            S.op("dve", "tensor_tensor", p[:], e[:], tab[:, tid_fn(kb), :], ALU.mult, r=[e, tab], w=[p])
            ptiles[n] = (p, vt, i)

        def stage_b(n):
            p, vt, i = ptiles.pop(n)
            S.op("pe", "matmul", oT[:], lhsT=vt[:, i, :], rhs=p[:], start=(n == 0), stop=(n == nblk - 1),
                 r=[vt, p], w=[oT])
            S.op("pe", "matmul", den[:], lhsT=self.ones_bf[:], rhs=p[:], start=(n == 0), stop=(n == nblk - 1),
                 r=[p, self.ones_bf], w=[den])

        for n in range(nblk + LA):
            if n < nblk:
                stage_a(n)
            if n >= LA:
                stage_b(n - LA)
        return oT, den

    def attn_rings(self, pre):
        S = self.S
        return {
            "kt": S.ring(pre + "kt", [128, 2048], BF16, 3),
            "vt": S.ring(pre + "vt", [128, 16, 128], BF16, 3),
            "psl": S.psring(pre + "psl", [128, 512], F32, 3),
            "e": S.ring(pre + "e", [128, 512], BF16, 3),
            "p": S.ring(pre + "p", [128, 512], BF16, 5),
            "oT": S.psring(pre + "oT", [128, 512], F32, 2),
            "den": S.psring(pre + "den", [128, 512], F32, 1),
        }

    def load_qT(self, qr, base, g, j):
        S = self.S
        qt = qr.next()
        S.dma("sp", qt[:], self.qT_all.t.ap()[base + 4 * g:base + 4 * g + 4, :, j * 128:(j + 1) * 128]
              .rearrange("h p q -> p h q"), r=[self.qT_all], w=[qt])
        return qt

    def store_y(self, y, hbase, g, j):
        S = self.S
        S.dma("sp", self.yT_all.t.ap()[hbase + 4 * g:hbase + 4 * g + 4, :, j * 128:(j + 1) * 128]
              .rearrange("h p q -> p h q"), y[:].rearrange("p (h q) -> p h q", h=4), r=[y], w=[self.yT_all])

    def dsa_attn(self):
        S = self.S
        nj = NJ if "small" not in self.dbg else 2
        with S.scope():
            P = self.attn_rings("da_")
            tab = S.sb("da_tab", [128, 21, 512], BF16)
            mT = S.ring("da_mT", [128, 128, 128], BF16, 1)
            qr = S.ring("da_q", [128, 4, 128], BF16, 2)
            rdr = S.ring("da_rd", [128, 512], F32, 2)
            yr = S.ring("da_y", [128, 512], BF16, 2)
            for g in range(4):
                self.load_tab("dsa", g, tab, 21)
                for j in range(nj):
                    nkb = 8 * j + 8
                    m = mT.next()
                    S.dma("pool", m[:, 0:nkb, :], self.maskT_d.t.ap()[j, :, 0:nkb, :], r=[self.maskT_d], w=[m])
                    qt = self.load_qT(qr, 0, g, j)
                    oT, den = self.attn(P, qt[:].rearrange("p h q -> p (h q)"), qt,
                                        self.kT_all.t.ap()[g], self.kT_all, self.v_all.t.ap()[0, g], self.v_all,
                                        0, nkb, tab, lambda kb, j=j: min(8 * j + 7 - kb, 20),
                                        lambda kb, m=m: (self.ident[:], m[:, kb, :], [m, self.ident]))
                    rd = rdr.next()
                    S.op("dve", "tensor_scalar", rd[:], den[:], 1e-30, None, ALU.max, r=[den], w=[rd])
                    S.op("dve", "reciprocal", rd[:], rd[:], r=[rd], w=[rd])
                    y = yr.next()
                    S.op("dve", "tensor_tensor", y[:], oT[:], rd[:], ALU.mult, r=[oT, rd], w=[y])
                    self.store_y(y, 0, g, j)


    def gelu_tanh(self, out, x, n, tmp):
        S = self.S
        S.op("dve", "tensor_tensor", tmp[:, 0:n], x[:, 0:n], x[:, 0:n], ALU.mult, r=[x], w=[tmp])
        S.op("dve", "tensor_scalar", tmp[:, 0:n], tmp[:, 0:n], 0.044715, 1.0, ALU.mult, ALU.add, r=[tmp], w=[tmp])
        S.op("dve", "tensor_tensor", tmp[:, 0:n], tmp[:, 0:n], x[:, 0:n], ALU.mult, r=[tmp, x], w=[tmp])
        S.op("act", "activation", tmp[:, 0:n], tmp[:, 0:n], AF.Sigmoid, scale=1.5957691216057308, r=[tmp], w=[tmp])
        S.op("dve", "tensor_tensor", out[:, 0:n], tmp[:, 0:n], x[:, 0:n], ALU.mult, r=[tmp, x], w=[out])

    def nsa_compress(self):
        S = self.S
        self.kcT_d = S.dram("kcT_d", [4, 128, 1024], BF16)
        self.vc_d = S.dram("vc_d", [4, 128, 8, 128], BF16)
        with S.scope():
            w1 = S.sb("cp_w1", [128, 32, 256], BF16)
            w2 = S.sb("cp_w2", [128, 2, 128], BF16)
            peT = S.sb("cp_pe", [128, 32], BF16)
            pb = S.sb("cp_pb", [128, 2], F32)
            xr = S.ring("cp_x", [128, 8208], BF16, 2)
            hx = S.ring("cp_hx", [128, 512], F32, 2)
            tmpr = S.ring("cp_tmp", [128, 512], F32, 2)
            hid = [S.sb(f"cp_hid{i}", [128, 512], BF16) for i in range(2)]
            otr = S.ring("cp_ot", [128, 512], BF16, 2)
            sqr = S.ring("cp_sq", [128, 512], BF16, 1)
            rrr = S.ring("cp_rr", [128, 512], F32, 1)
            psr = S.psring("cp_ps", [128, 512], F32, 3)
            ps2r = S.psring("cp_ps2", [128, 512], F32, 1)
            psb = S.ps("cp_psb", [128, 2], F32)
            for kv in range(2):
                sfx = "k" if kv == 0 else "v"
                w1src = self.din("cmp_w1_" + sfx, [32, 128, 256])
                w2src = self.din("cmp_w2_" + sfx, [256, 128])
                pesrc = self.din("cmp_peT_" + sfx, [128, 32])
                S.dma("pool", w1[:], w1src.ap().rearrange("l d e -> d l e"), w=[w1])
                S.dma("pool", w2[:], w2src.ap().rearrange("(c e) d -> e c d", c=2), w=[w2])
                S.dma("pool", peT[:], pesrc.ap()[:, :], w=[peT])
                for ec in range(2):
                    for l in range(32):
                        S.op("pe", "matmul", psb[:, ec:ec + 1], lhsT=w1[:, l, ec * 128:(ec + 1) * 128],
                             rhs=peT[:, l:l + 1], start=(l == 0), stop=(l == 31), r=[w1, peT], w=[psb])
                S.op("dve", "tensor_copy", pb[:], psb[:], r=[psb], w=[pb])
                for g in range(4):
                    ch = (5 if kv == 0 else 9) + g
                    for nt in range(2):
                        n0 = nt * 512
                        nn = 512 if nt == 0 else 511
                        xt = xr.next()
                        S.dma("sp", xt[:, 0:16 * nn + 16], self.kT_all.t.ap()[ch, :, 16 * n0:16 * n0 + 16 * nn + 16],
                              r=[self.kT_all], w=[xt])
                        xv = xt[:, 0:8208].rearrange("p (n s) -> p n s", s=16)
                        for ec in range(2):
                            ps = psr.next()
                            for l in range(32):
                                S.op("pe", "matmul", ps[:, 0:nn], lhsT=w1[:, l, ec * 128:(ec + 1) * 128],
                                     rhs=xv[:, l // 16:l // 16 + nn, l % 16], start=(l == 0), stop=(l == 31),
                                     r=[w1, xt], w=[ps])
                            x32 = hx.next()
                            S.op("act", "activation", x32[:, 0:nn], ps[:, 0:nn], AF.Identity, bias=pb[:, ec:ec + 1],
                                 r=[ps, pb], w=[x32])
                            self.gelu_tanh(hid[ec], x32, nn, tmpr.next())
                        if kv == 0:
                            ps = psr.next()
                            for ec in range(2):
                                S.op("pe", "matmul", ps[:, 0:nn], lhsT=w2[:, ec, :], rhs=hid[ec][:, 0:nn],
                                     start=(ec == 0), stop=(ec == 1), r=[w2, hid[ec]], w=[ps])
                            ot = otr.next()
                            S.op("pool", "memset", ot[:], 0.0, w=[ot])
                            self.evac_T("n3", ps, ot, nn, (sqr.next(), ps2r.next(), rrr.next()))
                            S.dma("sp", self.kcT_d.t.ap()[g, :, n0:n0 + 512], ot[:], r=[ot], w=[self.kcT_d])
                        else:
                            ot = otr.next()
                            S.op("pool", "memset", ot[:], 0.0, w=[ot])
                            ps = psr.next()
                            for nb in range(4):
                                m = min(128, nn - nb * 128)
                                for ec in range(2):
                                    S.op("pe", "matmul", ps[0:m, nb * 128:(nb + 1) * 128],
                                         lhsT=hid[ec][:, nb * 128:nb * 128 + m], rhs=w2[:, ec, :],
                                         start=(ec == 0), stop=(ec == 1), r=[w2, hid[ec]], w=[ps])
                            for nb in range(4):
                                m = min(128, nn - nb * 128)
                                S.op("act", "activation", ot[0:m, nb * 128:(nb + 1) * 128],
                                     ps[0:m, nb * 128:(nb + 1) * 128], AF.Identity, r=[ps], w=[ot])
                            S.dma("sp", self.vc_d.t.ap()[g, :, nt * 4:(nt + 1) * 4, :],
                                  ot[:].rearrange("p (b d) -> p b d", b=4), r=[ot], w=[self.vc_d])
            if "cmp" in self.dbg:
                o = self.dout("d_kcT", [4, 128, 1024], BF16)
                S.dma("sp", o.ap()[:, :, :], self.kcT_d.t.ap()[:, :, :], r=[self.kcT_d], w=[self.outs["d_kcT"][1]])
                o = self.dout("d_vc", [4, 128, 8, 128], BF16)
                S.dma("sp", o.ap()[:, :, :, :], self.vc_d.t.ap()[:, :, :, :], r=[self.vc_d], w=[self.outs["d_vc"][1]])

    def nsa_attn(self):
        S = self.S
        nj = NJ if "small" not in self.dbg else 2
        with S.scope():
            P = self.attn_rings("na_")
            keep = S.ring("na_keep", [128, 512], BF16, 9)
            tab_s = S.sb("na_tabs", [128, 21, 512], BF16)
            tab_w = S.sb("na_tabw", [128, 12, 512], BF16)
            tab_c = S.sb("na_tabc", [128, 5, 512], BF16)
            qr = S.ring("na_q", [128, 4, 128], BF16, 2)
            ftab = S.sb("na_ftab", [128, NJ, 256], F32)
            ov = S.sb("na_ov", [128, 8, 256], BF16)
            yexp = S.sb("na_yexp", [128, 8192], BF16)
            S.dma("sp", ftab[:], self.din("ftab", [NJ, 128, 256]).ap().rearrange("j p m -> p j m"), w=[ftab])
            S.dma("pool", ov[:], self.din("ov", [1024, 256]).ap().rearrange("(c p) m -> p c m", p=128), w=[ov])
            S.dma("pool", yexp[:], self.din("yexp", [128, 8192]).ap()[:, :], w=[yexp])
            rdr = S.ring("na_rd", [128, 512], F32, 2)
            Rr = S.ring("na_R", [128, 512], F32, 2)
            yacc = S.sb("na_yacc", [128, 512], F32)
            ytmp = S.sb("na_ytmp", [128, 512], F32)
            yr = S.ring("na_y", [128, 512], BF16, 2)
            pnr = S.ring("na_pn", [128, 512], BF16, 2)
            rg = S.ring("na_rg", [128, 512], BF16, 2)
            imp = S.sb("na_imp", [128, 256], F32)
            imp3 = S.sb("na_imp3", [128, 256], F32)
            m8 = S.sb("na_m8", [128, 16], F32)
            mb = S.sb("na_mb", [128, 256], BF16)
            mbT = S.sb("na_mbT", [128, 2, 128], BF16)
            mkr = S.ring("na_mk", [128, 128], BF16, 3)
            psm = S.psring("na_psm", [128, 512], F32, 1)
            pst = S.ps("na_pst", [128, 1024], BF16)

            def finish_branch(br, oT, den, g, j):
                r = rg.next()
                for h in range(4):
                    col = (4 * g + h) * 3 + br
                    S.op("dve", "tensor_scalar", r[:, h * 128:(h + 1) * 128], self.ident[:],
                         self.gbs[:, j, col:col + 1], None, ALU.mult, r=[self.ident, self.gbs], w=[r])
                pg = psm.next()
                S.op("pe", "matmul", pg[:], lhsT=self.ones_bf[:], rhs=r[:], start=True, stop=True,
                     r=[r, self.ones_bf], w=[pg])
                rd = rdr.next()
                S.op("dve", "tensor_scalar", rd[:], den[:], 1e-30, None, ALU.max, r=[den], w=[rd])
                S.op("dve", "reciprocal", rd[:], rd[:], r=[rd], w=[rd])
                R = Rr.next()
                S.op("dve", "tensor_tensor", R[:], rd[:], pg[:], ALU.mult, r=[rd, pg], w=[R])
                if br == 0:
                    S.op("dve", "tensor_tensor", yacc[:], oT[:], R[:], ALU.mult, r=[oT, R], w=[yacc])
                else:
                    S.op("dve", "tensor_tensor", ytmp[:], oT[:], R[:], ALU.mult, r=[oT, R], w=[ytmp])
                    S.op("dve", "tensor_tensor", yacc[:], yacc[:], ytmp[:], ALU.add, r=[yacc, ytmp], w=[yacc])
                return rd

            for g in range(4):
                self.load_tab("slc", g, tab_s, 21)
                self.load_tab("win", g, tab_w, 12)
                self.load_tab("cmp", g, tab_c, 5)
                for j in range(nj):
                    qt = self.load_qT(qr, 32, g, j)
                    qv = qt[:].rearrange("p h q -> p (h q)")
                    ncc = j // 2 + 1
                    keep.i = 0
                    oT, den = self.attn(P, qv, qt, self.kcT_d.t.ap()[g], self.kcT_d, self.vc_d.t.ap()[g], self.vc_d,
                                        0, ncc, tab_c, lambda kb, j=j: min(j - 2 * kb, 4), None, keep=keep)
                    rd = finish_branch(0, oT, den, g, j)
                    pi = psm.next()
                    for cc in range(ncc):
                        pn = pnr.next()
                        S.op("dve", "tensor_tensor", pn[:], keep.bufs[cc][:], rd[:], ALU.mult,
                             r=[keep.bufs[cc], rd], w=[pn])
                        for h in range(4):
                            S.op("pe", "matmul", pi[:, 0:256], lhsT=pn[:, h * 128:(h + 1) * 128], rhs=ov[:, cc, :],
                                 start=(cc == 0 and h == 0), stop=(cc == ncc - 1 and h == 3), r=[pn, ov], w=[pi])
                    S.op("dve", "tensor_tensor", imp[:], pi[:, 0:256], ftab[:, j, :], ALU.add, r=[pi, ftab], w=[imp])
                    S.op("dve", "max", m8[:, 0:8], imp[:], r=[imp], w=[m8])
                    S.op("dve", "match_replace", imp3[:], m8[:, 0:8], imp[:], -3.0e38, r=[m8, imp], w=[imp3])
                    S.op("dve", "max", m8[:, 8:16], imp3[:], r=[imp3], w=[m8])
                    S.op("dve", "tensor_scalar", mb[:], imp[:], m8[:, 15:16], -30000.0, ALU.is_lt, ALU.mult,
                         r=[imp, m8], w=[mb])
                    for hh in range(2):
                        S.op("pe", "transpose", pst[:, hh * 128:(hh + 1) * 128], mb[:, hh * 128:(hh + 1) * 128],
                             self.ident[:], r=[mb, self.ident], w=[pst])
                    S.op("act", "activation", mbT[:].rearrange("p a b -> p (a b)"), pst[:, 0:256], AF.Identity,
                         r=[pst], w=[mbT])

                    def slc_mask(kb):
                        return (yexp[:, (kb % 64) * 128:(kb % 64 + 1) * 128], mbT[:, kb // 64, :], [yexp, mbT])

                    nkb = 8 * j + 8
                    oT, den = self.attn(P, qv, qt, self.kT_all.t.ap()[13 + g], self.kT_all,
                                        self.v_all.t.ap()[1, g], self.v_all, 0, nkb, tab_s,
                                        lambda kb, j=j: min(8 * j + 7 - kb, 20), slc_mask)
                    finish_branch(1, oT, den, g, j)
                    kb0 = max(0, 8 * j - 4)
                    oT, den = self.attn(P, qv, qt, self.kT_all.t.ap()[17 + g], self.kT_all,
                                        self.v_all.t.ap()[2, g], self.v_all, kb0, nkb, tab_w,
                                        lambda kb, j=j: kb - (8 * j - 4), None)
                    finish_branch(2, oT, den, g, j)
                    y = yr.next()
                    S.op("act", "activation", y[:], yacc[:], AF.Identity, r=[yacc], w=[y])
                    self.store_y(y, 16, g, j)


    def row_bcast(self, dst, col0):
        S = self.S
        with S.scope():
            tl = S.ring("rb_l", [128, 128], F32, 2)
            ps = S.psring("rb_ps", [128, 512], F32, 2)
            onesf = S.sb("rb_ones", [128, 128], F32)
            S.op("dve", "memset", onesf[:], 1.0, w=[onesf])
            for k4 in range(8):
                p = ps.next()
                for i in range(4):
                    kc = k4 * 4 + i
                    t = tl.next()
                    S.op("dve", "tensor_scalar", t[:], onesf[:], self.modc[:, col0 + kc:col0 + kc + 1], None, ALU.mult,
                         r=[onesf, self.modc], w=[t])
                    S.op("pe", "matmul", p[:, i * 128:(i + 1) * 128], lhsT=t[:], rhs=self.identf[:], start=True,
                         stop=True, r=[t, self.identf], w=[p])
                S.op("act", "activation", dst[:, k4 * 512:(k4 + 1) * 512], p[:], AF.Identity, r=[p], w=[dst])

    def merge(self):
        S = self.S
        self.x1_d = S.dram("x1_d", [NOWN, D], F32)
        self.mT_d = S.dram("mT_d", [32, 128, NOWN], BF16)
        self.hT2_own = S.dram("hT2_own", [4, 128, KC, 512], BF16)
        wa_src = self.din("w_branch_a", [2048, D]).ap().rearrange("(h p) n -> p h n", p=128)
        wb_src = self.din("w_branch_b", [2048, D]).ap().rearrange("(h p) n -> p h n", p=128)
        ya = self.yT_all.t.ap()
        qTa = self.qT_all.t.ap()
        with S.scope():
            wa = S.sb("mg_wa", [128, 16, 1024], BF16)
            wb = S.sb("mg_wb", [128, 16, 1024], BF16)
            yar = S.ring("mg_ya", [128, 16, 512], BF16, 2)
            ybr = S.ring("mg_yb", [128, 16, 512], BF16, 2)
            gr = S.ring("mg_g", [128, 2, 512], BF16, 3)
            t1r = S.ring("mg_t1", [128, 512], F32, 2)
            t2r = S.ring("mg_t2", [128, 512], F32, 2)
            mr = S.ring("mg_m", [128, 512], BF16, 3)
            psr = S.psring("mg_ps", [128, 512], F32, 4)
            for ccg in range(4):
                for h in range(16):
                    S.dma("pool", wa[:, h, :], wa_src[:, h, ccg * 1024:(ccg + 1) * 1024], w=[wa])
                    S.dma("pool", wb[:, h, :], wb_src[:, h, ccg * 1024:(ccg + 1) * 1024], w=[wb])
                for tt in range(4):
                    yat = yar.next()
                    ybt = ybr.next()
                    S.dma("sp", yat[:], ya[0:16, :, tt * 512:(tt + 1) * 512].rearrange("h p q -> p h q"),
                          r=[self.yT_all], w=[yat])
                    S.dma("sp", ybt[:], ya[16:32, :, tt * 512:(tt + 1) * 512].rearrange("h p q -> p h q"),
                          r=[self.yT_all], w=[ybt])
                    for ci in range(8):
                        cc = ccg * 8 + ci
                        gt = gr.next()
                        S.dma("sp", gt[:, 0, :], qTa[48 + cc, :, tt * 512:(tt + 1) * 512], r=[self.qT_all], w=[gt])
                        S.dma("sp", gt[:, 1, :], qTa[80 + cc, :, tt * 512:(tt + 1) * 512], r=[self.qT_all], w=[gt])
                        pA = psr.next()
                        for h in range(16):
                            S.op("pe", "matmul", pA[:], lhsT=wa[:, h, ci * 128:(ci + 1) * 128], rhs=yat[:, h, :],
                                 start=(h == 0), stop=(h == 15), r=[wa, yat], w=[pA])
                        pB = psr.next()
                        for h in range(16):
                            S.op("pe", "matmul", pB[:], lhsT=wb[:, h, ci * 128:(ci + 1) * 128], rhs=ybt[:, h, :],
                                 start=(h == 0), stop=(h == 15), r=[wb, ybt], w=[pB])
                        t1 = t1r.next()
                        t2 = t2r.next()
                        S.op("dve", "tensor_tensor", t1[:], pA[:], gt[:, 0, :], ALU.mult, r=[pA, gt], w=[t1])
                        S.op("dve", "tensor_tensor", t2[:], pB[:], gt[:, 1, :], ALU.mult, r=[pB, gt], w=[t2])
                        m = mr.next()
                        S.op("pool", "tensor_tensor", m[:], t1[:], t2[:], ALU.add, r=[t1, t2], w=[m])
                        S.dma("sp", self.mT_d.t.ap()[cc, :, tt * 512:(tt + 1) * 512], m[:], r=[m], w=[self.mT_d])
        with S.scope():
            gtB = S.sb("mo_gtB", [128, D], F32)
            self.row_bcast(gtB, 64)
            wo_src = self.din("w_out", [D, D]).ap().rearrange("(kc p) n -> p kc n", p=128)
            mt = S.sb("mo_mt", [128, KC, 512], BF16)
            wo = S.sb("mo_wo", [128, KC, 512], BF16)
            xts = [S.sb(f"mo_x{i}", [128, D], F32) for i in range(4)]
            tmpr = S.ring("mo_tmp", [128, 512], F32, 2)
            psr = S.psring("mo_ps", [128, 512], F32, 3)
            with S.scope():
                pass
            for tg in range(4):
                S.dma("sp", mt[:], self.mT_d.t.ap()[:, :, tg * 512:(tg + 1) * 512].rearrange("c p q -> p c q"),
                      r=[self.mT_d], w=[mt])
                for tb in range(4):
                    r0 = (tg * 4 + tb) * 128
                    S.dma("sp", xts[tb][:], self.i_xown.ap()[r0:r0 + 128, :], w=[xts[tb]])
                for ct in range(8):
                    for kc in range(KC):
                        S.dma("pool", wo[:, kc, :], wo_src[:, kc, ct * 512:(ct + 1) * 512], w=[wo])
                    for tb in range(4):
                        ps = psr.next()
                        for kc in range(KC):
                            S.op("pe", "matmul", ps[:], lhsT=mt[:, kc, tb * 128:(tb + 1) * 128], rhs=wo[:, kc, :],
                                 start=(kc == 0), stop=(kc == KC - 1), r=[mt, wo], w=[ps])
                        tmp = tmpr.next()
                        sl = slice(ct * 512, (ct + 1) * 512)
                        S.op("dve", "tensor_tensor", tmp[:], ps[:], gtB[:, sl], ALU.mult, r=[ps, gtB], w=[tmp])
                        S.op("pool", "tensor_tensor", xts[tb][:, sl], xts[tb][:, sl], tmp[:], ALU.add,
                             r=[xts[tb], tmp], w=[xts[tb]])
                for tb in range(4):
                    r0 = (tg * 4 + tb) * 128
                    S.dma("sp", self.x1_d.t.ap()[r0:r0 + 128, :], xts[tb][:], r=[xts[tb]], w=[self.x1_d])
        self.phase1(self.x1_d.t, self.hT2_own, NJ, AB_off=64, src_tok=self.x1_d)


    def peer(self):
        S = self.S
        NE = 16384
        self.uT_d = S.dram("uT_d", [128, 128, KC, 128], BF16)
        self.qpT_d = S.dram("qpT_d", [16, 128, NOWN], BF16)
        u_src = self.din("peer_u", [NE, D]).ap()
        v_src = self.din("peer_v", [NE, D]).ap()
        self.vbf_d = S.dram("vbf_d", [NE, D], BF16)
        with S.scope():
            vcr = S.ring("pu_v", [128, D], BF16, 2)
            ur = S.ring("pu_u", [128, D], BF16, 2)
            utr = S.ring("pu_ut", [128, KC, 128], BF16, 2)
            psr = S.psring("pu_ps", [128, 1024], BF16, 3)
            nec = 128
            for ec in range(nec):
                ut = ur.next()
                S.dma("pool", ut[:], u_src[ec * 128:(ec + 1) * 128, :], w=[ut])
                utt = utr.next()
                for k4 in range(4):
                    pt = psr.next()
                    for i in range(8):
                        kc = k4 * 8 + i
                        S.op("pe", "transpose", pt[:, i * 128:(i + 1) * 128], ut[:, kc * 128:(kc + 1) * 128],
                             self.ident[:], r=[ut, self.ident], w=[pt])
                    dst = utt[:, k4 * 8:(k4 + 1) * 8, :].rearrange("p a b -> p (a b)")
                    if k4 % 2 == 0:
                        S.op("act", "activation", dst, pt[:], AF.Identity, r=[pt], w=[utt])
                    else:
                        S.op("dve", "tensor_copy", dst, pt[:], r=[pt], w=[utt])
                S.dma("sp", self.uT_d.t.ap()[ec], utt[:], r=[utt], w=[self.uT_d])
                vc_ = vcr.next()
                S.dma("pool", vc_[:], v_src[ec * 128:(ec + 1) * 128, :], w=[vc_])
                S.dma("sp", self.vbf_d.t.ap()[ec * 128:(ec + 1) * 128, :], vc_[:], r=[vc_], w=[self.vbf_d])
        with S.scope():
            hts = [S.sb(f"pq_h{i}", [128, KC, 512], BF16) for i in range(2)]
            wr = S.ring("pq_w", [128, KC, 512], BF16, 2)
            psr = S.psring("pq_ps", [128, 512], F32, 3)
            otr = S.ring("pq_ot", [128, 512], BF16, 3)
            for half in range(2):
                for i in range(2):
                    S.dma("sp", hts[i][:], self.hT2_own.t.ap()[half * 2 + i], r=[self.hT2_own], w=[hts[i]])
                for gq in range(4):
                    wt = wr.next()
                    self.load_w(self.din("w_peer_q", [D, 2048]) if "w_peer_q" not in self.inp else self.inp["w_peer_q"],
                                gq * 512, 512, wt)
                    for i in range(2):
                        tt = half * 2 + i
                        for ci in range(4):
                            ch = gq * 4 + ci
                            ps = psr.next()
                            for kc in range(KC):
                                S.op("pe", "matmul", ps[:], lhsT=wt[:, kc, ci * 128:(ci + 1) * 128],
                                     rhs=hts[i][:, kc, :], start=(kc == 0), stop=(kc == KC - 1),
                                     r=[wt, hts[i]], w=[ps])
                            ot = otr.next()
                            S.op("act", "activation", ot[:], ps[:], AF.Identity, r=[ps], w=[ot])
                            S.dma("sp", self.qpT_d.t.ap()[ch, :, tt * 512:(tt + 1) * 512], ot[:], r=[ot],
                                  w=[self.qpT_d])
        with S.scope():
            keysT = S.sb("pe_keys", [128, 16, 128], BF16)
            S.dma("pool", keysT[:], self.din("peer_keysT", [16, 128, 128]).ap().rearrange("c d n -> d c n"),
                  w=[keysT])
            ngrp = 8 if "small" not in self.dbg else 1
            nec = 128
            for tg in range(ngrp):
                with S.scope():
                    actT = S.sb("pe_actT", [128, 128, 256], BF16)
                    with S.scope():
                        W = [S.sb(f"pe_W{i}", [128, NE], BF16) for i in range(2)]
                        with S.scope():
                            qp = S.sb("pe_qp", [128, 16, 256], BF16)
                            S.dma("sp", qp[:], self.qpT_d.t.ap()[:, :, tg * 256:(tg + 1) * 256]
                                  .rearrange("c p q -> p c q"), r=[self.qpT_d], w=[qp])
                            ssb = S.sb("pe_s", [128, 16, 128], F32)
                            pss = S.psring("pe_pss", [128, 512], F32, 2)
                            S4 = S.ring("pe_S4", [128, 1024], F32, 3)
                            E4 = S.ring("pe_E4", [128, 1024], BF16, 5)
                            sm = {k: S.sb("pe_" + k, [128, n], F32) for k, n in
                                  (("t1", 16), ("t2", 16), ("sr", 128), ("cand", 256), ("cr", 256), ("tc", 16),
                                   ("e16", 16), ("den", 1), ("nthr", 1), ("bias", 1), ("thr", 1))}
                            for tb in range(2):
                                for c4 in range(4):
                                    ps = pss.next()
                                    for i in range(4):
                                        ch = c4 * 4 + i
                                        S.op("pe", "matmul", ps[:, i * 128:(i + 1) * 128],
                                             lhsT=qp[:, ch, tb * 128:(tb + 1) * 128], rhs=keysT[:, ch, :],
                                             start=True, stop=True, r=[qp, keysT], w=[ps])
                                    S.op("act", "activation", ssb[:, c4 * 4:(c4 + 1) * 4, :].rearrange("p a b -> p (a b)"),
                                         ps[:], AF.Identity, r=[ps], w=[ssb])
                                for h in range(8):
                                    s1 = ssb[:, 2 * h, :]
                                    s2 = ssb[:, 2 * h + 1, :]
                                    for (sx, tx) in ((s1, sm["t1"]), (s2, sm["t2"])):
                                        S.op("dve", "max", tx[:, 0:8], sx, r=[ssb], w=[tx])
                                        S.op("dve", "match_replace", sm["sr"][:], tx[:, 0:8], sx, -3.0e38,
                                             r=[ssb, tx], w=[sm["sr"]])
                                        S.op("dve", "max", tx[:, 8:16], sm["sr"][:], r=[sm["sr"]], w=[tx])
                                    cand = sm["cand"]
                                    S.op("dve", "tensor_tensor", cand[:].rearrange("p (a b) -> p a b", a=16),
                                         sm["t1"][:].unsqueeze(2).broadcast_to([128, 16, 16]),
                                         sm["t2"][:].unsqueeze(1).broadcast_to([128, 16, 16]), ALU.add,
                                         r=[sm["t1"], sm["t2"]], w=[cand])
                                    tc_ = sm["tc"]
                                    S.op("dve", "max", tc_[:, 0:8], cand[:], r=[cand], w=[tc_])
                                    S.op("dve", "match_replace", sm["cr"][:], tc_[:, 0:8], cand[:], -3.0e38,
                                         r=[cand, tc_], w=[sm["cr"]])
                                    S.op("dve", "max", tc_[:, 8:16], sm["cr"][:], r=[sm["cr"]], w=[tc_])
                                    thr, nthr, den, bias = sm["thr"], sm["nthr"], sm["den"], sm["bias"]
                                    S.op("dve", "tensor_copy", thr[:], tc_[:, 15:16], r=[tc_], w=[thr])
                                    S.op("dve", "tensor_scalar", nthr[:], tc_[:, 15:16], -1.0, None, ALU.mult,
                                         r=[tc_], w=[nthr])
                                    S.op("act", "activation", sm["e16"][:], tc_[:], AF.Exp, bias=nthr[:, 0:1],
                                         accum_out=den[:], r=[tc_, nthr], w=[sm["e16"], den])
                                    S.op("act", "activation", den[:], den[:], AF.Ln, r=[den], w=[den])
                                    S.op("dve", "tensor_tensor", bias[:], nthr[:], den[:], ALU.subtract,
                                         r=[nthr, den], w=[bias])
                                    pend = {}

                                    def gate_a(q8, h=h, tb=tb, s1=s1, s2=s2, bias=bias, thr=thr, pend=pend):
                                        i0 = q8 * 8
                                        s4 = S4.next()
                                        S.op("pool" if q8 % 2 else "dve", "tensor_tensor",
                                             s4[:].rearrange("p (a b) -> p a b", a=8),
                                             s1[:, i0:i0 + 8].unsqueeze(2).broadcast_to([128, 8, 128]),
                                             s2.unsqueeze(1).broadcast_to([128, 8, 128]), ALU.add,
                                             r=[ssb], w=[s4])
                                        e4 = E4.next()
                                        S.op("act", "activation", e4[:], s4[:], AF.Exp, bias=bias[:, 0:1],
                                             r=[s4, bias], w=[e4])
                                        wsl = W[tb][:, q8 * 1024:(q8 + 1) * 1024]
                                        if h == 0:
                                            S.op("dve", "scalar_tensor_tensor", wsl, s4[:], thr[:, 0:1], e4[:],
                                                 ALU.is_ge, ALU.mult, r=[s4, thr, e4], w=[W[tb]])
                                        else:
                                            S.op("dve", "scalar_tensor_tensor", e4[:], s4[:], thr[:, 0:1], e4[:],
                                                 ALU.is_ge, ALU.mult, r=[s4, thr, e4], w=[e4])
                                            pend[q8] = e4

                                    def gate_b(q8, tb=tb, pend=pend):
                                        e4 = pend.pop(q8)
                                        wsl = W[tb][:, q8 * 1024:(q8 + 1) * 1024]
                                        S.op("pool", "tensor_tensor", wsl, wsl, e4[:], ALU.add,
                                             r=[W[tb], e4], w=[W[tb]])

                                    for n in range(16 + 2):
                                        if n < 16:
                                            gate_a(n)
                                        if n >= 2 and h > 0:
                                            gate_b(n - 2)
                        with S.scope():
                            h2 = S.sb("pe_h2", [128, KC, 256], BF16)
                            S.dma("sp", h2[:], self.hT2_own.t.ap()[tg // 2, :, :, (tg % 2) * 256:(tg % 2 + 1) * 256],
                                  r=[self.hT2_own], w=[h2])
                            uTr = S.ring("pe_uT", [128, KC, 128], BF16, 2)
                            psa = S.psring("pe_psa", [128, 256], F32, 2)
                            pst = S.psring("pe_pst", [128, 256], BF16, 2)
                            xg = S.ring("pe_xg", [128, 256], F32, 2)
                            tg_ = S.ring("pe_tg", [128, 256], F32, 2)
                            gl = S.ring("pe_gl", [128, 256], F32, 2)
                            for ec in range(nec):
                                uT = uTr.next()
                                S.dma("sp" if ec % 2 == 0 else "pool", uT[:], self.uT_d.t.ap()[ec],
                                      r=[self.uT_d], w=[uT])
                                pa = psa.next()
                                for kc in range(KC):
                                    S.op("pe", "matmul", pa[:], lhsT=uT[:, kc, :], rhs=h2[:, kc, :],
                                         start=(kc == 0), stop=(kc == KC - 1), r=[uT, h2], w=[pa])
                                pt = pst.next()
                                for tb in range(2):
                                    S.op("pe", "transpose", pt[:, tb * 128:(tb + 1) * 128],
                                         W[tb][:, ec * 128:(ec + 1) * 128], self.ident[:],
                                         r=[W[tb], self.ident], w=[pt])
                                x = xg.next()
                                S.op("act", "activation", x[:], pa[:], AF.Identity, r=[pa], w=[x])
                                g_ = gl.next()
                                self.gelu_tanh(g_, x, 256, tg_.next())
                                S.op("dve", "tensor_tensor", actT[:, ec, :], g_[:], pt[:], ALU.mult,
                                     r=[g_, pt], w=[actT])
                    with S.scope():
                        gtB = S.sb("pe_gtB", [128, D], F32)
                        self.row_bcast(gtB, 160)
                        vr = S.ring("pe_v", [128, 1024], BF16, 4)
                        x1 = [S.sb(f"pe_x1{i}", [128, D], F32) for i in range(2)]
                        pso = [S.ps(f"pe_pso{i}", [128, 512], F32) for i in range(4)]
                        tmpr = S.ring("pe_tmp", [128, 512], F32, 2)
                        for tb in range(2):
                            r0 = tg * 256 + tb * 128
                            S.dma("sp", x1[tb][:], self.x1_d.t.ap()[r0:r0 + 128, :], r=[self.x1_d], w=[x1[tb]])
                        for dq in range(4):
                            for ec in range(nec):
                                vt = vr.next()
                                S.dma("sp" if ec % 2 == 0 else "act", vt[:],
                                      self.vbf_d.t.ap()[ec * 128:(ec + 1) * 128, dq * 1024:(dq + 1) * 1024],
                                      r=[self.vbf_d], w=[vt])
                                for tb in range(2):
                                    for d2 in range(2):
                                        S.op("pe", "matmul", pso[tb * 2 + d2][:],
                                             lhsT=actT[:, ec, tb * 128:(tb + 1) * 128],
                                             rhs=vt[:, d2 * 512:(d2 + 1) * 512], start=(ec == 0), stop=(ec == nec - 1),
                                             r=[actT, vt], w=[pso[tb * 2 + d2]])
                            for tb in range(2):
                                for d2 in range(2):
                                    sl = slice(dq * 1024 + d2 * 512, dq * 1024 + (d2 + 1) * 512)
                                    tmp = tmpr.next()
                                    S.op("dve", "tensor_tensor", tmp[:], pso[tb * 2 + d2][:], gtB[:, sl], ALU.mult,
                                         r=[pso[tb * 2 + d2], gtB], w=[tmp])
                                    S.op("pool", "tensor_tensor", x1[tb][:, sl], x1[tb][:, sl], tmp[:], ALU.add,
                                         r=[x1[tb], tmp], w=[x1[tb]])
                        out = self.outs["out"]
                        for tb in range(2):
                            r0 = tg * 256 + tb * 128
                            S.dma("sp", out[0].ap()[r0:r0 + 128, :], x1[tb][:], r=[x1[tb]], w=[out[1]])


def rel_bucket_np(d):
    n = np.maximum(d, 0)
    nf = np.maximum(n, 1).astype(np.float32)
    lb = 16 + (np.log(nf / np.float32(16)) / np.float32(np.log(2048 / 16)) * np.float32(16)).astype(np.int32)
    return np.where(n < 16, n, np.minimum(lb, 31))


def _bias_tile(rel_bias, heads0, dist, valid):
    bk = rel_bucket_np(dist)
    vals = rel_bias[bk][:, :, heads0:heads0 + 16]
    vals = np.where(valid[:, :, None], vals, np.float32(-30000.0))
    return np.ascontiguousarray(vals.reshape(128, 128, 4, 4).transpose(2, 0, 3, 1).reshape(4, 128, 512)).astype(np.float32)


def make_tables(rel_bias, c):
    k = np.arange(128)[:, None]
    q = np.arange(128)[None, :]
    out = {}
    for name, h0 in (("dsa", 0), ("slc", 16)):
        tiles = []
        for op in range(21):
            dist = 128 * (op + c - 7) + q - k
            tiles.append(_bias_tile(rel_bias, h0, dist, dist >= 0))
        out["tab_" + name] = np.stack(tiles)
    tiles = []
    for w in range(12):
        dist = 128 * (c + 4 - w) + q - k
        tiles.append(_bias_tile(rel_bias, 16, dist, (dist >= 0) & (dist < 512)))
    out["tab_win"] = np.stack(tiles)
    tiles = []
    for e8 in range(5):
        e = 8 * e8 if e8 < 4 else 64
        dist = 128 * (e + c) + q - (16 * k + 31)
        tiles.append(_bias_tile(rel_bias, 16, dist, dist >= 0))
    out["tab_cmp"] = np.stack(tiles)
    ft = np.zeros((NJ, 128, 256), np.float32)
    mm = np.arange(256)[None, :]
    for j in range(NJ):
        tq = 128 * (8 * j + c) + np.arange(128)[:, None]
        cur = tq // 64
        forced = (mm == 0) | (mm == cur) | (mm == cur - 1)
        adm = mm * 64 <= tq
        ft[j] = np.where(adm, np.where(forced, 1e9, 0.0), -1e30)
    out["ftab"] = ft
    kk = np.arange(1024)[None, :]
    qq = np.arange(128)[:, None]
    out["cmask"] = np.where(kk <= 128 * c + qq, 0.0, -1e30).astype(np.float32)
    return out

def make_consts():
    n = np.arange(1024)[:, None]
    m = np.arange(256)[None, :]
    ov = ((16 * n < 64 * m + 64) & (16 * n + 32 > 64 * m) & (n < 1023)).astype(np.float32)
    cidx = np.arange(8192)[None, :]
    yexp = (np.arange(128)[:, None] == cidx // 64).astype(np.float32)
    return {"ov": ov, "yexp": yexp}


IN_SPLITS = (2048, 512, 512, 2048, 64, 32, 2048, 3072, 48, 8192)


def prep_inputs(inp):
    offs = np.concatenate([[0], np.cumsum(IN_SPLITS)])
    w_in = inp["w_in"][0]
    seg = lambda i: w_in[:, offs[i]:offs[i + 1]]
    qa, ka, va, qi, ki, wi, qb, kvb, gb, gm = [seg(i) for i in range(10)]
    kv = lambda i: kvb[:, i * 512:(i + 1) * 512]
    w_kT = np.ascontiguousarray(np.concatenate([ka, ki, ki, kv(0), kv(1), kv(2), kv(4)], axis=1))
    w_v = np.ascontiguousarray(np.concatenate([va, kv(3), kv(5)], axis=1))
    w_qT = np.ascontiguousarray(np.concatenate([qa, qi, qb, gm], axis=1))
    w_qs = np.ascontiguousarray(np.concatenate([wi, gb], axis=1))
    colT = lambda v: np.ascontiguousarray(v.reshape(-1, 128).T)
    x = inp["x"][0]
    shared = {
        "xfull": x,
        "cT": colT(inp["c"][0]),
        "w_ada": inp["w_ada"][0],
        "b_adaT": colT(inp["b_ada"][0]),
        "gnT": np.ascontiguousarray(np.concatenate([colT(inp["g_mix"][0]), colT(inp["g_ffn"][0])], axis=1)),
        "gains": np.ascontiguousarray(np.stack([inp[k][0] for k in
                                                ("gq_a", "gk_a", "gq_b", "gk_cmp", "gk_slc", "gk_win")], axis=1)),
        "w_kT": w_kT, "w_v": w_v, "w_qT": w_qT, "w_qs": w_qs,
        "cmp_w1_k": inp["cmp_w1_k"][0], "cmp_w1_v": inp["cmp_w1_v"][0],
        "cmp_w2_k": inp["cmp_w2_k"][0], "cmp_w2_v": inp["cmp_w2_v"][0],
        "cmp_peT_k": np.ascontiguousarray(inp["cmp_pe_k"][0].T), "cmp_peT_v": np.ascontiguousarray(inp["cmp_pe_v"][0].T),
        "w_branch_a": inp["w_branch_a"][0], "w_branch_b": inp["w_branch_b"][0], "w_out": inp["w_out"][0],
        "w_peer_q": inp["w_peer_q"][0],
        "peer_keysT": np.ascontiguousarray(inp["peer_sub_keys"][0].reshape(16, 128, 128).transpose(0, 2, 1)),
        "peer_u": inp["peer_u"][0], "peer_v": inp["peer_v"][0],
    }
    shared.update(make_consts())
    maps = []
    xb = x.reshape(NTB, 128, D)
    for c in range(NCORES):
        m = dict(shared)
        m["xown"] = np.ascontiguousarray(xb[c::8].reshape(NOWN, D))
        m.update(make_tables(inp["rel_bias"], c))
        maps.append(m)
    return maps


_CACHE = {}


def kernel(**inputs):
    inputs = {k: np.asarray(v) for k, v in inputs.items()}
    if "prog" not in _CACHE:
        p = Prog()
        p.build()
        _CACHE["prog"] = p
    p = _CACHE["prog"]
    maps = prep_inputs(inputs)
    maps = [{k: np.ascontiguousarray(v, dtype=np.float32) for k, v in m.items() if k in p.inp} for m in maps]
    res = run_bass_kernel_spmd(p.nc, maps, core_ids=list(range(NCORES)))
    out = np.zeros((1, T, D), np.float32)
    ob = out[0].reshape(NTB, 128, D)
    for c in range(NCORES):
        ob[c::8] = res.results[c]["out"].reshape(NJ, 128, D)
    return out
```

```python
import contextlib
import numpy as np
import ml_dtypes
import concourse.bass as bass
import concourse.mybir as mybir
from concourse.bass_utils import run_bass_kernel_spmd

F32 = mybir.dt.float32
BF16 = mybir.dt.bfloat16
AF = mybir.ActivationFunctionType
ALU = mybir.AluOpType
AX = mybir.AxisListType

NCORES = 8
T = 16384
D = 4096
NTB = T // 128
NOWN = 2048
NJ = 16
EPS = 1e-6
KC = 32


class Tok:
    __slots__ = ("lw", "rd", "dsem", "dcnt", "name")

    def __init__(self, name=""):
        self.lw = None
        self.rd = {}
        self.dsem = None
        self.dcnt = 0
        self.name = name


class Buf:
    def __init__(self, t, tok):
        self.t = t
        self.tok = tok

    def __getitem__(self, k):
        return self.t[k]


class Ring:
    def __init__(self, bufs):
        self.bufs = bufs
        self.i = 0

    def next(self):
        b = self.bufs[self.i % len(self.bufs)]
        self.i += 1
        return b


class Sched:
    def __init__(self, nc):
        self.nc = nc
        self.E = {"pe": nc.tensor, "act": nc.scalar, "dve": nc.vector, "pool": nc.gpsimd, "sp": nc.sync}
        self.sem = {k: nc.semaphore("se_" + k).__enter__() for k in self.E}
        self.cnt = {k: 0 for k in self.E}
        self.seen = {k: {} for k in self.E}
        self.dsems = []
        self.free_dsems = []
        self.dsem_cnt = {}
        self.scope_toks = [[]]
        self.ninst = 0
        self.stack = None

    def scope(self):
        return _Scope(self)

    def _enter(self, cm):
        return self.stack.enter_context(cm)

    def _nm(self, name):
        self.nuid = getattr(self, "nuid", 0) + 1
        return f"{name}_{self.nuid}"

    def sb(self, name, shape, dtype):
        return Buf(self._enter(self.nc.sbuf_tensor(self._nm(name), list(shape), dtype)), Tok(name))

    def ps(self, name, shape, dtype):
        return Buf(self._enter(self.nc.psum_tensor(self._nm(name), list(shape), dtype)), Tok(name))

    def dram(self, name, shape, dtype):
        return Buf(self.nc.dram_tensor(name, list(shape), dtype, kind="Internal"), Tok(name))

    def ring(self, name, shape, dtype, n):
        return Ring([self.sb(f"{name}{i}", shape, dtype) for i in range(n)])

    def psring(self, name, shape, dtype, n):
        return Ring([self.ps(f"{name}{i}", shape, dtype) for i in range(n)])

    @staticmethod
    def _toks(xs):
        return [x.tok if isinstance(x, Buf) else x for x in xs]

    @staticmethod
    def _deps(r, w):
        deps = []
        for t in r:
            if t.lw is not None:
                deps.append(t.lw)
        for t in w:
            if t.lw is not None:
                deps.append(t.lw)
            deps.extend(t.rd.values())
        return deps

    def _wait(self, eng, deps, skip_own=False):
        seen = self.seen[eng]
        own = self.sem[eng]
        for sem, val in deps:
            if skip_own and sem is own:
                continue
            k = id(sem)
            if seen.get(k, 0) < val:
                self.E[eng].wait_ge(sem, val)
                seen[k] = val

    @staticmethod
    def _commit(me, r, w):
        k = id(me[0])
        for t in r:
            t.rd[k] = me
        for t in w:
            t.lw = me
            t.rd = {}

    def op(self, eng, meth, *args, r=(), w=(), **kw):
        r = self._toks(r)
        w = self._toks(w)
        self._wait(eng, self._deps(r, w), skip_own=(eng == "pe"))
        inst = getattr(self.E[eng], meth)(*args, **kw)
        self.cnt[eng] += 1
        inst.then_inc(self.sem[eng], 1)
        self._commit((self.sem[eng], self.cnt[eng]), r, w)
        self.ninst += 1
        return inst

    def dma(self, q, out, in_, r=(), w=(), **kw):
        r = self._toks(r)
        w = self._toks(w)
        self._wait(q, self._deps(r, w))
        inst = self.E[q].dma_start(out=out, in_=in_, **kw)
        t = w[0]
        if t.dsem is None:
            if self.free_dsems:
                t.dsem, t.dcnt = self.free_dsems.pop()
            else:
                t.dsem = self.nc.semaphore(f"sd{len(self.dsems)}").__enter__()
                t.dcnt = 0
                self.dsems.append(t.dsem)
            self.scope_toks[-1].append(t)
        t.dcnt += 16
        inst.then_inc(t.dsem, 16)
        self.dsem_cnt[id(t.dsem)] = (t.dsem, t.dcnt)
        self._commit((t.dsem, t.dcnt), r, w)
        self.ninst += 1
        return inst

    def barrier(self):
        deps = [(self.sem[k], self.cnt[k]) for k in self.E if self.cnt[k] > 0]
        deps += list(self.dsem_cnt.values())
        for eng in self.E:
            self._wait(eng, deps)

    def release_scope_sems(self, toks):
        for t in toks:
            if t.dsem is not None:
                self.free_dsems.append((t.dsem, t.dcnt))
                t.dsem = None
                t.dcnt = 0


class _Scope:
    def __init__(self, S):
        self.S = S

    def __enter__(self):
        self.prev = self.S.stack
        self.es = contextlib.ExitStack()
        self.es.__enter__()
        self.S.stack = self.es
        self.S.scope_toks.append([])
        return self

    def __exit__(self, *a):
        self.S.barrier()
        self.S.release_scope_sems(self.S.scope_toks.pop())
        self.S.stack = self.prev
        return self.es.__exit__(*a)


KCH = (["n1"] * 4) + ["c"] + (["c"] * 8) + (["n4"] * 4) + (["n5"] * 4)
NKCH = len(KCH)
QCH = (["n0"] * 16) + (["c"] * 16) + (["n2"] * 16) + (["s"] * 64)
NQCH = len(QCH)


class Prog:
    def __init__(self, stop="all", dbg=()):
        self.stop = stop
        self.dbg = set(dbg)
        nc = bass.Bass("TRN2", target_bir_lowering=False)
        self.nc = nc
        self.S = Sched(nc)
        self.inp = {}
        self.outs = {}

    def din(self, name, shape, dtype=F32):
        t = self.nc.dram_tensor(name, list(shape), dtype, kind="ExternalInput")
        self.inp[name] = t
        return t

    INSHAPES = {
        "xfull": [T, D], "xown": [NOWN, D], "cT": [128, KC], "w_ada": [D, 6 * D], "b_adaT": [128, 192],
        "gnT": [128, 64], "gains": [128, 6], "w_kT": [D, 21 * 128], "w_v": [D, 1536],
        "w_qT": [D, 112 * 128], "w_qs": [D, 80],
    }

    def __getattr__(self, name):
        if name.startswith("i_") and name[2:] in self.INSHAPES:
            nm = name[2:]
            if nm not in self.inp:
                shp = list(self.INSHAPES[nm])
                if "small" in self.dbg and nm == "xfull":
                    shp[0] = 1024
                if "small" in self.dbg and nm == "w_qT":
                    shp[1] = 1024
                self.din(nm, shp)
            return self.inp[nm]
        raise AttributeError(name)

    def dout(self, name, shape, dtype=F32):
        t = self.nc.dram_tensor(name, list(shape), dtype, kind="ExternalOutput")
        self.outs[name] = (t, Tok(name))
        return t

    def build(self):
        nc, S = self.nc, self.S
        self.hT_all = S.dram("hT_all", [32, 128, KC, 512], BF16)
        self.hT_own = S.dram("hT_own", [4, 128, KC, 512], BF16)
        self.kT_all = S.dram("kT_all", [NKCH, 128, T], BF16)
        self.v_all = S.dram("v_all", [3, 4, 128, NTB, 128], BF16)
        self.qT_all = S.dram("qT_all", [NQCH, 128, NOWN], BF16)

        with S.scope():
            self.consts()
            with S.scope():
                if "nomod" in self.dbg:
                    self.din("modc_in", [128, 192])
                    S.dma("sp", self.modc[:], self.inp["modc_in"].ap()[:, :], w=[self.modc])
                    self.mod_to_AB()
                else:
                    self.phase0()
            if self.stop == "p0":
                return self.finish()
            if "inj" in self.dbg:
                self.inject()
                self.attention_and_rest()
                return self.finish()
            with S.scope():
                self.phase1(self.i_xfull, self.hT_all, NTB if "small" not in self.dbg else 8)
                self.phase1(self.i_xown, self.hT_own, NJ)
            if self.stop == "p1":
                return self.finish()
            with S.scope():
                self.proj_kv()
            with S.scope():
                self.proj_q()
            if self.stop == "p2":
                return self.finish()
            self.attention_and_rest()
            return self.finish()

    def inject(self):
        S = self.S
        self.kT_all = Buf(self.din("kT_in", [NKCH, 128, T], BF16), Tok("kT_in"))
        self.v_all = Buf(self.din("v_in", [3, 4, 128, NTB, 128], BF16), Tok("v_in"))
        self.qT_all = Buf(self.din("qT_in", [NQCH, 128, NOWN], BF16), Tok("qT_in"))
        S.dma("sp", self.wis[:], self.din("wis_in", [128, NJ, 32]).ap()[:, :, :], w=[self.wis])
        S.dma("sp", self.gbs[:], self.din("gbs_in", [128, NJ, 48]).ap()[:, :, :], w=[self.gbs])

    def attention_and_rest(self):
        S = self.S
        self.yT_all = S.dram("yT_all", [32, 128, NOWN], BF16)
        if "yinj" in self.dbg:
            self.yT_all = Buf(self.din("yT_in", [32, 128, NOWN], BF16), Tok("yT_in"))
        else:
            self.prep_tables()
        cum = "cum" in self.dbg
        lvl = ["dsa", "nsa", "merge", "all"].index(self.stop) if (cum and self.stop in ("dsa", "nsa", "merge", "all")) else -1
        if self.stop in ("dsa", "all") or lvl >= 0:
            self.dsa_index()
            self.dsa_attn()
        if self.stop in ("nsa", "all") or lvl >= 1:
            self.nsa_compress()
            self.nsa_attn()
        if self.stop in ("merge", "mp", "all") or lvl >= 2:
            self.merge()
        if self.stop == "peer" and "inj" in self.dbg:
            self.x1_d = Buf(self.din("x1_in", [NOWN, D]), Tok("x1_in"))
            self.hT2_own = S.dram("hT2_own", [4, 128, KC, 512], BF16)
            self.phase1(self.x1_d.t, self.hT2_own, NJ, AB_off=64, src_tok=self.x1_d)
        if self.stop in ("peer", "mp", "all"):
            self.dout("out", [NOWN, D])
            self.peer()
        if "y" in self.dbg:
            o = self.dout("d_yT", [32, 128, 256], BF16)
            S.dma("sp", o.ap()[:, :, :], self.yT_all.t.ap()[:, :, 0:256], r=[self.yT_all],
                  w=[self.outs["d_yT"][1]])

    def finish(self):
        S = self.S
        toks = [tok for (_, tok) in self.outs.values()]
        deps = [t.lw for t in toks if t.lw is not None]
        S._wait("sp", deps)
        S.barrier()
        return self.nc

    def consts(self):
        S = self.S
        self.identf = S.sb("identf", [128, 128], F32)
        self.ident = S.sb("ident", [128, 128], BF16)
        self.ones_bf = S.sb("ones_bf", [128, 128], BF16)
        self.modc = S.sb("modc", [128, 192], F32)
        self.AB = S.sb("AB", [128, 128], F32)
        self.gn = S.sb("gn", [128, 64], F32)
        self.gcol = S.sb("gcol", [128, 6], F32)
        S.op("pool", "memset", self.identf[:], 1.0, w=[self.identf])
        S.op("pool", "affine_select", self.identf[:], self.identf[:], pattern=[[-1, 128]],
             compare_op=ALU.is_equal, fill=0.0, base=0, channel_multiplier=1,
             r=[self.identf], w=[self.identf])
        S.op("dve", "tensor_copy", self.ident[:], self.identf[:], r=[self.identf], w=[self.ident])
        S.op("dve", "memset", self.ones_bf[:], 1.0, w=[self.ones_bf])
        self.wis = S.sb("wis", [128, NJ, 32], F32)
        self.gbs = S.sb("gbs", [128, NJ, 48], F32)
        self.eps128 = S.sb("eps128", [128, 1], F32)
        S.op("dve", "memset", self.eps128[:], 128.0 * EPS, w=[self.eps128])
        S.dma("sp", self.gn[:], self.i_gnT.ap()[:, :], w=[self.gn])
        S.dma("sp", self.gcol[:], self.i_gains.ap()[:, :], w=[self.gcol])
        S.op("dve", "tensor_scalar", self.gcol[:], self.gcol[:], float(np.sqrt(128.0)), None, ALU.mult,
             r=[self.gcol], w=[self.gcol])

    def phase0(self):
        S = self.S
        cT = S.sb("cTs", [128, KC], F32)
        bT = S.sb("bTs", [128, 192], F32)
        S.dma("sp", cT[:], self.i_cT.ap()[:, :], w=[cT])
        S.dma("sp", bT[:], self.i_b_adaT.ap()[:, :], w=[bT])
        ps = S.ps("ps_mod", [128, 192], F32)
        prow = S.psring("ps_row", [1, 512], F32, 2)
        rows = S.ring("modrow", [1, 512], F32, 2)
        one = S.sb("one11", [1, 1], F32)
        S.op("dve", "memset", one[:], 1.0, w=[one])
        wr = S.ring("wada", [128, KC, 512], F32, 2)
        wa = self.i_w_ada.ap().rearrange("(kc p) n -> p kc n", p=128)
        qs = ["sp", "pool"]
        for ct in range(48):
            wt = wr.next()
            for hh in range(2):
                S.dma(qs[hh], wt[:, hh * 16:(hh + 1) * 16, :], wa[:, hh * 16:(hh + 1) * 16, ct * 512:(ct + 1) * 512],
                      w=[wt])
            pr = prow.next()
            for kc in range(KC):
                S.op("pe", "matmul", pr[:], lhsT=cT[:, kc:kc + 1], rhs=wt[:, kc, :],
                     start=(kc == 0), stop=(kc == KC - 1), r=[wt, cT], w=[pr])
            row = rows.next()
            S.op("act", "activation", row[:], pr[:], AF.Identity, r=[pr], w=[row])
            for i in range(4):
                jc = ct * 4 + i
                S.op("pe", "matmul", ps[:, jc:jc + 1], lhsT=row[0:1, i * 128:(i + 1) * 128], rhs=one[0:1, 0:1],
                     start=True, stop=True, r=[row, one], w=[ps])
        S.op("dve", "tensor_tensor", self.modc[:], ps[:], bT[:], ALU.add, r=[ps, bT], w=[self.modc])
        self.mod_to_AB()
        if "modc" in self.dbg:
            o = self.dout("d_modc", [128, 192])
            S.dma("sp", o.ap()[:, :], self.modc[:], r=[self.modc], w=[self.outs["d_modc"][1]])

    def mod_to_AB(self):
        S = self.S
        m, AB, gn = self.modc, self.AB, self.gn
        S.op("dve", "scalar_tensor_tensor", AB[:, 0:32], m[:, 32:64], 1.0, gn[:, 0:32], ALU.add, ALU.mult,
             r=[m, gn], w=[AB])
        S.op("dve", "tensor_copy", AB[:, 32:64], m[:, 0:32], r=[m], w=[AB])
        S.op("dve", "scalar_tensor_tensor", AB[:, 64:96], m[:, 128:160], 1.0, gn[:, 32:64], ALU.add, ALU.mult,
             r=[m, gn], w=[AB])
        S.op("dve", "tensor_copy", AB[:, 96:128], m[:, 96:128], r=[m], w=[AB])

    def norm_hT(self, xt, AB_off, hT, col0, ps_ring, xn_ring, junk, small):
        S = self.S
        ss, rstd = small
        S.op("act", "activation", junk[:], xt[:], AF.Square, accum_out=ss[:], r=[xt], w=[junk, ss])
        S.op("dve", "tensor_scalar", rstd[:], ss[:], 1.0 / D, EPS, ALU.mult, ALU.add, r=[ss], w=[rstd])
        S.op("act", "activation", rstd[:], rstd[:], AF.Sqrt, r=[rstd], w=[rstd])
        S.op("dve", "reciprocal", rstd[:], rstd[:], r=[rstd], w=[rstd])
        xn = xn_ring.next()
        S.op("dve", "tensor_scalar", xn[:], xt[:], rstd[:, 0:1], None, ALU.mult, r=[xt, rstd], w=[xn])
        for k4 in range(KC // 8):
            pt = ps_ring.next()
            for i in range(8):
                kc = k4 * 8 + i
                S.op("pe", "transpose", pt[:, i * 128:(i + 1) * 128], xn[:, kc * 128:(kc + 1) * 128],
                     self.ident[:], r=[xn, self.ident], w=[pt])
            for i in range(8):
                kc = k4 * 8 + i
                A = self.AB[:, AB_off + kc:AB_off + kc + 1]
                B = self.AB[:, AB_off + 32 + kc:AB_off + 32 + kc + 1]
                if i % 2 == 0:
                    S.op("act", "activation", hT[:, kc, col0:col0 + 128], pt[:, i * 128:(i + 1) * 128],
                         AF.Identity, bias=B, scale=A, r=[pt, self.AB], w=[hT])
                else:
                    S.op("dve", "tensor_scalar", hT[:, kc, col0:col0 + 128], pt[:, i * 128:(i + 1) * 128],
                         A, B, ALU.mult, ALU.add, r=[pt, self.AB], w=[hT])

    def phase1(self, xsrc, hdst, nblk, AB_off=0, src_tok=None):
        S = self.S
        with S.scope():
            xr = S.ring("p1x", [128, D], F32, 2)
            xnr = S.ring("p1xn", [128, D], BF16, 2)
            junk = S.sb("p1junk", [128, D], BF16)
            hr = S.ring("p1h", [128, KC, 512], BF16, 2)
            psr = S.psring("p1ps", [128, 1024], BF16, 3)
            smalls = [(S.sb(f"p1ss{i}", [128, 1], F32), S.sb(f"p1rs{i}", [128, 1], F32)) for i in range(2)]
            xa = xsrc.ap()
            for tb in range(nblk):
                if tb % 4 == 0:
                    hT = hr.next()
                xt = xr.next()
                S.dma("sp", xt[:], xa[tb * 128:(tb + 1) * 128, :], r=([src_tok] if src_tok is not None else []),
                      w=[xt])
                self.norm_hT(xt, AB_off, hT, (tb % 4) * 128, psr, xnr, junk, smalls[tb % 2])
                if tb % 4 == 3:
                    S.dma("pool", hdst.t.ap()[tb // 4], hT[:], r=[hT], w=[hdst])

    def load_w(self, wsrc, col0, ncols, wt, q="pool"):
        S = self.S
        wa = wsrc.ap()
        for kc in range(KC):
            S.dma(q, wt[:, kc, 0:ncols], wa[kc * 128:(kc + 1) * 128, col0:col0 + ncols], w=[wt])

    def evac_T(self, kind, ps, ot, n, tmp):
        S = self.S
        if kind == "c":
            S.op("act", "activation", ot[:, 0:n], ps[:, 0:n], AF.Identity, r=[ps], w=[ot])
        elif kind == "s":
            S.op("act", "activation", ot[:, 0:n], ps[:, 0:n], AF.Sigmoid, r=[ps], w=[ot])
        else:
            gi = int(kind[1:])
            sq, ps2, rr = tmp
            S.op("act", "activation", sq[:, 0:n], ps[:, 0:n], AF.Square, r=[ps], w=[sq])
            S.op("pe", "matmul", ps2[:, 0:n], lhsT=self.ones_bf[:], rhs=sq[:, 0:n], start=True, stop=True,
                 r=[sq, self.ones_bf], w=[ps2])
            S.op("act", "activation", rr[:, 0:n], ps2[:, 0:n], AF.Sqrt, bias=self.eps128[:, 0:1],
                 r=[ps2, self.eps128], w=[rr])
            S.op("dve", "reciprocal", rr[:, 0:n], rr[:, 0:n], r=[rr], w=[rr])
            S.op("dve", "scalar_tensor_tensor", ot[:, 0:n], ps[:, 0:n], self.gcol[:, gi:gi + 1], rr[:, 0:n],
                 ALU.mult, ALU.mult, r=[ps, self.gcol, rr], w=[ot])

    def proj_kv(self):
        S = self.S
        ntile = 32 if "small" not in self.dbg else 2
        hr = S.ring("kvh", [128, KC, 512], BF16, 2)
        wk = S.sb("kvwk", [128, KC, 7 * 128], BF16)
        wv = S.sb("kvwv", [128, KC, 512], BF16)
        psr = S.psring("kvps", [128, 512], F32, 4)
        ps2r = S.psring("kvps2", [128, 512], F32, 2)
        otr = S.ring("kvot", [128, 512], BF16, 4)
        sqr = S.ring("kvsq", [128, 512], BF16, 2)
        rrr = S.ring("kvrr", [128, 512], F32, 2)
        kTa = self.kT_all.t.ap()
        va = self.v_all.t.ap()
        for p in range(3):
            self.load_w(self.i_w_kT, p * 7 * 128, 7 * 128, wk)
            self.load_w(self.i_w_v, p * 512, 512, wv)
            for tt in range(ntile):
                hT = hr.next()
                S.dma("sp", hT[:], self.hT_all.t.ap()[tt], r=[self.hT_all], w=[hT])
                for ci in range(7):
                    ch = p * 7 + ci
                    ps = psr.next()
                    for kc in range(KC):
                        S.op("pe", "matmul", ps[:], lhsT=wk[:, kc, ci * 128:(ci + 1) * 128], rhs=hT[:, kc, :],
                             start=(kc == 0), stop=(kc == KC - 1), r=[wk, hT], w=[ps])
                    ot = otr.next()
                    self.evac_T(KCH[ch], ps, ot, 512, (sqr.next(), ps2r.next(), rrr.next()))
                    S.dma("sp", kTa[ch, :, tt * 512:(tt + 1) * 512], ot[:], r=[ot], w=[self.kT_all])
                for tb in range(4):
                    ps = psr.next()
                    for kc in range(KC):
                        S.op("pe", "matmul", ps[:], lhsT=hT[:, kc, tb * 128:(tb + 1) * 128], rhs=wv[:, kc, :],
                             start=(kc == 0), stop=(kc == KC - 1), r=[wv, hT], w=[ps])
                    ot = otr.next()
                    S.op("dve", "tensor_copy", ot[:], ps[:], r=[ps], w=[ot])
                    blk = tt * 4 + tb
                    S.dma("sp", va[p, :, :, blk, :].rearrange("g p d -> p g d"),
                          ot[:].rearrange("p (g d) -> p g d", g=4), r=[ot], w=[self.v_all])
        if "kv" in self.dbg:
            o = self.dout("d_kT", [NKCH, 128, 1024], BF16)
            S.dma("sp", o.ap()[:, :, :], kTa[:, :, 0:1024], r=[self.kT_all], w=[self.outs["d_kT"][1]])
            o = self.dout("d_v", [3, 4, 128, 8, 128], BF16)
            for i3 in range(3):
                S.dma("sp", o.ap()[i3], va[i3, :, :, 0:8, :], r=[self.v_all], w=[self.outs["d_v"][1]])

    def proj_q(self):
        S = self.S
        hT4 = S.sb("qh", [128, 4, KC, 512], BF16) if False else None
        hts = [S.sb(f"qh{i}", [128, KC, 512], BF16) for i in range(2)]
        G = 4
        wr = S.ring("qw", [128, KC, G * 128], BF16, 2)
        psr = S.psring("qps", [128, 512], F32, 4)
        ps2r = S.psring("qps2", [128, 512], F32, 2)
        otr = S.ring("qot", [128, 512], BF16, 4)
        sqr = S.ring("qsq", [128, 512], BF16, 2)
        rrr = S.ring("qrr", [128, 512], F32, 2)
        qTa = self.qT_all.t.ap()
        ngrp = NQCH // G if "small" not in self.dbg else 2
        wqs = S.sb("qwqs", [128, KC, 80], BF16)
        self.load_w(self.i_w_qs, 0, 80, wqs)
        for half in range(2):
            for i in range(2):
                S.dma("sp", hts[i][:], self.hT_own.t.ap()[half * 2 + i], r=[self.hT_own], w=[hts[i]])
            for i in range(2):
                for tb in range(4):
                    if "noqs" in self.dbg:
                        continue
                    j = (half * 2 + i) * 4 + tb
                    ps = psr.next()
                    for kc in range(KC):
                        S.op("pe", "matmul", ps[:, 0:80], lhsT=hts[i][:, kc, tb * 128:(tb + 1) * 128],
                             rhs=wqs[:, kc, :], start=(kc == 0), stop=(kc == KC - 1), r=[wqs, hts[i]], w=[ps])
                    S.op("act", "activation", self.wis[:, j, :], ps[:, 0:32], AF.Identity,
                         scale=float(1.0 / (8.0 * np.sqrt(32.0))), r=[ps], w=[self.wis])
                    S.op("act", "activation", self.gbs[:, j, :], ps[:, 32:80], AF.Sigmoid, r=[ps], w=[self.gbs])
            for g in range(ngrp):
                wt = wr.next()
                self.load_w(self.i_w_qT, g * G * 128, G * 128, wt)
                for i in range(2):
                    tt = half * 2 + i
                    for ci in range(G):
                        ch = g * G + ci
                        ps = psr.next()
                        for kc in range(KC):
                            S.op("pe", "matmul", ps[:], lhsT=wt[:, kc, ci * 128:(ci + 1) * 128],
                                 rhs=hts[i][:, kc, :], start=(kc == 0), stop=(kc == KC - 1),
                                 r=[wt, hts[i]], w=[ps])
                        ot = otr.next()
                        self.evac_T(QCH[ch], ps, ot, 512, (sqr.next(), ps2r.next(), rrr.next()))
                        S.dma("sp", qTa[ch, :, tt * 512:(tt + 1) * 512], ot[:], r=[ot], w=[self.qT_all])
        if "q" in self.dbg:
            o = self.dout("d_qT", [NQCH, 128, 512], BF16)
            S.dma("sp", o.ap()[:, :, :], qTa[:, :, 0:512], r=[self.qT_all], w=[self.outs["d_qT"][1]])


    def prep_tables(self):
        S = self.S
        self.tabs = {}
        with S.scope():
            raw = S.ring("tbraw", [128, 4, 512], F32, 2)
            et = S.ring("tbe", [128, 4, 512], BF16, 2)
            for name, nt in (("dsa", 21), ("slc", 21), ("win", 12), ("cmp", 5)):
                src = self.din("tab_" + name, [nt, 4, 128, 512])
                dst = S.dram("tabe_" + name, [nt, 4, 128, 512], BF16)
                self.tabs[name] = dst
                for t in range(nt):
                    r = raw.next()
                    S.dma("sp", r[:], src.ap()[t].rearrange("g p n -> p g n"), w=[r])
                    e = et.next()
                    S.op("act", "activation", e[:], r[:], AF.Exp, r=[r], w=[e])
                    S.dma("pool", dst.t.ap()[t].rearrange("g p n -> p g n"), e[:], r=[e], w=[dst])

    def load_tab(self, name, g, tab, nt):
        S = self.S
        src = self.tabs[name]
        S.dma("pool", tab[:, 0:nt, :], src.t.ap()[:, g].rearrange("o p n -> p o n"), r=[src], w=[tab])

    def dsa_index(self):
        S = self.S
        nj = NJ if "small" not in self.dbg else 2
        self.maskT_d = S.dram("maskT_d", [NJ, 128, 128, 128], BF16)
        with S.scope():
            score = S.sb("ix_score", [128, T], F32)
            junk = S.sb("ix_junk", [128, 4096], BF16)
            mst = S.sb("ix_mst", [128, 128, 128], BF16)
            qi = S.ring("ix_qi", [128, 16, 128], BF16, 2)
            dh = S.ring("ix_dh", [128, 32, 128], BF16, 2)
            kir = S.ring("ix_ki", [128, 2048], BF16, 2)
            rl = S.ring("ix_rl", [128, 512], BF16, 6)
            cm = S.sb("ix_cm", [128, 1024], F32)
            jf = S.sb("ix_jf", [128, 1024], F32)
            half = S.sb("ix_half", [128, 1], F32)
            sm = {k: S.sb("ix_" + k, [128, 1], F32) for k in ("lo", "hi", "mid", "cnt", "pred", "d1", "d2", "t1", "t2")}
            ps_s = S.psring("ix_pss", [128, 512], F32, 4)
            ps_c = S.psring("ix_psc", [128, 512], F32, 2)
            ps_t = S.psring("ix_pst", [128, 1024], BF16, 2)
            S.dma("sp", cm[:], self.din("cmask", [128, 1024]).ap()[:, :], w=[cm])
            S.op("dve", "memset", half[:], 0.5, w=[half])
            qTa = self.qT_all.t.ap()
            kia = self.kT_all.t.ap()[4]
            for j in range(nj):
                nkb = 8 * j + 8
                Tk = nkb * 128
                q = qi.next()
                S.dma("sp", q[:], qTa[16:32, :, j * 128:(j + 1) * 128].rearrange("c p q -> p c q"),
                      r=[self.qT_all], w=[q])
                d = dh.next()
                for h in range(32):
                    S.op("act", "activation", d[:, h, :], self.ident[:], AF.Identity,
                         scale=self.wis[:, j, h:h + 1], r=[self.ident, self.wis], w=[d])
                items = [(kt, h) for kt in range(nkb // 4) for h in range(32)]
                kis = {}
                pcs = {}
                rls = {}

                def ix_a(kt, h):
                    if h == 0 and kt % 4 == 0:
                        ki = kir.next()
                        n = min(2048, Tk - kt * 512)
                        S.dma("sp", ki[:, 0:n], kia[:, kt * 512:kt * 512 + n], r=[self.kT_all], w=[ki])
                        kis[kt // 4] = ki
                    ki = kis[kt // 4]
                    ko = (kt % 4) * 512
                    pb = (h % 2) * 64
                    ps = ps_s.next()
                    S.op("pe", "matmul", ps[:], lhsT=q[pb:pb + 64, h // 2, :], rhs=ki[pb:pb + 64, ko:ko + 512],
                         start=True, stop=True, r=[q, ki], w=[ps])
                    r = rl.next()
                    if h % 2 == 0:
                        S.op("act", "activation", r[:], ps[:], AF.Relu, r=[ps], w=[r])
                    else:
                        S.op("dve", "tensor_scalar", r[:], ps[:], 0.0, None, ALU.max, r=[ps], w=[r])
                    rls[(kt, h)] = r

                def ix_b(kt, h):
                    if h == 0:
                        pcs[kt] = ps_c.next()
                    pc = pcs[kt]
                    r = rls.pop((kt, h))
                    S.op("pe", "matmul", pc[:], lhsT=d[:, h, :], rhs=r[:], start=(h == 0), stop=(h == 31),
                         r=[d, r], w=[pc])
                    if h == 31:
                        if kt >= nkb // 4 - 2:
                            co = (kt - (nkb // 4 - 2)) * 512
                            S.op("dve", "tensor_tensor", score[:, kt * 512:(kt + 1) * 512], pc[:], cm[:, co:co + 512],
                                 ALU.add, r=[pc, cm], w=[score])
                        else:
                            S.op("dve", "tensor_copy", score[:, kt * 512:(kt + 1) * 512], pc[:],
                                 r=[pc], w=[score])

                LAI = 3
                for n in range(len(items) + LAI):
                    if n < len(items):
                        ix_a(*items[n])
                    if n >= LAI:
                        ix_b(*items[n - LAI])
                lo, hi, mid, cnt, pred = sm["lo"], sm["hi"], sm["mid"], sm["cnt"], sm["pred"]
                d1, d2, t1, t2 = sm["d1"], sm["d2"], sm["t1"], sm["t2"]
                S.op("dve", "tensor_tensor", jf[:], score[:, Tk - 1024:Tk], cm[:], ALU.subtract,
                     r=[score, cm], w=[jf])
                S.op("dve", "tensor_reduce", t1[:], jf[:], AX.X, ALU.min, r=[jf], w=[t1])
                if Tk > 1024:
                    S.op("dve", "tensor_reduce", t2[:], score[:, 0:Tk - 1024], AX.X, ALU.min, r=[score], w=[t2])
                    S.op("dve", "tensor_tensor", lo[:], t1[:], t2[:], ALU.min, r=[t1, t2], w=[lo])
                else:
                    S.op("dve", "tensor_copy", lo[:], t1[:], r=[t1], w=[lo])
                S.op("dve", "tensor_reduce", hi[:], score[:, 0:Tk], AX.X, ALU.max, r=[score], w=[hi])
                S.op("dve", "tensor_scalar", hi[:], hi[:], 1.0, None, ALU.add, r=[hi], w=[hi])
                for it in range(24):
                    S.op("dve", "scalar_tensor_tensor", mid[:], lo[:], hi[:, 0:1], half[:], ALU.add, ALU.mult,
                         r=[lo, hi, half], w=[mid])
                    nch = (Tk + 4095) // 4096
                    for ci in range(nch):
                        c0 = ci * 4096
                        n = min(4096, Tk - c0)
                        init = 0.0 if ci == 0 else cnt[:, 0:1]
                        S.op("dve", "tensor_scalar", junk[:, 0:n], score[:, c0:c0 + n], mid[:, 0:1], init,
                             ALU.is_ge, ALU.add, accum_out=cnt[:], r=[score, mid, cnt], w=[junk, cnt])
                    S.op("dve", "tensor_scalar", pred[:], cnt[:], 255.5, None, ALU.is_ge, r=[cnt], w=[pred])
                    S.op("dve", "tensor_tensor", d1[:], mid[:], lo[:], ALU.subtract, r=[mid, lo], w=[d1])
                    S.op("dve", "tensor_tensor", d2[:], hi[:], mid[:], ALU.subtract, r=[mid, hi], w=[d2])
                    S.op("dve", "scalar_tensor_tensor", lo[:], d1[:], pred[:, 0:1], lo[:], ALU.mult, ALU.add,
                         r=[d1, pred, lo], w=[lo])
                    S.op("dve", "scalar_tensor_tensor", hi[:], d2[:], pred[:, 0:1], mid[:], ALU.mult, ALU.add,
                         r=[d2, pred, mid], w=[hi])
                for c8 in range(nkb // 8):
                    S.op("dve", "tensor_scalar", junk[:, 0:1024], score[:, c8 * 1024:(c8 + 1) * 1024], lo[:, 0:1],
                         -30000.0, ALU.is_lt, ALU.mult, r=[score, lo], w=[junk])
                    pt = ps_t.next()
                    for i in range(8):
                        S.op("pe", "transpose", pt[:, i * 128:(i + 1) * 128], junk[:, i * 128:(i + 1) * 128],
                             self.ident[:], r=[junk, self.ident], w=[pt])
                    S.op("act", "activation", mst[:, c8 * 8:(c8 + 1) * 8, :].rearrange("p a b -> p (a b)"), pt[:],
                         AF.Identity, r=[pt], w=[mst])
                S.dma("sp", self.maskT_d.t.ap()[j, :, 0:nkb, :], mst[:, 0:nkb, :], r=[mst], w=[self.maskT_d])
            if "ix" in self.dbg:
                o = self.dout("d_score", [128, 2048])
                S.dma("sp", o.ap()[:, :], score[:, 0:2048], r=[score], w=[self.outs["d_score"][1]])
                o = self.dout("d_lo", [128, 1])
                S.dma("sp", o.ap()[:, :], sm["lo"][:], r=[sm["lo"]], w=[self.outs["d_lo"][1]])

    def attn(self, P, qT, qtok, kT_dram, ksrc, v_dram, vsrc, kb0, kb1, tab, tid_fn, mask_fn, keep=None):
        S = self.S
        oT = P["oT"].next()
        den = P["den"].next()
        scale = float(128.0 ** -0.5)
        LA = 2
        nblk = kb1 - kb0
        chunks = {}

        def load_chunk(ci):
            c0 = kb0 + ci * 16
            if c0 >= kb1 or ci in chunks:
                return
            nb = min(16, kb1 - c0)
            kt = P["kt"].next()
            vt = P["vt"].next()
            S.dma("sp", kt[:, 0:nb * 128], kT_dram[:, c0 * 128:(c0 + nb) * 128], r=[ksrc], w=[kt])
            S.dma("sp", vt[:, 0:nb, :], v_dram[:, c0:c0 + nb, :], r=[vsrc], w=[vt])
            chunks[ci] = (kt, vt)

        ptiles = {}

        def stage_a(n):
            kb = kb0 + n
            ci, i = divmod(n, 16)
            if i == 0:
                load_chunk(ci)
                load_chunk(ci + 1)
            kt, vt = chunks[ci]
            psl = P["psl"].next()
            S.op("pe", "matmul", psl[:], lhsT=kt[:, i * 128:(i + 1) * 128], rhs=qT, start=True,
                 stop=(mask_fn is None), r=[kt, qtok], w=[psl])
            if mask_fn is not None:
                mlhs, mrhs, mtoks = mask_fn(kb)
                S.op("pe", "matmul", psl[:].rearrange("p (h q) -> p h q", h=4), lhsT=mlhs,
                     rhs=mrhs.unsqueeze(1).broadcast_to([128, 4, 128]), start=False, stop=True,
                     r=mtoks, w=[psl])
            e = P["e"].next()
            S.op("act", "activation", e[:], psl[:], AF.Exp, scale=scale, r=[psl], w=[e])
            p = (keep if keep is not None else P["p"]).next()
            S.op("dve", "tensor_tensor", p[:], e[:], tab[:, tid_fn(kb), :], ALU.mult, r=[e, tab], w=[p])
            ptiles[n] = (p, vt, i)

        def stage_b(n):
            p, vt, i = ptiles.pop(n)
            S.op("pe", "matmul", oT[:], lhsT=vt[:, i, :], rhs=p[:], start=(n == 0), stop=(n == nblk - 1),
                 r=[vt, p], w=[oT])
            S.op("pe", "matmul", den[:], lhsT=self.ones_bf[:], rhs=p[:], start=(n == 0), stop=(n == nblk - 1),
                 r=[p, self.ones_bf], w=[den])

        for n in range(nblk + LA):
            if n < nblk:
                stage_a(n)
            if n >= LA:
                stage_b(n - LA)
        return oT, den

    def attn_rings(self, pre):
        S = self.S
        return {
            "kt": S.ring(pre + "kt", [128, 2048], BF16, 3),
            "vt": S.ring(pre + "vt", [128, 16, 128], BF16, 3),
            "psl": S.psring(pre + "psl", [128, 512], F32, 3),
            "e": S.ring(pre + "e", [128, 512], BF16, 3),
            "p": S.ring(pre + "p", [128, 512], BF16, 5),
            "oT": S.psring(pre + "oT", [128, 512], F32, 2),
            "den": S.psring(pre + "den", [128, 512], F32, 1),
        }

    def load_qT(self, qr, base, g, j):
        S = self.S
        qt = qr.next()
        S.dma("sp", qt[:], self.qT_all.t.ap()[base + 4 * g:base + 4 * g + 4, :, j * 128:(j + 1) * 128]
              .rearrange("h p q -> p h q"), r=[self.qT_all], w=[qt])
        return qt

    def store_y(self, y, hbase, g, j):
        S = self.S
        S.dma("sp", self.yT_all.t.ap()[hbase + 4 * g:hbase + 4 * g + 4, :, j * 128:(j + 1) * 128]
              .rearrange("h p q -> p h q"), y[:].rearrange("p (h q) -> p h q", h=4), r=[y], w=[self.yT_all])

    def dsa_attn(self):
        S = self.S
        nj = NJ if "small" not in self.dbg else 2
        with S.scope():
            P = self.attn_rings("da_")
            tab = S.sb("da_tab", [128, 21, 512], BF16)
            mT = S.ring("da_mT", [128, 128, 128], BF16, 1)
            qr = S.ring("da_q", [128, 4, 128], BF16, 2)
            rdr = S.ring("da_rd", [128, 512], F32, 2)
            yr = S.ring("da_y", [128, 512], BF16, 2)
            for g in range(4):
                self.load_tab("dsa", g, tab, 21)
                for j in range(nj):
                    nkb = 8 * j + 8
                    m = mT.next()
                    S.dma("pool", m[:, 0:nkb, :], self.maskT_d.t.ap()[j, :, 0:nkb, :], r=[self.maskT_d], w=[m])
                    qt = self.load_qT(qr, 0, g, j)
                    oT, den = self.attn(P, qt[:].rearrange("p h q -> p (h q)"), qt,
                                        self.kT_all.t.ap()[g], self.kT_all, self.v_all.t.ap()[0, g], self.v_all,
                                        0, nkb, tab, lambda kb, j=j: min(8 * j + 7 - kb, 20),
                                        lambda kb, m=m: (self.ident[:], m[:, kb, :], [m, self.ident]))
                    rd = rdr.next()
                    S.op("dve", "tensor_scalar", rd[:], den[:], 1e-30, None, ALU.max, r=[den], w=[rd])
                    S.op("dve", "reciprocal", rd[:], rd[:], r=[rd], w=[rd])
                    y = yr.next()
                    S.op("dve", "tensor_tensor", y[:], oT[:], rd[:], ALU.mult, r=[oT, rd], w=[y])
                    self.store_y(y, 0, g, j)


    def gelu_tanh(self, out, x, n, tmp):
        S = self.S
        S.op("dve", "tensor_tensor", tmp[:, 0:n], x[:, 0:n], x[:, 0:n], ALU.mult, r=[x], w=[tmp])
        S.op("dve", "tensor_scalar", tmp[:, 0:n], tmp[:, 0:n], 0.044715, 1.0, ALU.mult, ALU.add, r=[tmp], w=[tmp])
        S.op("dve", "tensor_tensor", tmp[:, 0:n], tmp[:, 0:n], x[:, 0:n], ALU.mult, r=[tmp, x], w=[tmp])
        S.op("act", "activation", tmp[:, 0:n], tmp[:, 0:n], AF.Sigmoid, scale=1.5957691216057308, r=[tmp], w=[tmp])
        S.op("dve", "tensor_tensor", out[:, 0:n], tmp[:, 0:n], x[:, 0:n], ALU.mult, r=[tmp, x], w=[out])

    def nsa_compress(self):
        S = self.S
        self.kcT_d = S.dram("kcT_d", [4, 128, 1024], BF16)
        self.vc_d = S.dram("vc_d", [4, 128, 8, 128], BF16)
        with S.scope():
            w1 = S.sb("cp_w1", [128, 32, 256], BF16)
            w2 = S.sb("cp_w2", [128, 2, 128], BF16)
            peT = S.sb("cp_pe", [128, 32], BF16)
            pb = S.sb("cp_pb", [128, 2], F32)
            xr = S.ring("cp_x", [128, 8208], BF16, 2)
            hx = S.ring("cp_hx", [128, 512], F32, 2)
            tmpr = S.ring("cp_tmp", [128, 512], F32, 2)
            hid = [S.sb(f"cp_hid{i}", [128, 512], BF16) for i in range(2)]
            otr = S.ring("cp_ot", [128, 512], BF16, 2)
            sqr = S.ring("cp_sq", [128, 512], BF16, 1)
            rrr = S.ring("cp_rr", [128, 512], F32, 1)
            psr = S.psring("cp_ps", [128, 512], F32, 3)
            ps2r = S.psring("cp_ps2", [128, 512], F32, 1)
            psb = S.ps("cp_psb", [128, 2], F32)
            for kv in range(2):
                sfx = "k" if kv == 0 else "v"
                w1src = self.din("cmp_w1_" + sfx, [32, 128, 256])
                w2src = self.din("cmp_w2_" + sfx, [256, 128])
                pesrc = self.din("cmp_peT_" + sfx, [128, 32])
                S.dma("pool", w1[:], w1src.ap().rearrange("l d e -> d l e"), w=[w1])
                S.dma("pool", w2[:], w2src.ap().rearrange("(c e) d -> e c d", c=2), w=[w2])
                S.dma("pool", peT[:], pesrc.ap()[:, :], w=[peT])
                for ec in range(2):
                    for l in range(32):
                        S.op("pe", "matmul", psb[:, ec:ec + 1], lhsT=w1[:, l, ec * 128:(ec + 1) * 128],
                             rhs=peT[:, l:l + 1], start=(l == 0), stop=(l == 31), r=[w1, peT], w=[psb])
                S.op("dve", "tensor_copy", pb[:], psb[:], r=[psb], w=[pb])
                for g in range(4):
                    ch = (5 if kv == 0 else 9) + g
                    for nt in range(2):
                        n0 = nt * 512
                        nn = 512 if nt == 0 else 511
                        xt = xr.next()
                        S.dma("sp", xt[:, 0:16 * nn + 16], self.kT_all.t.ap()[ch, :, 16 * n0:16 * n0 + 16 * nn + 16],
                              r=[self.kT_all], w=[xt])
                        xv = xt[:, 0:8208].rearrange("p (n s) -> p n s", s=16)
                        for ec in range(2):
                            ps = psr.next()
                            for l in range(32):
                                S.op("pe", "matmul", ps[:, 0:nn], lhsT=w1[:, l, ec * 128:(ec + 1) * 128],
                                     rhs=xv[:, l // 16:l // 16 + nn, l % 16], start=(l == 0), stop=(l == 31),
                                     r=[w1, xt], w=[ps])
                            x32 = hx.next()
                            S.op("act", "activation", x32[:, 0:nn], ps[:, 0:nn], AF.Identity, bias=pb[:, ec:ec + 1],
                                 r=[ps, pb], w=[x32])
                            self.gelu_tanh(hid[ec], x32, nn, tmpr.next())
                        if kv == 0:
                            ps = psr.next()
                            for ec in range(2):
                                S.op("pe", "matmul", ps[:, 0:nn], lhsT=w2[:, ec, :], rhs=hid[ec][:, 0:nn],
                                     start=(ec == 0), stop=(ec == 1), r=[w2, hid[ec]], w=[ps])
                            ot = otr.next()
                            S.op("pool", "memset", ot[:], 0.0, w=[ot])
                            self.evac_T("n3", ps, ot, nn, (sqr.next(), ps2r.next(), rrr.next()))
                            S.dma("sp", self.kcT_d.t.ap()[g, :, n0:n0 + 512], ot[:], r=[ot], w=[self.kcT_d])
                        else:
                            ot = otr.next()
                            S.op("pool", "memset", ot[:], 0.0, w=[ot])
                            ps = psr.next()
                            for nb in range(4):
                                m = min(128, nn - nb * 128)
                                for ec in range(2):
                                    S.op("pe", "matmul", ps[0:m, nb * 128:(nb + 1) * 128],
                                         lhsT=hid[ec][:, nb * 128:nb * 128 + m], rhs=w2[:, ec, :],
                                         start=(ec == 0), stop=(ec == 1), r=[w2, hid[ec]], w=[ps])
                            for nb in range(4):
                                m = min(128, nn - nb * 128)
                                S.op("act", "activation", ot[0:m, nb * 128:(nb + 1) * 128],
                                     ps[0:m, nb * 128:(nb + 1) * 128], AF.Identity, r=[ps], w=[ot])
                            S.dma("sp", self.vc_d.t.ap()[g, :, nt * 4:(nt + 1) * 4, :],
                                  ot[:].rearrange("p (b d) -> p b d", b=4), r=[ot], w=[self.vc_d])
            if "cmp" in self.dbg:
                o = self.dout("d_kcT", [4, 128, 1024], BF16)
                S.dma("sp", o.ap()[:, :, :], self.kcT_d.t.ap()[:, :, :], r=[self.kcT_d], w=[self.outs["d_kcT"][1]])
                o = self.dout("d_vc", [4, 128, 8, 128], BF16)
                S.dma("sp", o.ap()[:, :, :, :], self.vc_d.t.ap()[:, :, :, :], r=[self.vc_d], w=[self.outs["d_vc"][1]])

    def nsa_attn(self):
        S = self.S
        nj = NJ if "small" not in self.dbg else 2
        with S.scope():
            P = self.attn_rings("na_")
            keep = S.ring("na_keep", [128, 512], BF16, 9)
            tab_s = S.sb("na_tabs", [128, 21, 512], BF16)
            tab_w = S.sb("na_tabw", [128, 12, 512], BF16)
            tab_c = S.sb("na_tabc", [128, 5, 512], BF16)
            qr = S.ring("na_q", [128, 4, 128], BF16, 2)
            ftab = S.sb("na_ftab", [128, NJ, 256], F32)
            ov = S.sb("na_ov", [128, 8, 256], BF16)
            yexp = S.sb("na_yexp", [128, 8192], BF16)
            S.dma("sp", ftab[:], self.din("ftab", [NJ, 128, 256]).ap().rearrange("j p m -> p j m"), w=[ftab])
            S.dma("pool", ov[:], self.din("ov", [1024, 256]).ap().rearrange("(c p) m -> p c m", p=128), w=[ov])
            S.dma("pool", yexp[:], self.din("yexp", [128, 8192]).ap()[:, :], w=[yexp])
            rdr = S.ring("na_rd", [128, 512], F32, 2)
            Rr = S.ring("na_R", [128, 512], F32, 2)
            yacc = S.sb("na_yacc", [128, 512], F32)
            ytmp = S.sb("na_ytmp", [128, 512], F32)
            yr = S.ring("na_y", [128, 512], BF16, 2)
            pnr = S.ring("na_pn", [128, 512], BF16, 2)
            rg = S.ring("na_rg", [128, 512], BF16, 2)
            imp = S.sb("na_imp", [128, 256], F32)
            imp3 = S.sb("na_imp3", [128, 256], F32)
            m8 = S.sb("na_m8", [128, 16], F32)
            mb = S.sb("na_mb", [128, 256], BF16)
            mbT = S.sb("na_mbT", [128, 2, 128], BF16)
            mkr = S.ring("na_mk", [128, 128], BF16, 3)
            psm = S.psring("na_psm", [128, 512], F32, 1)
            pst = S.ps("na_pst", [128, 1024], BF16)

            def finish_branch(br, oT, den, g, j):
                r = rg.next()
                for h in range(4):
                    col = (4 * g + h) * 3 + br
                    S.op("dve", "tensor_scalar", r[:, h * 128:(h + 1) * 128], self.ident[:],
                         self.gbs[:, j, col:col + 1], None, ALU.mult, r=[self.ident, self.gbs], w=[r])
                pg = psm.next()
                S.op("pe", "matmul", pg[:], lhsT=self.ones_bf[:], rhs=r[:], start=True, stop=True,
                     r=[r, self.ones_bf], w=[pg])
                rd = rdr.next()
                S.op("dve", "tensor_scalar", rd[:], den[:], 1e-30, None, ALU.max, r=[den], w=[rd])
                S.op("dve", "reciprocal", rd[:], rd[:], r=[rd], w=[rd])
                R = Rr.next()
                S.op("dve", "tensor_tensor", R[:], rd[:], pg[:], ALU.mult, r=[rd, pg], w=[R])
                if br == 0:
                    S.op("dve", "tensor_tensor", yacc[:], oT[:], R[:], ALU.mult, r=[oT, R], w=[yacc])
                else:
                    S.op("dve", "tensor_tensor", ytmp[:], oT[:], R[:], ALU.mult, r=[oT, R], w=[ytmp])
                    S.op("dve", "tensor_tensor", yacc[:], yacc[:], ytmp[:], ALU.add, r=[yacc, ytmp], w=[yacc])
                return rd

            for g in range(4):
                self.load_tab("slc", g, tab_s, 21)
                self.load_tab("win", g, tab_w, 12)
                self.load_tab("cmp", g, tab_c, 5)
                for j in range(nj):
                    qt = self.load_qT(qr, 32, g, j)
                    qv = qt[:].rearrange("p h q -> p (h q)")
                    ncc = j // 2 + 1
                    keep.i = 0
                    oT, den = self.attn(P, qv, qt, self.kcT_d.t.ap()[g], self.kcT_d, self.vc_d.t.ap()[g], self.vc_d,
                                        0, ncc, tab_c, lambda kb, j=j: min(j - 2 * kb, 4), None, keep=keep)
                    rd = finish_branch(0, oT, den, g, j)
                    pi = psm.next()
                    for cc in range(ncc):
                        pn = pnr.next()
                        S.op("dve", "tensor_tensor", pn[:], keep.bufs[cc][:], rd[:], ALU.mult,
                             r=[keep.bufs[cc], rd], w=[pn])
                        for h in range(4):
                            S.op("pe", "matmul", pi[:, 0:256], lhsT=pn[:, h * 128:(h + 1) * 128], rhs=ov[:, cc, :],
                                 start=(cc == 0 and h == 0), stop=(cc == ncc - 1 and h == 3), r=[pn, ov], w=[pi])
                    S.op("dve", "tensor_tensor", imp[:], pi[:, 0:256], ftab[:, j, :], ALU.add, r=[pi, ftab], w=[imp])
                    S.op("dve", "max", m8[:, 0:8], imp[:], r=[imp], w=[m8])
                    S.op("dve", "match_replace", imp3[:], m8[:, 0:8], imp[:], -3.0e38, r=[m8, imp], w=[imp3])
                    S.op("dve", "max", m8[:, 8:16], imp3[:], r=[imp3], w=[m8])
                    S.op("dve", "tensor_scalar", mb[:], imp[:], m8[:, 15:16], -30000.0, ALU.is_lt, ALU.mult,
                         r=[imp, m8], w=[mb])
                    for hh in range(2):
                        S.op("pe", "transpose", pst[:, hh * 128:(hh + 1) * 128], mb[:, hh * 128:(hh + 1) * 128],
                             self.ident[:], r=[mb, self.ident], w=[pst])
                    S.op("act", "activation", mbT[:].rearrange("p a b -> p (a b)"), pst[:, 0:256], AF.Identity,
                         r=[pst], w=[mbT])

                    def slc_mask(kb):
                        return (yexp[:, (kb % 64) * 128:(kb % 64 + 1) * 128], mbT[:, kb // 64, :], [yexp, mbT])

                    nkb = 8 * j + 8
                    oT, den = self.attn(P, qv, qt, self.kT_all.t.ap()[13 + g], self.kT_all,
                                        self.v_all.t.ap()[1, g], self.v_all, 0, nkb, tab_s,
                                        lambda kb, j=j: min(8 * j + 7 - kb, 20), slc_mask)
                    finish_branch(1, oT, den, g, j)
                    kb0 = max(0, 8 * j - 4)
                    oT, den = self.attn(P, qv, qt, self.kT_all.t.ap()[17 + g], self.kT_all,
                                        self.v_all.t.ap()[2, g], self.v_all, kb0, nkb, tab_w,
                                        lambda kb, j=j: kb - (8 * j - 4), None)
                    finish_branch(2, oT, den, g, j)
                    y = yr.next()
                    S.op("act", "activation", y[:], yacc[:], AF.Identity, r=[yacc], w=[y])
                    self.store_y(y, 16, g, j)


    def row_bcast(self, dst, col0):
        S = self.S
        with S.scope():
            tl = S.ring("rb_l", [128, 128], F32, 2)
            ps = S.psring("rb_ps", [128, 512], F32, 2)
            onesf = S.sb("rb_ones", [128, 128], F32)
            S.op("dve", "memset", onesf[:], 1.0, w=[onesf])
            for k4 in range(8):
                p = ps.next()
                for i in range(4):
                    kc = k4 * 4 + i
                    t = tl.next()
                    S.op("dve", "tensor_scalar", t[:], onesf[:], self.modc[:, col0 + kc:col0 + kc + 1], None, ALU.mult,
                         r=[onesf, self.modc], w=[t])
                    S.op("pe", "matmul", p[:, i * 128:(i + 1) * 128], lhsT=t[:], rhs=self.identf[:], start=True,
                         stop=True, r=[t, self.identf], w=[p])
                S.op("act", "activation", dst[:, k4 * 512:(k4 + 1) * 512], p[:], AF.Identity, r=[p], w=[dst])

    def merge(self):
        S = self.S
        self.x1_d = S.dram("x1_d", [NOWN, D], F32)
        self.mT_d = S.dram("mT_d", [32, 128, NOWN], BF16)
        self.hT2_own = S.dram("hT2_own", [4, 128, KC, 512], BF16)
        wa_src = self.din("w_branch_a", [2048, D]).ap().rearrange("(h p) n -> p h n", p=128)
        wb_src = self.din("w_branch_b", [2048, D]).ap().rearrange("(h p) n -> p h n", p=128)
        ya = self.yT_all.t.ap()
        qTa = self.qT_all.t.ap()
        with S.scope():
            wa = S.sb("mg_wa", [128, 16, 1024], BF16)
            wb = S.sb("mg_wb", [128, 16, 1024], BF16)
            yar = S.ring("mg_ya", [128, 16, 512], BF16, 2)
            ybr = S.ring("mg_yb", [128, 16, 512], BF16, 2)
            gr = S.ring("mg_g", [128, 2, 512], BF16, 3)
            t1r = S.ring("mg_t1", [128, 512], F32, 2)
            t2r = S.ring("mg_t2", [128, 512], F32, 2)
            mr = S.ring("mg_m", [128, 512], BF16, 3)
            psr = S.psring("mg_ps", [128, 512], F32, 4)
            for ccg in range(4):
                for h in range(16):
                    S.dma("pool", wa[:, h, :], wa_src[:, h, ccg * 1024:(ccg + 1) * 1024], w=[wa])
                    S.dma("pool", wb[:, h, :], wb_src[:, h, ccg * 1024:(ccg + 1) * 1024], w=[wb])
                for tt in range(4):
                    yat = yar.next()
                    ybt = ybr.next()
                    S.dma("sp", yat[:], ya[0:16, :, tt * 512:(tt + 1) * 512].rearrange("h p q -> p h q"),
                          r=[self.yT_all], w=[yat])
                    S.dma("sp", ybt[:], ya[16:32, :, tt * 512:(tt + 1) * 512].rearrange("h p q -> p h q"),
                          r=[self.yT_all], w=[ybt])
                    for ci in range(8):
                        cc = ccg * 8 + ci
                        gt = gr.next()
                        S.dma("sp", gt[:, 0, :], qTa[48 + cc, :, tt * 512:(tt + 1) * 512], r=[self.qT_all], w=[gt])
                        S.dma("sp", gt[:, 1, :], qTa[80 + cc, :, tt * 512:(tt + 1) * 512], r=[self.qT_all], w=[gt])
                        pA = psr.next()
                        for h in range(16):
                            S.op("pe", "matmul", pA[:], lhsT=wa[:, h, ci * 128:(ci + 1) * 128], rhs=yat[:, h, :],
                                 start=(h == 0), stop=(h == 15), r=[wa, yat], w=[pA])
                        pB = psr.next()
                        for h in range(16):
                            S.op("pe", "matmul", pB[:], lhsT=wb[:, h, ci * 128:(ci + 1) * 128], rhs=ybt[:, h, :],
                                 start=(h == 0), stop=(h == 15), r=[wb, ybt], w=[pB])
                        t1 = t1r.next()
                        t2 = t2r.next()
                        S.op("dve", "tensor_tensor", t1[:], pA[:], gt[:, 0, :], ALU.mult, r=[pA, gt], w=[t1])
                        S.op("dve", "tensor_tensor", t2[:], pB[:], gt[:, 1, :], ALU.mult, r=[pB, gt], w=[t2])
                        m = mr.next()
                        S.op("pool", "tensor_tensor", m[:], t1[:], t2[:], ALU.add, r=[t1, t2], w=[m])
                        S.dma("sp", self.mT_d.t.ap()[cc, :, tt * 512:(tt + 1) * 512], m[:], r=[m], w=[self.mT_d])
        with S.scope():
            gtB = S.sb("mo_gtB", [128, D], F32)
            self.row_bcast(gtB, 64)
            wo_src = self.din("w_out", [D, D]).ap().rearrange("(kc p) n -> p kc n", p=128)
            mtr = S.ring("mo_mt", [128, KC, 512], BF16, 2)
            wor = S.ring("mo_wo", [128, KC, 512], BF16, 2)
            xsr = S.ring("mo_xs", [128, 512], F32, 4)
            tmpr = S.ring("mo_tmp", [128, 512], F32, 3)
            psr = S.psring("mo_ps", [128, 512], F32, 4)
            for ct in range(8):
                wo = wor.next()
                sl = slice(ct * 512, (ct + 1) * 512)
                for kc in range(KC):
                    S.dma("pool", wo[:, kc, :], wo_src[:, kc, sl], w=[wo])
                for tg in range(4):
                    mt = mtr.next()
                    S.dma("sp", mt[:], self.mT_d.t.ap()[:, :, tg * 512:(tg + 1) * 512].rearrange("c p q -> p c q"),
                          r=[self.mT_d], w=[mt])
                    for tb in range(4):
                        r0 = (tg * 4 + tb) * 128
                        xs = xsr.next()
                        S.dma("act", xs[:], self.i_xown.ap()[r0:r0 + 128, sl], w=[xs])
                        ps = psr.next()
                        for kc in range(KC):
                            S.op("pe", "matmul", ps[:], lhsT=mt[:, kc, tb * 128:(tb + 1) * 128], rhs=wo[:, kc, :],
                                 start=(kc == 0), stop=(kc == KC - 1), r=[mt, wo], w=[ps])
                        tmp = tmpr.next()
                        S.op("dve", "tensor_tensor", tmp[:], ps[:], gtB[:, sl], ALU.mult, r=[ps, gtB], w=[tmp])
                        S.op("pool", "tensor_tensor", tmp[:], tmp[:], xs[:], ALU.add, r=[tmp, xs], w=[tmp])
                        S.dma("sp", self.x1_d.t.ap()[r0:r0 + 128, sl], tmp[:], r=[tmp], w=[self.x1_d])
        self.phase1(self.x1_d.t, self.hT2_own, NJ, AB_off=64, src_tok=self.x1_d)


    def peer(self):
        S = self.S
        NE = 16384
        self.uT_d = S.dram("uT_d", [128, 128, KC, 128], BF16)
        self.qpT_d = S.dram("qpT_d", [16, 128, NOWN], BF16)
        u_src = self.din("peer_u", [NE, D]).ap()
        v_src = self.din("peer_v", [NE, D]).ap()
        self.vbf_d = S.dram("vbf_d", [NE, D], BF16)
        with S.scope():
            vcr = S.ring("pu_v", [128, D], BF16, 2)
            ur = S.ring("pu_u", [128, D], BF16, 2)
            utr = S.ring("pu_ut", [128, KC, 128], BF16, 2)
            psr = S.psring("pu_ps", [128, 1024], BF16, 3)
            nec = 128
            for ec in range(nec):
                ut = ur.next()
                S.dma("pool", ut[:], u_src[ec * 128:(ec + 1) * 128, :], w=[ut])
                utt = utr.next()
                for k4 in range(4):
                    pt = psr.next()
                    for i in range(8):
                        kc = k4 * 8 + i
                        S.op("pe", "transpose", pt[:, i * 128:(i + 1) * 128], ut[:, kc * 128:(kc + 1) * 128],
                             self.ident[:], r=[ut, self.ident], w=[pt])
                    dst = utt[:, k4 * 8:(k4 + 1) * 8, :].rearrange("p a b -> p (a b)")
                    if k4 % 2 == 0:
                        S.op("act", "activation", dst, pt[:], AF.Identity, r=[pt], w=[utt])
                    else:
                        S.op("dve", "tensor_copy", dst, pt[:], r=[pt], w=[utt])
                S.dma("sp", self.uT_d.t.ap()[ec], utt[:], r=[utt], w=[self.uT_d])
                vc_ = vcr.next()
                S.dma("pool", vc_[:], v_src[ec * 128:(ec + 1) * 128, :], w=[vc_])
                S.dma("sp", self.vbf_d.t.ap()[ec * 128:(ec + 1) * 128, :], vc_[:], r=[vc_], w=[self.vbf_d])
        with S.scope():
            hts = [S.sb(f"pq_h{i}", [128, KC, 512], BF16) for i in range(2)]
            wr = S.ring("pq_w", [128, KC, 512], BF16, 2)
            psr = S.psring("pq_ps", [128, 512], F32, 3)
            otr = S.ring("pq_ot", [128, 512], BF16, 3)
            for half in range(2):
                for i in range(2):
                    S.dma("sp", hts[i][:], self.hT2_own.t.ap()[half * 2 + i], r=[self.hT2_own], w=[hts[i]])
                for gq in range(4):
                    wt = wr.next()
                    self.load_w(self.din("w_peer_q", [D, 2048]) if "w_peer_q" not in self.inp else self.inp["w_peer_q"],
                                gq * 512, 512, wt)
                    for i in range(2):
                        tt = half * 2 + i
                        for ci in range(4):
                            ch = gq * 4 + ci
                            ps = psr.next()
                            for kc in range(KC):
                                S.op("pe", "matmul", ps[:], lhsT=wt[:, kc, ci * 128:(ci + 1) * 128],
                                     rhs=hts[i][:, kc, :], start=(kc == 0), stop=(kc == KC - 1),
                                     r=[wt, hts[i]], w=[ps])
                            ot = otr.next()
                            S.op("act", "activation", ot[:], ps[:], AF.Identity, r=[ps], w=[ot])
                            S.dma("sp", self.qpT_d.t.ap()[ch, :, tt * 512:(tt + 1) * 512], ot[:], r=[ot],
                                  w=[self.qpT_d])
        with S.scope():
            keysT = S.sb("pe_keys", [128, 16, 128], BF16)
            S.dma("pool", keysT[:], self.din("peer_keysT", [16, 128, 128]).ap().rearrange("c d n -> d c n"),
                  w=[keysT])
            ngrp = 8 if "small" not in self.dbg else 1
            nec = 128
            for tg in range(ngrp):
                with S.scope():
                    actT = S.sb("pe_actT", [128, 128, 256], BF16)
                    with S.scope():
                        W = [S.sb(f"pe_W{i}", [128, NE], BF16) for i in range(2)]
                        with S.scope():
                            qp = S.sb("pe_qp", [128, 16, 256], BF16)
                            S.dma("sp", qp[:], self.qpT_d.t.ap()[:, :, tg * 256:(tg + 1) * 256]
                                  .rearrange("c p q -> p c q"), r=[self.qpT_d], w=[qp])
                            ssb = S.sb("pe_s", [128, 16, 128], F32)
                            pss = S.psring("pe_pss", [128, 512], F32, 2)
                            S4 = S.ring("pe_S4", [128, 1024], F32, 3)
                            E4 = S.ring("pe_E4", [128, 1024], BF16, 5)
                            sm = {k: S.sb("pe_" + k, [128, n], F32) for k, n in
                                  (("t1", 16), ("t2", 16), ("sr", 128), ("cand", 256), ("cr", 256), ("tc", 16),
                                   ("e16", 16), ("den", 1), ("nthr", 1), ("bias", 1), ("thr", 1))}
                            for tb in range(2):
                                for c4 in range(4):
                                    ps = pss.next()
                                    for i in range(4):
                                        ch = c4 * 4 + i
                                        S.op("pe", "matmul", ps[:, i * 128:(i + 1) * 128],
                                             lhsT=qp[:, ch, tb * 128:(tb + 1) * 128], rhs=keysT[:, ch, :],
                                             start=True, stop=True, r=[qp, keysT], w=[ps])
                                    S.op("act", "activation", ssb[:, c4 * 4:(c4 + 1) * 4, :].rearrange("p a b -> p (a b)"),
                                         ps[:], AF.Identity, r=[ps], w=[ssb])
                                for h in range(8):
                                    s1 = ssb[:, 2 * h, :]
                                    s2 = ssb[:, 2 * h + 1, :]
                                    for (sx, tx) in ((s1, sm["t1"]), (s2, sm["t2"])):
                                        S.op("dve", "max", tx[:, 0:8], sx, r=[ssb], w=[tx])
                                        S.op("dve", "match_replace", sm["sr"][:], tx[:, 0:8], sx, -3.0e38,
                                             r=[ssb, tx], w=[sm["sr"]])
                                        S.op("dve", "max", tx[:, 8:16], sm["sr"][:], r=[sm["sr"]], w=[tx])
                                    cand = sm["cand"]
                                    S.op("dve", "tensor_tensor", cand[:].rearrange("p (a b) -> p a b", a=16),
                                         sm["t1"][:].unsqueeze(2).broadcast_to([128, 16, 16]),
                                         sm["t2"][:].unsqueeze(1).broadcast_to([128, 16, 16]), ALU.add,
                                         r=[sm["t1"], sm["t2"]], w=[cand])
                                    tc_ = sm["tc"]
                                    S.op("dve", "max", tc_[:, 0:8], cand[:], r=[cand], w=[tc_])
                                    S.op("dve", "match_replace", sm["cr"][:], tc_[:, 0:8], cand[:], -3.0e38,
                                         r=[cand, tc_], w=[sm["cr"]])
                                    S.op("dve", "max", tc_[:, 8:16], sm["cr"][:], r=[sm["cr"]], w=[tc_])
                                    thr, nthr, den, bias = sm["thr"], sm["nthr"], sm["den"], sm["bias"]
                                    S.op("dve", "tensor_copy", thr[:], tc_[:, 15:16], r=[tc_], w=[thr])
                                    S.op("dve", "tensor_scalar", nthr[:], tc_[:, 15:16], -1.0, None, ALU.mult,
                                         r=[tc_], w=[nthr])
                                    S.op("act", "activation", sm["e16"][:], tc_[:], AF.Exp, bias=nthr[:, 0:1],
                                         accum_out=den[:], r=[tc_, nthr], w=[sm["e16"], den])
                                    S.op("act", "activation", den[:], den[:], AF.Ln, r=[den], w=[den])
                                    S.op("dve", "tensor_tensor", bias[:], nthr[:], den[:], ALU.subtract,
                                         r=[nthr, den], w=[bias])
                                    pend = {}

                                    def gate_a(q8, h=h, tb=tb, s1=s1, s2=s2, bias=bias, thr=thr, pend=pend):
                                        i0 = q8 * 8
                                        s4 = S4.next()
                                        S.op("pool" if q8 % 2 else "dve", "tensor_tensor",
                                             s4[:].rearrange("p (a b) -> p a b", a=8),
                                             s1[:, i0:i0 + 8].unsqueeze(2).broadcast_to([128, 8, 128]),
                                             s2.unsqueeze(1).broadcast_to([128, 8, 128]), ALU.add,
                                             r=[ssb], w=[s4])
                                        e4 = E4.next()
                                        S.op("act", "activation", e4[:], s4[:], AF.Exp, bias=bias[:, 0:1],
                                             r=[s4, bias], w=[e4])
                                        wsl = W[tb][:, q8 * 1024:(q8 + 1) * 1024]
                                        if h == 0:
                                            S.op("dve", "scalar_tensor_tensor", wsl, s4[:], thr[:, 0:1], e4[:],
                                                 ALU.is_ge, ALU.mult, r=[s4, thr, e4], w=[W[tb]])
                                        else:
                                            S.op("dve", "scalar_tensor_tensor", e4[:], s4[:], thr[:, 0:1], e4[:],
                                                 ALU.is_ge, ALU.mult, r=[s4, thr, e4], w=[e4])
                                            pend[q8] = e4

                                    def gate_b(q8, tb=tb, pend=pend):
                                        e4 = pend.pop(q8)
                                        wsl = W[tb][:, q8 * 1024:(q8 + 1) * 1024]
                                        S.op("pool", "tensor_tensor", wsl, wsl, e4[:], ALU.add,
                                             r=[W[tb], e4], w=[W[tb]])

                                    for n in range(16 + 2):
                                        if n < 16:
                                            gate_a(n)
                                        if n >= 2 and h > 0:
                                            gate_b(n - 2)
                        with S.scope():
                            h2 = S.sb("pe_h2", [128, KC, 256], BF16)
                            S.dma("sp", h2[:], self.hT2_own.t.ap()[tg // 2, :, :, (tg % 2) * 256:(tg % 2 + 1) * 256],
                                  r=[self.hT2_own], w=[h2])
                            uTr = S.ring("pe_uT", [128, KC, 128], BF16, 2)
                            psa = S.psring("pe_psa", [128, 256], F32, 2)
                            pst = S.psring("pe_pst", [128, 256], BF16, 2)
                            xg = S.ring("pe_xg", [128, 256], F32, 2)
                            tg_ = S.ring("pe_tg", [128, 256], F32, 2)
                            gl = S.ring("pe_gl", [128, 256], F32, 2)
                            for ec in range(nec):
                                uT = uTr.next()
                                S.dma("sp" if ec % 2 == 0 else "pool", uT[:], self.uT_d.t.ap()[ec],
                                      r=[self.uT_d], w=[uT])
                                pa = psa.next()
                                for kc in range(KC):
                                    S.op("pe", "matmul", pa[:], lhsT=uT[:, kc, :], rhs=h2[:, kc, :],
                                         start=(kc == 0), stop=(kc == KC - 1), r=[uT, h2], w=[pa])
                                pt = pst.next()
                                for tb in range(2):
                                    S.op("pe", "transpose", pt[:, tb * 128:(tb + 1) * 128],
                                         W[tb][:, ec * 128:(ec + 1) * 128], self.ident[:],
                                         r=[W[tb], self.ident], w=[pt])
                                x = xg.next()
                                S.op("act", "activation", x[:], pa[:], AF.Identity, r=[pa], w=[x])
                                g_ = gl.next()
                                self.gelu_tanh(g_, x, 256, tg_.next())
                                S.op("dve", "tensor_tensor", actT[:, ec, :], g_[:], pt[:], ALU.mult,
                                     r=[g_, pt], w=[actT])
                    with S.scope():
                        gtB = S.sb("pe_gtB", [128, D], F32)
                        self.row_bcast(gtB, 160)
                        vr = S.ring("pe_v", [128, 1024], BF16, 4)
                        x1 = [S.sb(f"pe_x1{i}", [128, D], F32) for i in range(2)]
                        pso = [S.ps(f"pe_pso{i}", [128, 512], F32) for i in range(4)]
                        tmpr = S.ring("pe_tmp", [128, 512], F32, 2)
                        for tb in range(2):
                            r0 = tg * 256 + tb * 128
                            S.dma("sp", x1[tb][:], self.x1_d.t.ap()[r0:r0 + 128, :], r=[self.x1_d], w=[x1[tb]])
                        for dq in range(4):
                            for ec in range(nec):
                                vt = vr.next()
                                S.dma("sp" if ec % 2 == 0 else "act", vt[:],
                                      self.vbf_d.t.ap()[ec * 128:(ec + 1) * 128, dq * 1024:(dq + 1) * 1024],
                                      r=[self.vbf_d], w=[vt])
                                for tb in range(2):
                                    for d2 in range(2):
                                        S.op("pe", "matmul", pso[tb * 2 + d2][:],
                                             lhsT=actT[:, ec, tb * 128:(tb + 1) * 128],
                                             rhs=vt[:, d2 * 512:(d2 + 1) * 512], start=(ec == 0), stop=(ec == nec - 1),
                                             r=[actT, vt], w=[pso[tb * 2 + d2]])
                            for tb in range(2):
                                for d2 in range(2):
                                    sl = slice(dq * 1024 + d2 * 512, dq * 1024 + (d2 + 1) * 512)
                                    tmp = tmpr.next()
                                    S.op("dve", "tensor_tensor", tmp[:], pso[tb * 2 + d2][:], gtB[:, sl], ALU.mult,
                                         r=[pso[tb * 2 + d2], gtB], w=[tmp])
                                    S.op("pool", "tensor_tensor", x1[tb][:, sl], x1[tb][:, sl], tmp[:], ALU.add,
                                         r=[x1[tb], tmp], w=[x1[tb]])
                        out = self.outs["out"]
                        for tb in range(2):
                            r0 = tg * 256 + tb * 128
                            S.dma("sp", out[0].ap()[r0:r0 + 128, :], x1[tb][:], r=[x1[tb]], w=[out[1]])


def rel_bucket_np(d):
    n = np.maximum(d, 0)
    nf = np.maximum(n, 1).astype(np.float32)
    lb = 16 + (np.log(nf / np.float32(16)) / np.float32(np.log(2048 / 16)) * np.float32(16)).astype(np.int32)
    return np.where(n < 16, n, np.minimum(lb, 31))


def _bias_tile(rel_bias, heads0, dist, valid):
    bk = rel_bucket_np(dist)
    vals = rel_bias[bk][:, :, heads0:heads0 + 16]
    vals = np.where(valid[:, :, None], vals, np.float32(-30000.0))
    return np.ascontiguousarray(vals.reshape(128, 128, 4, 4).transpose(2, 0, 3, 1).reshape(4, 128, 512)).astype(np.float32)


def make_tables(rel_bias, c):
    k = np.arange(128)[:, None]
    q = np.arange(128)[None, :]
    out = {}
    for name, h0 in (("dsa", 0), ("slc", 16)):
        tiles = []
        for op in range(21):
            dist = 128 * (op + c - 7) + q - k
            tiles.append(_bias_tile(rel_bias, h0, dist, dist >= 0))
        out["tab_" + name] = np.stack(tiles)
    tiles = []
    for w in range(12):
        dist = 128 * (c + 4 - w) + q - k
        tiles.append(_bias_tile(rel_bias, 16, dist, (dist >= 0) & (dist < 512)))
    out["tab_win"] = np.stack(tiles)
    tiles = []
    for e8 in range(5):
        e = 8 * e8 if e8 < 4 else 64
        dist = 128 * (e + c) + q - (16 * k + 31)
        tiles.append(_bias_tile(rel_bias, 16, dist, dist >= 0))
    out["tab_cmp"] = np.stack(tiles)
    ft = np.zeros((NJ, 128, 256), np.float32)
    mm = np.arange(256)[None, :]
    for j in range(NJ):
        tq = 128 * (8 * j + c) + np.arange(128)[:, None]
        cur = tq // 64
        forced = (mm == 0) | (mm == cur) | (mm == cur - 1)
        adm = mm * 64 <= tq
        ft[j] = np.where(adm, np.where(forced, 1e9, 0.0), -1e30)
    out["ftab"] = ft
    kk = np.arange(1024)[None, :]
    qq = np.arange(128)[:, None]
    out["cmask"] = np.where(kk <= 128 * c + qq, 0.0, -1e30).astype(np.float32)
    return out

def make_consts():
    n = np.arange(1024)[:, None]
    m = np.arange(256)[None, :]
    ov = ((16 * n < 64 * m + 64) & (16 * n + 32 > 64 * m) & (n < 1023)).astype(np.float32)
    cidx = np.arange(8192)[None, :]
    yexp = (np.arange(128)[:, None] == cidx // 64).astype(np.float32)
    return {"ov": ov, "yexp": yexp}


IN_SPLITS = (2048, 512, 512, 2048, 64, 32, 2048, 3072, 48, 8192)


def prep_inputs(inp):
    offs = np.concatenate([[0], np.cumsum(IN_SPLITS)])
    w_in = inp["w_in"][0]
    seg = lambda i: w_in[:, offs[i]:offs[i + 1]]
    qa, ka, va, qi, ki, wi, qb, kvb, gb, gm = [seg(i) for i in range(10)]
    kv = lambda i: kvb[:, i * 512:(i + 1) * 512]
    w_kT = np.ascontiguousarray(np.concatenate([ka, ki, ki, kv(0), kv(1), kv(2), kv(4)], axis=1))
    w_v = np.ascontiguousarray(np.concatenate([va, kv(3), kv(5)], axis=1))
    w_qT = np.ascontiguousarray(np.concatenate([qa, qi, qb, gm], axis=1))
    w_qs = np.ascontiguousarray(np.concatenate([wi, gb], axis=1))
    colT = lambda v: np.ascontiguousarray(v.reshape(-1, 128).T)
    x = inp["x"][0]
    shared = {
        "xfull": x,
        "cT": colT(inp["c"][0]),
        "w_ada": inp["w_ada"][0],
        "b_adaT": colT(inp["b_ada"][0]),
        "gnT": np.ascontiguousarray(np.concatenate([colT(inp["g_mix"][0]), colT(inp["g_ffn"][0])], axis=1)),
        "gains": np.ascontiguousarray(np.stack([inp[k][0] for k in
                                                ("gq_a", "gk_a", "gq_b", "gk_cmp", "gk_slc", "gk_win")], axis=1)),
        "w_kT": w_kT, "w_v": w_v, "w_qT": w_qT, "w_qs": w_qs,
        "cmp_w1_k": inp["cmp_w1_k"][0], "cmp_w1_v": inp["cmp_w1_v"][0],
        "cmp_w2_k": inp["cmp_w2_k"][0], "cmp_w2_v": inp["cmp_w2_v"][0],
        "cmp_peT_k": np.ascontiguousarray(inp["cmp_pe_k"][0].T), "cmp_peT_v": np.ascontiguousarray(inp["cmp_pe_v"][0].T),
        "w_branch_a": inp["w_branch_a"][0], "w_branch_b": inp["w_branch_b"][0], "w_out": inp["w_out"][0],
        "w_peer_q": inp["w_peer_q"][0],
        "peer_keysT": np.ascontiguousarray(inp["peer_sub_keys"][0].reshape(16, 128, 128).transpose(0, 2, 1)),
        "peer_u": inp["peer_u"][0], "peer_v": inp["peer_v"][0],
    }
    shared.update(make_consts())
    maps = []
    xb = x.reshape(NTB, 128, D)
    for c in range(NCORES):
        m = dict(shared)
        m["xown"] = np.ascontiguousarray(xb[c::8].reshape(NOWN, D))
        m.update(make_tables(inp["rel_bias"], c))
        maps.append(m)
    return maps


_CACHE = {}


def kernel(**inputs):
    inputs = {k: np.asarray(v) for k, v in inputs.items()}
    if "prog" not in _CACHE:
        p = Prog()
        p.build()
        _CACHE["prog"] = p
    p = _CACHE["prog"]
    maps = prep_inputs(inputs)
    maps = [{k: np.ascontiguousarray(v, dtype=np.float32) for k, v in m.items() if k in p.inp} for m in maps]
    res = run_bass_kernel_spmd(p.nc, maps, core_ids=list(range(NCORES)))
    out = np.zeros((1, T, D), np.float32)
    ob = out[0].reshape(NTB, 128, D)
    for c in range(NCORES):
        ob[c::8] = res.results[c]["out"].reshape(NJ, 128, D)
    return out
```
